# Optimizing a Trainium2 kernel written in Bass

```python
import math
import jax, jax.numpy as jnp
from jax import lax
import numpy as np

D_MODEL = 1024
BATCH = 16
SEQ = 2048
DEPTH = 2

GRID_W = 64
CTX_LEN = 256
N_MIXERS = 2
NA_HEADS = 16
NA_HEAD_DIM = D_MODEL // NA_HEADS
NA_WIN_R = 8
NA_WIN_C = 16
NA_QCOLS = 16
NA_KCOLS = NA_QCOLS + NA_WIN_C
DIFF_HEADS = 8
DIFF_HEAD_DIM = 64
DIFF_V_DIM = 2 * DIFF_HEAD_DIM
Q_BLOCK = 128
ROPE_THETA = 10000.0
ROPE_AXIS_DIM = DIFF_HEAD_DIM // 2
D_FF = 2816
N_EXPERTS = 8
TOP_K = 2
D_FF_EXPERT = 3584
NORM_EPS = 1e-6

kernel_name = "hybrid_natten_diffattn_moe_dit"


def rmsnorm(x, g):
    x32 = x.astype(jnp.float32)
    y = x32 * lax.rsqrt(jnp.mean(x32 * x32, axis=-1, keepdims=True) + NORM_EPS)
    return y.astype(x.dtype) * g


def adaln_params(cond, w_ada, b_ada):
    return jnp.split(jax.nn.silu(cond) @ w_ada + b_ada, 6, axis=-1)


def modulate(h, shift, scale):
    return h * (1 + scale) + shift


def swiglu(h, w_gate, w_up, w_down):
    return (jax.nn.silu(h @ w_gate) * (h @ w_up)) @ w_down


def moe_swiglu(h, w_router, w_gate, w_up, w_down):
    logits = (h @ w_router).astype(jnp.float32)
    top_vals, top_idx = lax.top_k(logits, TOP_K)
    top_w = jax.nn.softmax(top_vals, axis=-1)
    gates = jnp.sum(jax.nn.one_hot(top_idx, N_EXPERTS, dtype=jnp.float32) * top_w[..., None], axis=-2)
    gates = gates.astype(h.dtype)
    y = jnp.zeros_like(h)
    for e in range(N_EXPERTS):
        y = y + gates[..., e:e + 1] * swiglu(h, w_gate[e], w_up[e], w_down[e])
    return y


def full_attention(q, k, v):
    s = jnp.einsum('bqhd,bkhd->bhqk', q, k) * (q.shape[-1] ** -0.5)
    p = jax.nn.softmax(s.astype(jnp.float32), axis=-1).astype(v.dtype)
    return jnp.einsum('bhqk,bkhd->bqhd', p, v)


def neighbourhood_tables(rows):
    wr = min(NA_WIN_R, rows)
    nqb = GRID_W // NA_QCOLS
    qcols = np.arange(GRID_W).reshape(nqb, NA_QCOLS)
    kstart = np.clip(NA_QCOLS * np.arange(nqb) - (NA_KCOLS - NA_QCOLS) // 2, 0, GRID_W - NA_KCOLS)
    kcols = kstart[:, None] + np.arange(NA_KCOLS)[None, :]
    wstart = np.clip(qcols - NA_WIN_C // 2, 0, GRID_W - NA_WIN_C)
    kc = kcols[:, None, :]
    mask = (kc >= wstart[..., None]) & (kc < wstart[..., None] + NA_WIN_C)
    mask = np.broadcast_to(mask[:, :, None, :], (nqb, NA_QCOLS, wr, NA_KCOLS)).reshape(nqb, NA_QCOLS, wr * NA_KCOLS)
    dc = np.clip(kc - qcols[..., None], -(NA_WIN_C - 1), NA_WIN_C - 1) + (NA_WIN_C - 1)
    return wr, nqb, kcols, mask, dc


def neighbourhood_attention(h, hc, w_qkv, rpb, w_o, ctx_queries):
    B, S, _ = h.shape
    C = hc.shape[1]
    rows = S // GRID_W
    H, dh = NA_HEADS, NA_HEAD_DIM
    scale = dh ** -0.5
    q, k, v = jnp.split(h @ w_qkv, 3, axis=-1)
    qc, kc, vc = jnp.split(hc @ w_qkv, 3, axis=-1)
    q_grid = q.reshape(B, rows, GRID_W, H, dh)
    k_grid = k.reshape(B, rows, GRID_W, H, dh)
    v_grid = v.reshape(B, rows, GRID_W, H, dh)
    kc = kc.reshape(B, C, H, dh)
    vc = vc.reshape(B, C, H, dh)
    wr, nqb, kcols, mask, dc = neighbourhood_tables(rows)
    n_lat = wr * NA_KCOLS

    def row_step(r):
        rs = jnp.clip(r - wr // 2, 0, rows - wr)
        q_r = lax.dynamic_index_in_dim(q_grid, r, axis=1, keepdims=False).reshape(B, nqb, NA_QCOLS, H, dh)
        k_band = lax.dynamic_slice_in_dim(k_grid, rs, wr, axis=1)[:, :, kcols]
        v_band = lax.dynamic_slice_in_dim(v_grid, rs, wr, axis=1)[:, :, kcols]
        k_blk = jnp.swapaxes(k_band, 1, 2).reshape(B, nqb, n_lat, H, dh)
        v_blk = jnp.swapaxes(v_band, 1, 2).reshape(B, nqb, n_lat, H, dh)
        dr = rs + jnp.arange(wr) - r + (NA_WIN_R - 1)
        bias = jnp.take(rpb[:, dr], dc, axis=2)
        bias = jnp.transpose(bias, (0, 2, 3, 1, 4)).reshape(H, nqb, NA_QCOLS, n_lat)
        s_lat = jnp.einsum('bnqhd,bnkhd->bhnqk', q_r, k_blk) * scale
        s_lat = jnp.where(mask, s_lat.astype(jnp.float32) + bias.astype(jnp.float32), -jnp.inf)
        s_ctx = (jnp.einsum('bnqhd,bkhd->bhnqk', q_r, kc) * scale).astype(jnp.float32)
        p = jax.nn.softmax(jnp.concatenate([s_lat, s_ctx], axis=-1), axis=-1).astype(v.dtype)
        o = (jnp.einsum('bhnqk,bnkhd->bnqhd', p[..., :n_lat], v_blk)
             + jnp.einsum('bhnqk,bkhd->bnqhd', p[..., n_lat:], vc))
        return o.reshape(B, GRID_W, H * dh)

    o = lax.map(row_step, jnp.arange(rows))
    y = jnp.moveaxis(o, 0, 1).reshape(B, S, H * dh) @ w_o
    yc = None
    if ctx_queries:
        oc = full_attention(qc.reshape(B, C, H, dh), kc, vc)
        yc = oc.reshape(B, C, H * dh) @ w_o
    return y, yc


def rope_rotate(x, cos, sin):
    x1, x2 = jnp.split(x, 2, axis=-1)
    return jnp.concatenate([x1 * cos - x2 * sin, x2 * cos + x1 * sin], axis=-1)


def axial_rope(x, rows_pos, cols_pos):
    inv_freq = ROPE_THETA ** (-jnp.arange(0, ROPE_AXIS_DIM, 2, dtype=jnp.float32) / ROPE_AXIS_DIM)
    ang_r = (rows_pos[:, None] * inv_freq)[:, None, None, :]
    ang_c = (cols_pos[:, None] * inv_freq)[:, None, None, :]
    cr, sr = jnp.cos(ang_r).astype(x.dtype), jnp.sin(ang_r).astype(x.dtype)
    cc, sc = jnp.cos(ang_c).astype(x.dtype), jnp.sin(ang_c).astype(x.dtype)
    return jnp.concatenate([rope_rotate(x[..., :ROPE_AXIS_DIM], cr, sr),
                            rope_rotate(x[..., ROPE_AXIS_DIM:], cc, sc)], axis=-1)


def diff_attend(q, k, v, lam):
    s = jnp.einsum('bqhmd,bkhmd->bhmqk', q, k) * (DIFF_HEAD_DIM ** -0.5)
    p = jax.nn.softmax(s.astype(jnp.float32), axis=-1)
    a = (p[:, :, 0] - lam * p[:, :, 1]).astype(v.dtype)
    return jnp.einsum('bhqk,bkhe->bqhe', a, v)


def head_rmsnorm(o, g):
    o32 = o.astype(jnp.float32)
    y = o32 * lax.rsqrt(jnp.mean(o32 * o32, axis=-1, keepdims=True) + NORM_EPS)
    return y.astype(o.dtype) * g


def diff_attention(h, hc, layer_idx, w_qkv, lq1, lk1, lq2, lk2, subln, w_o, ctx_queries):
    B, S, _ = h.shape
    C = hc.shape[1]
    H, dh, E = DIFF_HEADS, DIFF_HEAD_DIM, DIFF_V_DIM
    lam_init = 0.8 - 0.6 * math.exp(-0.3 * layer_idx)
    lam = (jnp.exp(jnp.sum(lq1.astype(jnp.float32) * lk1.astype(jnp.float32)))
           - jnp.exp(jnp.sum(lq2.astype(jnp.float32) * lk2.astype(jnp.float32))) + lam_init)
    q, k, v = jnp.split(h @ w_qkv, 3, axis=-1)
    qc, kc, vc = jnp.split(hc @ w_qkv, 3, axis=-1)
    q = q.reshape(B, S, H, 2, dh)
    k = k.reshape(B, S, H, 2, dh)
    v = v.reshape(B, S, H, E)
    kc = kc.reshape(B, C, H, 2, dh)
    vc = vc.reshape(B, C, H, E)
    pos = jnp.arange(S)
    rows_pos = (pos // GRID_W).astype(jnp.float32)
    cols_pos = (pos % GRID_W).astype(jnp.float32)
    q = axial_rope(q, rows_pos, cols_pos)
    k = axial_rope(k, rows_pos, cols_pos)
    k_all = jnp.concatenate([k, kc], axis=1)
    v_all = jnp.concatenate([v, vc], axis=1)
    nb = S // Q_BLOCK
    q_blocks = jnp.moveaxis(q.reshape(B, nb, Q_BLOCK, H, 2, dh), 1, 0)
    o = lax.map(lambda qb: diff_attend(qb, k_all, v_all, lam), q_blocks)
    o = jnp.moveaxis(o, 0, 1).reshape(B, S, H, E)
    y = (head_rmsnorm(o, subln) * (1 - lam_init)).reshape(B, S, H * E) @ w_o
    yc = None
    if ctx_queries:
        oc = diff_attend(qc.reshape(B, C, H, 2, dh), kc, vc, lam)
        yc = (head_rmsnorm(oc, subln) * (1 - lam_init)).reshape(B, C, H * E) @ w_o
    return y, yc


def setup_inputs(seed: int = 0) -> dict:
    key = jax.random.key(seed)
    ks = iter(jax.random.split(key, 40))
    D = D_MODEL

    def nrm(shape, scale):
        return jax.random.normal(next(ks), shape, jnp.float32) * scale

    def gain(n):
        return 1.0 + nrm((n,), 0.01)

    inp = {}
    inp["x"] = nrm((BATCH, SEQ, D), 1.0)
    inp["c"] = nrm((BATCH, D), 1.0)
    inp["ctx"] = nrm((BATCH, CTX_LEN, D), 1.0)
    inp["c_ctx"] = nrm((D,), 1.0)
    inp["l0_w_ada"] = nrm((D, 6 * D), 0.5 * D ** -0.5)
    inp["l0_b_ada"] = nrm((6 * D,), 0.02)
    inp["l0_norm_mix"] = gain(D)
    inp["l0_w_qkv"] = nrm((D, 3 * D), D ** -0.5)
    inp["l0_rpb"] = nrm((NA_HEADS, 2 * NA_WIN_R - 1, 2 * NA_WIN_C - 1), 0.1)
    inp["l0_w_o"] = nrm((D, D), D ** -0.5)
    inp["l0_norm_ffn"] = gain(D)
    inp["l0_w_gate"] = nrm((D, D_FF), D ** -0.5)
    inp["l0_w_up"] = nrm((D, D_FF), D ** -0.5)
    inp["l0_w_down"] = nrm((D_FF, D), D_FF ** -0.5)
    inp["l1_w_ada"] = nrm((D, 6 * D), 0.5 * D ** -0.5)
    inp["l1_b_ada"] = nrm((6 * D,), 0.02)
    inp["l1_norm_mix"] = gain(D)
    inp["l1_w_qkv"] = nrm((D, 3 * D), D ** -0.5)
    inp["l1_lambda_q1"] = nrm((DIFF_HEAD_DIM,), 0.1)
    inp["l1_lambda_k1"] = nrm((DIFF_HEAD_DIM,), 0.1)
    inp["l1_lambda_q2"] = nrm((DIFF_HEAD_DIM,), 0.1)
    inp["l1_lambda_k2"] = nrm((DIFF_HEAD_DIM,), 0.1)
    inp["l1_subln"] = gain(DIFF_V_DIM)
    inp["l1_w_o"] = nrm((D, D), D ** -0.5)
    inp["l1_norm_ffn"] = gain(D)
    inp["l1_w_router"] = nrm((D, N_EXPERTS), D ** -0.5)
    inp["l1_w_gate"] = nrm((N_EXPERTS, D, D_FF_EXPERT), D ** -0.5)
    inp["l1_w_up"] = nrm((N_EXPERTS, D, D_FF_EXPERT), D ** -0.5)
    inp["l1_w_down"] = nrm((N_EXPERTS, D_FF_EXPERT, D), D_FF_EXPERT ** -0.5)
    inp["final_norm"] = gain(D)
    return inp


def reference(x, c, ctx, c_ctx,
              l0_w_ada, l0_b_ada, l0_norm_mix, l0_w_qkv, l0_rpb, l0_w_o,
              l0_norm_ffn, l0_w_gate, l0_w_up, l0_w_down,
              l1_w_ada, l1_b_ada, l1_norm_mix, l1_w_qkv,
              l1_lambda_q1, l1_lambda_k1, l1_lambda_q2, l1_lambda_k2, l1_subln, l1_w_o,
              l1_norm_ffn, l1_w_router, l1_w_gate, l1_w_up, l1_w_down,
              final_norm):
    layers = [
        dict(w_ada=l0_w_ada, b_ada=l0_b_ada, norm_mix=l0_norm_mix, w_qkv=l0_w_qkv, rpb=l0_rpb,
             w_o=l0_w_o, norm_ffn=l0_norm_ffn, w_gate=l0_w_gate, w_up=l0_w_up, w_down=l0_w_down),
        dict(w_ada=l1_w_ada, b_ada=l1_b_ada, norm_mix=l1_norm_mix, w_qkv=l1_w_qkv,
             lq1=l1_lambda_q1, lk1=l1_lambda_k1, lq2=l1_lambda_q2, lk2=l1_lambda_k2, subln=l1_subln,
             w_o=l1_w_o, norm_ffn=l1_norm_ffn, w_router=l1_w_router,
             w_gate=l1_w_gate, w_up=l1_w_up, w_down=l1_w_down),
    ]
    for i in range(DEPTH):
        p = layers[i]
        ctx_needed = i < DEPTH - 1
        sh1, sc1, g1, sh2, sc2, g2 = adaln_params(c[:, None, :], p["w_ada"], p["b_ada"])
        csh1, csc1, cg1, csh2, csc2, cg2 = adaln_params(c_ctx, p["w_ada"], p["b_ada"])
        h = modulate(rmsnorm(x, p["norm_mix"]), sh1, sc1)
        hc = modulate(rmsnorm(ctx, p["norm_mix"]), csh1, csc1)
        if i % N_MIXERS == 0:
            y, yc = neighbourhood_attention(h, hc, p["w_qkv"], p["rpb"], p["w_o"], ctx_needed)
        else:
            y, yc = diff_attention(h, hc, i, p["w_qkv"], p["lq1"], p["lk1"], p["lq2"], p["lk2"],
                                   p["subln"], p["w_o"], ctx_needed)
        x = x + g1 * y
        if i % 2 == 0:
            ffn = lambda t: swiglu(t, p["w_gate"], p["w_up"], p["w_down"])
        else:
            ffn = lambda t: moe_swiglu(t, p["w_router"], p["w_gate"], p["w_up"], p["w_down"])
        x = x + g2 * ffn(modulate(rmsnorm(x, p["norm_ffn"]), sh2, sc2))
        if ctx_needed:
            ctx = ctx + cg1 * yc
            ctx = ctx + cg2 * ffn(modulate(rmsnorm(ctx, p["norm_ffn"]), csh2, csc2))
    return rmsnorm(x, final_norm)
```

```python
import math
from contextlib import ExitStack

import numpy as np
import concourse.bass as bass
import concourse.mybir as mybir
from concourse.bass_utils import run_bass_kernel_spmd

F32 = mybir.dt.float32
BF16 = mybir.dt.bfloat16
I32 = mybir.dt.int32
AF = mybir.ActivationFunctionType
ALU = mybir.AluOpType
AX = mybir.AxisListType

ENGS = ("pe", "act", "dve", "pool", "sp")
N_DMA_SEMS = 12


class Op:
    __slots__ = ("eng", "fn", "deps", "is_dma", "signal", "sem", "val", "idx", "prev_dma")

    def __init__(self, eng, fn, is_dma, idx):
        self.eng = eng
        self.fn = fn
        self.deps = set()
        self.is_dma = is_dma
        self.signal = is_dma
        self.sem = None
        self.val = 0
        self.idx = idx
        self.prev_dma = None


class Sched:
    def __init__(self, nc):
        self.nc = nc
        self.ops = []
        self.last_w = {}
        self.readers = {}

    def op(self, eng, fn, reads=(), writes=(), dma=False):
        o = Op(eng, fn, dma, len(self.ops))
        self.ops.append(o)
        for t in reads:
            w = self.last_w.get(t)
            if w is not None:
                self._dep(o, w, raw=True)
        for t in writes:
            w = self.last_w.get(t)
            if w is not None:
                self._dep(o, w, raw=False)
            rd = self.readers.get(t)
            if rd:
                for k, r in rd.items():
                    if k == "dma":
                        for rr in r:
                            self._dep(o, rr, raw=False)
                    else:
                        self._dep(o, r, raw=False)
        for t in reads:
            rd = self.readers.setdefault(t, {})
            if dma:
                rd.setdefault("dma", []).append(o)
            else:
                rd[eng] = o
        for t in writes:
            self.last_w[t] = o
            self.readers[t] = {}
        return o

    def _dep(self, o, d, raw):
        if d is o:
            return
        if d.is_dma or o.is_dma:
            o.deps.add(d)
            d.signal = True
            return
        if d.eng == o.eng:
            if raw and o.eng != "pe":
                o.deps.add(d)
                d.signal = True
            return
        o.deps.add(d)
        d.signal = True

    def dma(self, q, out, in_, reads=(), writes=(), **kw):
        return self.op(q, lambda e: e.dma_start(out=out, in_=in_, **kw), reads, writes, dma=True)

    def mm(self, out, lhsT, rhs, start, stop, reads=(), writes=(), **kw):
        return self.op("pe", lambda e: e.matmul(out, lhsT, rhs, start=start, stop=stop, **kw),
                       reads, writes)

    def tr(self, out, in_, ident, reads=(), writes=()):
        return self.op("pe", lambda e: e.transpose(out, in_, ident), reads, writes)

    def emit(self, stack):
        nc = self.nc
        sems = {e: stack.enter_context(nc.semaphore("s_" + e)) for e in ENGS}
        dsems = {}
        dcount = {}
        dnext = {}
        dlast = {}
        cnt = {e: 0 for e in ENGS}
        per_eng = {e: [] for e in ENGS}
        for o in self.ops:
            per_eng[o.eng].append(o)
            if o.is_dma:
                q = o.eng
                if q not in dsems:
                    dsems[q] = [stack.enter_context(nc.semaphore("d_%s%d" % (q, i)))
                                for i in range(N_DMA_SEMS)]
                    dnext[q] = 0
                i = dnext[q]
                dnext[q] = (i + 1) % N_DMA_SEMS
                s = dsems[q][i]
                o.prev_dma = dlast.get(s)
                dlast[s] = o
                dcount[s] = dcount.get(s, 0) + 16
                o.sem = s
                o.val = dcount[s]
            elif o.signal:
                cnt[o.eng] += 1
                o.sem = sems[o.eng]
                o.val = cnt[o.eng]
        all_dma_final = dict(dcount)
        block = stack.enter_context(nc.Block())

        def run(eng_name):
            def body(e):
                waited = {}
                for o in per_eng[eng_name]:
                    need = {}
                    for d in o.deps:
                        if need.get(d.sem, 0) < d.val:
                            need[d.sem] = d.val
                    if o.prev_dma is not None:
                        p = o.prev_dma
                        if need.get(p.sem, 0) < p.val:
                            need[p.sem] = p.val
                    for s, v in need.items():
                        if waited.get(s, 0) < v:
                            e.wait_ge(s, v)
                            waited[s] = v
                    ins = o.fn(e)
                    if o.is_dma:
                        ins.then_inc(o.sem, 16)
                    elif o.signal:
                        ins.then_inc(o.sem, 1)
                if eng_name == "sp":
                    for s, v in all_dma_final.items():
                        if waited.get(s, 0) < v:
                            e.wait_ge(s, v)
            return body

        block.tensor(run("pe"))
        block.scalar(run("act"))
        block.vector(run("dve"))
        block.gpsimd(run("pool"))
        block.sync(run("sp"))

    def barrier(self):
        last = {}
        dmas = []
        for o in self.ops:
            if o.is_dma:
                dmas.append(o)
            else:
                last[o.eng] = o
        self._bar = list(last.values()) + dmas
        self._bar_seen = set()
        self.last_w = {}
        self.readers = {}

    _bar = None
    _bar_seen = None


_orig_op = Sched.op


def _op_with_barrier(self, eng, fn, reads=(), writes=(), dma=False):
    o = _orig_op(self, eng, fn, reads, writes, dma)
    if self._bar is not None and eng not in self._bar_seen:
        self._bar_seen.add(eng)
        for d in self._bar:
            if d is o:
                continue
            if d.is_dma or d.eng != eng:
                o.deps.add(d)
                d.signal = True
    return o


Sched.op = _op_with_barrier

NB = 2
SEQ = 2048
CTX = 256
T = SEQ + CTX
NT = T // 128
NTL = SEQ // 128
D = 1024
KC = 8
DFF = 2816
NE = 8
DFE = 3584
EPS = 1e-6
GRID_W = 64
VW = 1040


class Arena:
    def __init__(self, ap, words):
        self.ap = ap
        self.words = words
        self.top = 0

    def f32(self, n):
        assert self.top + n <= self.words, "arena overflow %d" % (self.top + n)
        a = self.ap[:, self.top:self.top + n]
        self.top += n
        return a

    def bf(self, n):
        assert n % 2 == 0
        return self.f32(n // 2).bitcast(BF16)


class Builder:
    def __init__(self, dbg=False, phases=None):
        self.dbg = dbg
        self.phases = phases
        self.nc = bass.Bass("TRN2", target_bir_lowering=False)
        self.S = Sched(self.nc)
        self.inputs = {}

    def din(self, name, shape, dt=F32):
        t = self.nc.dram_tensor(name, list(shape), dt, kind="ExternalInput").ap()
        self.inputs[name] = t
        return t

    def dscr(self, name, shape, dt):
        kind = "ExternalOutput" if self.dbg else "Internal"
        return self.nc.dram_tensor(name, list(shape), dt, kind=kind).ap()

    def want(self, ph):
        return self.phases is None or ph in self.phases

    def build(self):
        nc = self.nc
        shapes = {"x": [NB, SEQ, D], "ctx": [NB, CTX, D], "c": [NB, D], "c_ctx": [D],
                  "l0_w_gate": [D, DFF], "l0_w_up": [D, DFF], "l0_w_down": [DFF, D],
                  "na_bias": [5, 16, 128, 640], "l1_subln": [128], "l1_w_router": [D, NE],
                  "l1_w_gate": [NE, D, DFE], "l1_w_up": [NE, D, DFE], "l1_w_down": [NE, DFE, D],
                  "final_norm": [D], "ident": [128, 128], "rope_cs": [128, NTL, 64], "rope_sn": [128, NTL, 64]}
        for l in range(2):
            p = "l%d_" % l
            shapes.update({p + "w_ada": [D, 6 * D], p + "b_ada": [6 * D], p + "norm_mix": [D],
                           p + "w_qkv": [D, 3 * D], p + "w_o": [D, D], p + "norm_ffn": [D]})
        for k in ("q1", "k1", "q2", "k2"):
            shapes["l1_lambda_" + k] = [64]
        bld = self

        class Lazy(dict):
            def __missing__(d, name):
                if bld.dbg:
                    d[name] = bld.din(name, shapes[name])
                    return d[name]
                raise KeyError(name)

        I = Lazy()
        if not self.dbg:
            for name in sorted(shapes):
                I[name] = self.din(name, shapes[name])
        self.I = I
        self.out = nc.dram_tensor("out", [NB, SEQ, D], F32, kind="ExternalOutput").ap()
        self.MOD = self.dscr("MOD", [2, 3, 6 * D], F32)
        self.QT = self.dscr("QT", [NB, KC, 128, T], BF16)
        self.KT = self.dscr("KT", [NB, KC, 128, T], BF16)
        self.VS = self.dscr("VS", [NB, T, VW], BF16)
        self.OS = self.dscr("OS", [NB, T, D], BF16)
        self.XR1 = self.dscr("XR1", [NB, T, D], F32)
        self.XR2 = self.dscr("XR2", [NB, T, D], F32)
        self.H2T = self.dscr("H2T", [KC, 128, NB * T], BF16)
        self.GAT = self.dscr("GAT", [NB, SEQ, NE], F32)

        with ExitStack() as st:
            AW = 51200
            arena_t = st.enter_context(nc.sbuf_tensor("arena", [128, AW], F32))
            self.psum = st.enter_context(nc.psum_tensor("psum", [128, 8, 512], F32))
            self.A = Arena(arena_t, AW)
            self.ident_f = self.A.f32(128)
            self.ident = self.A.bf(128)
            S = self.S
            S.dma("sp", self.ident_f, I["ident"], writes=["ident_f"])
            S.dma("pool", self.ident, I["ident"], writes=["ident"])
            self.base = self.A.top
            if self.want("0"):
                self.phase0()
            for l in range(2):
                if self.want("A%d" % l):
                    self.phaseA(l)
                if self.want("B%d" % l):
                    (self.phaseB0 if l == 0 else self.phaseB1)()
                if self.want("C%d" % l):
                    self.phaseC(l)
                if self.want("D%d" % l):
                    (self.phaseD0 if l == 0 else self.phaseD1)()
            S.emit(st)
        return nc

    def new_phase(self):
        self.S.barrier()
        self.A.top = self.base

    def pbank(self, b):
        return self.psum[:, b, :]

    def pbank_bf(self, b, nb=1):
        v = self.psum[:, b:b + nb, :].rearrange("p a b -> p (a b)").bitcast(BF16)
        return v

    def phase0(self):
        S, A, I = self.S, self.A, self.I
        self.new_phase()
        cT = A.f32(24).rearrange("p (k j) -> p k j", k=KC)
        cs = A.f32(24).rearrange("p (k j) -> p k j", k=KC)
        srcs = [I["c"][0], I["c"][1], I["c_ctx"]]
        for j, src in enumerate(srcs):
            S.dma("sp", cT[:, :, j], src.rearrange("(k p) -> p k", p=128), writes=["cT"],
                  allow_slow_non_contiguous=True)
        S.op("act", lambda e: e.activation(cs, cT, AF.Silu), reads=["cT"], writes=["cs"])
        bt = A.f32(6 * D)
        modv = A.f32(6 * D)
        wb = [A.f32(KC * 512).rearrange("p (k n) -> p k n", k=KC) for _ in range(2)]
        it = 0
        for l in range(2):
            S.dma("sp", bt[0:3, :], I["l%d_b_ada" % l].partition_broadcast(3), reads=[], writes=["bt"])
            for n in range(12):
                b = it % 2
                it += 1
                S.dma("sp", wb[b], I["l%d_w_ada" % l][:, n * 512:(n + 1) * 512].rearrange("(k p) n -> p k n", p=128),
                      writes=[("wb", b)])
                pb = self.psum[0:3, n % 2, :]
                for k in range(KC):
                    S.mm(pb, cs[:, k, :], wb[b][:, k, :], k == 0, k == KC - 1,
                         reads=["cs", ("wb", b)], writes=[("p0", n % 2)])
                S.op("dve", lambda e, pb=pb, n=n: e.tensor_tensor(modv[0:3, n * 512:(n + 1) * 512], pb,
                                                                 bt[0:3, n * 512:(n + 1) * 512], ALU.add),
                     reads=[("p0", n % 2), "bt"], writes=["modv"])
            S.dma("sp", self.MOD[l], modv[0:3, :], reads=["modv"], writes=[("MOD", l)])

    def bc_row(self, dst, row, reads, tok):
        self.S.dma("sp", dst, row.partition_broadcast(128), reads=reads, writes=[tok])

    def mod_tiles(self, l, j, idx_scale, idx_shift, gain, tag):
        S, A = self.S, self.A
        tmp = A.f32(D)
        At = A.f32(D)
        SH = A.f32(D)
        self.bc_row(tmp, self.MOD[l, j, idx_scale * D:(idx_scale + 1) * D], [("MOD", l)], ("mtmp", tag))
        self.bc_row(SH, self.MOD[l, j, idx_shift * D:(idx_shift + 1) * D], [("MOD", l)], ("SH", tag))
        S.op("dve", lambda e: e.scalar_tensor_tensor(At, tmp, 1.0, gain, ALU.add, ALU.mult),
             reads=[("mtmp", tag), "gain"], writes=[("A", tag)])
        return At, SH

    def xsrc(self, l, bb, t):
        if l == 0:
            if t < NTL:
                return self.I["x"][bb, t * 128:(t + 1) * 128, :], []
            return self.I["ctx"][bb, (t - NTL) * 128:(t - NTL + 1) * 128, :], []
        return self.XR2[bb, t * 128:(t + 1) * 128, :], [("XR2", bb, t)]

    def rms_mod(self, xt, xtok, At, Atok, SH, SHtok, h_out, h_tok, junk, st, sttok, t1):
        S = self.S
        S.op("act", lambda e: e.activation(junk, xt, AF.Square, accum_out=st[:, 0:1]),
             reads=[xtok], writes=["junk", (sttok, 0)])
        S.op("act", lambda e: e.activation(st[:, 1:2], st[:, 0:1], AF.Sqrt, bias=EPS, scale=1.0 / D),
             reads=[(sttok, 0)], writes=[(sttok, 1)])
        S.op("dve", lambda e: e.reciprocal(st[:, 2:3], st[:, 1:2]), reads=[(sttok, 1)], writes=[(sttok, 2)])
        S.op("dve", lambda e: e.scalar_tensor_tensor(t1, xt, st[:, 2:3], At, ALU.mult, ALU.mult),
             reads=[xtok, (sttok, 2), Atok], writes=["t1"])
        S.op("dve", lambda e: e.tensor_tensor(h_out, t1, SH, ALU.add), reads=["t1", SHtok], writes=[h_tok])

    def phaseA(self, l):
        S, A, I = self.S, self.A, self.I
        self.new_phase()
        H, dv = (16, 64) if l == 0 else (8, 128)
        vw = H * (dv + 1)
        pre = "l%d_" % l
        wqkv = A.bf(KC * 3 * D).rearrange("p (k n) -> p k n", k=KC)
        for n in range(6):
            S.dma("pool", wqkv[:, :, n * 512:(n + 1) * 512],
                  I[pre + "w_qkv"][:, n * 512:(n + 1) * 512].rearrange("(k p) n -> p k n", p=128),
                  writes=[("wqkv", n)])
        gain = A.f32(D)
        self.bc_row(gain, I[pre + "norm_mix"], [], "gain")
        Ac, SHc = self.mod_tiles(l, 2, 1, 0, gain, "c")
        if l == 1:
            cs_t = A.f32(NTL * 64).rearrange("p (t j) -> p t j", t=NTL)
            sn_t = A.f32(NTL * 64).rearrange("p (t j) -> p t j", t=NTL)
            S.dma("sp", cs_t, I["rope_cs"], writes=["rope_cs"])
            S.dma("sp", sn_t, I["rope_sn"], writes=["rope_sn"])
        xts = [A.f32(D) for _ in range(2)]
        junk = A.bf(D)
        stt = A.f32(4)
        t1 = A.f32(D)
        hb = A.bf(D)
        hT = [A.bf(KC * 128).rearrange("p (k n) -> p k n", k=KC) for _ in range(2)]
        if l == 1:
            qk32 = A.f32(2 * D)
            r1 = A.f32(2 * D)
            r2 = A.f32(2 * D)
        qkb = A.bf(2 * D)
        qkT = [A.bf(16 * 128).rearrange("p (k n) -> p k n", k=16) for _ in range(2)]
        vaug = [A.bf(vw) for _ in range(2)]
        for i in range(2):
            S.op("pool", lambda e, i=i: e.memset(vaug[i], 1.0), writes=[("vaug", i)])
        pT = self.pbank_bf(0).rearrange("p (k n) -> p k n", k=KC)
        pQT = self.pbank_bf(5, 2).rearrange("p (k n) -> p k n", k=16)
        it = 0
        for bb in range(NB):
            mark = A.top
            Al, SHl = self.mod_tiles(l, bb, 1, 0, gain, "l")
            for t in range(NT):
                b2 = it % 2
                it += 1
                lat = t < NTL
                src, srd = self.xsrc(l, bb, t)
                xt = xts[b2]
                S.dma("sp", xt, src, reads=srd, writes=[("xt", b2)])
                self.rms_mod(xt, ("xt", b2), Al if lat else Ac, ("A", "l" if lat else "c"),
                             SHl if lat else SHc, ("SH", "l" if lat else "c"), hb, "hb", junk, stt, "stA", t1)
                for k in range(KC):
                    S.tr(pT[:, k, :], hb[:, k * 128:(k + 1) * 128], self.ident, reads=["hb", "ident"], writes=["pT"])
                S.op("act", lambda e, b2=b2: e.copy(hT[b2], pT), reads=["pT"], writes=[("hT", b2)])
                va = vaug[b2].rearrange("p (h d) -> p h d", h=H)
                for n in range(6):
                    pq = self.pbank(1 + n % 4)
                    for k in range(KC):
                        S.mm(pq, hT[b2][:, k, :], wqkv[:, k, n * 512:(n + 1) * 512], k == 0, k == KC - 1,
                             reads=[("hT", b2), ("wqkv", n)], writes=[("pq", n % 4)])
                    if n < 4:
                        dst = (qk32 if l == 1 else qkb)[:, n * 512:(n + 1) * 512]
                        dtok = ("qk", n)
                        if n < 2:
                            S.op("act", lambda e, dst=dst, pq=pq: e.mul(dst, pq, 0.125),
                                 reads=[("pq", n % 4)], writes=[dtok])
                        else:
                            S.op("act", lambda e, dst=dst, pq=pq: e.copy(dst, pq),
                                 reads=[("pq", n % 4)], writes=[dtok])
                    else:
                        hpb = 512 // dv
                        h0 = (n - 4) * hpb
                        S.op("dve", lambda e, pq=pq, h0=h0, hpb=hpb, va=va: e.tensor_copy(
                            va[:, h0:h0 + hpb, 0:dv], pq.rearrange("p (h d) -> p h d", h=hpb)),
                            reads=[("pq", n % 4)], writes=[("vaug", b2)])
                qtoks = [("qk", n) for n in range(4)]
                if l == 1:
                    if lat:
                        xv = qk32.rearrange("p (g j) -> p g j", g=32)
                        csb = cs_t[:, t, :].unsqueeze(1).broadcast_to([128, 32, 64])
                        S.op("pool", lambda e, xv=xv, csb=csb: e.tensor_tensor(
                            r1.rearrange("p (g j) -> p g j", g=32), xv, csb, ALU.mult),
                            reads=qtoks + ["rope_cs"], writes=["r1"])
                        x5 = qk32.rearrange("p (g a h j) -> p g a h j", g=32, a=2, h=2)
                        o5 = r2.rearrange("p (g a h j) -> p g a h j", g=32, a=2, h=2)
                        s4 = sn_t[:, t, :].rearrange("p (a h j) -> p a h j", a=2, h=2)
                        for hh in range(2):
                            snb = s4[:, :, hh, :].unsqueeze(1).broadcast_to([128, 32, 2, 16])
                            S.op("dve", lambda e, hh=hh, snb=snb: e.tensor_tensor(
                                o5[:, :, :, hh, :], x5[:, :, :, 1 - hh, :], snb, ALU.mult),
                                reads=qtoks + ["rope_sn"], writes=[("r2", hh)])
                        S.op("dve", lambda e: e.tensor_tensor(qkb, r1, r2, ALU.add),
                             reads=["r1", ("r2", 0), ("r2", 1)], writes=["qkb"])
                    else:
                        S.op("dve", lambda e: e.tensor_copy(qkb, qk32), reads=qtoks, writes=["qkb"])
                    qkb_r = ["qkb"]
                else:
                    qkb_r = qtoks
                for j in range(16):
                    S.tr(pQT[:, j, :], qkb[:, j * 128:(j + 1) * 128], self.ident, reads=qkb_r + ["ident"],
                         writes=["pQT"])
                S.op("act", lambda e, b2=b2: e.copy(qkT[b2], pQT), reads=["pQT"], writes=[("qkT", b2)])
                S.dma("sp", self.QT[bb, :, :, t * 128:(t + 1) * 128].rearrange("c p n -> p c n"),
                      qkT[b2][:, 0:8, :], reads=[("qkT", b2)], writes=[("QT", bb)])
                S.dma("sp", self.KT[bb, :, :, t * 128:(t + 1) * 128].rearrange("c p n -> p c n"),
                      qkT[b2][:, 8:16, :], reads=[("qkT", b2)], writes=[("KT", bb)])
                S.dma("sp", self.VS[bb, t * 128:(t + 1) * 128, 0:vw], vaug[b2], reads=[("vaug", b2)],
                      writes=[("VS", bb)])
            A.top = mark

    def phaseB0(self):
        S, A, I = self.S, self.A, self.I
        self.new_phase()
        bt32 = A.f32(5 * 2 * 640)
        EB = A.bf(5 * 2 * 640)
        EBv = EB.rearrange("p (v h n) -> p v h n", v=5, h=2)
        QTc = [A.bf(T) for _ in range(2)]
        KTc = [A.bf(T) for _ in range(2)]
        Vc = [A.bf(NT * 130).rearrange("p (t h d) -> p t h d", t=NT, h=2) for _ in range(2)]
        Oc = [A.bf(NT * 128).rearrange("p (t n) -> p t n", t=NT) for _ in range(2)]
        NEB = 3
        E = [A.bf(8 * 128).rearrange("p (j n) -> p j n", j=8) for _ in range(NEB)]
        rec = A.f32(8)
        pS = [self.psum[:, 2 * i:2 * i + 2, :].rearrange("p a (j n) -> p (a j) n", n=128) for i in range(2)]
        pO = self.psum[:, 4, :].rearrange("p (s n) -> p s n", s=4)
        var_of = {0: 0, 1: 1, 14: 3, 15: 4}
        it = 0
        qi = 0
        for c in range(KC):
            btv = bt32.rearrange("p (v h n) -> p v h n", v=5, h=2)
            for v5 in range(5):
                S.dma("sp", btv[:, v5], I["na_bias"][v5, 2 * c:2 * c + 2].rearrange("h p n -> p h n"),
                      writes=["bt32"])
            S.op("act", lambda e: e.activation(EB, bt32, AF.Exp), reads=["bt32"], writes=["EB"])
            for bb in range(NB):
                b2 = it % 2
                it += 1
                S.dma("sp", QTc[b2], self.QT[bb, c], writes=[("QTc", b2)])
                S.dma("sp", KTc[b2], self.KT[bb, c], writes=[("KTc", b2)])
                S.dma("sp", Vc[b2].rearrange("p t h d -> p t (h d)"),
                      self.VS[bb, :, 2 * c * 65:(2 * c + 2) * 65].rearrange("(t p) w -> p t w", p=128),
                      writes=[("Vc", b2)])
                for hh in range(2):
                    pr = slice(64 * hh, 64 * hh + 64)
                    for i in range(NT):
                        if i < NTL:
                            j0 = min(max(i - 2, 0), 11)
                            kts = [j0 + jj for jj in range(5)] + [16, 17]
                            v = var_of.get(i, 2)
                        else:
                            kts = [16, 17]
                            v = None
                        nk = len(kts)
                        sb = qi % 2
                        eb = qi % NEB
                        slot = qi % 4
                        qi += 1
                        for jj, kt in enumerate(kts):
                            S.mm(pS[sb][:, jj, :], KTc[b2][pr, kt * 128:(kt + 1) * 128],
                                 QTc[b2][pr, i * 128:(i + 1) * 128], True, True,
                                 reads=[("KTc", b2), ("QTc", b2)], writes=[("pS", sb)])
                        if nk > 4:
                            S.op("act", lambda e, sb=sb, eb=eb: e.activation(E[eb][:, 0:4, :], pS[sb][:, 0:4, :], AF.Exp),
                                 reads=[("pS", sb)], writes=[("E", eb)])
                            S.op("act", lambda e, sb=sb, eb=eb, nk=nk: e.activation(E[eb][:, 4:nk, :], pS[sb][:, 4:nk, :], AF.Exp),
                                 reads=[("pS", sb)], writes=[("E", eb)])
                        else:
                            S.op("act", lambda e, sb=sb, eb=eb, nk=nk: e.activation(E[eb][:, 0:nk, :], pS[sb][:, 0:nk, :], AF.Exp),
                                 reads=[("pS", sb)], writes=[("E", eb)])
                        if v is not None:
                            S.op("dve", lambda e, eb=eb, v=v, hh=hh: e.tensor_tensor(
                                E[eb][:, 0:5, :], E[eb][:, 0:5, :],
                                EBv[:, v, hh, :].rearrange("p (j n) -> p j n", j=5), ALU.mult),
                                reads=[("E", eb), "EB"], writes=[("E", eb)])
                        for jj, kt in enumerate(kts):
                            S.mm(pO[:, slot, 0:65], E[eb][:, jj, :], Vc[b2][:, kt, hh, :], jj == 0, jj == nk - 1,
                                 reads=[("E", eb), ("Vc", b2)], writes=[("pO", slot)])
                        S.op("dve", lambda e, slot=slot: e.reciprocal(rec[:, slot:slot + 1], pO[:, slot, 64:65]),
                             reads=[("pO", slot)], writes=[("rec", slot)])
                        S.op("dve", lambda e, slot=slot, i=i, hh=hh, b2=b2: e.tensor_scalar(
                            Oc[b2][:, i, 64 * hh:64 * hh + 64], pO[:, slot, 0:64], rec[:, slot:slot + 1], None, ALU.mult),
                            reads=[("pO", slot), ("rec", slot)], writes=[("Oc", b2)])
                S.dma("sp", self.OS[bb, :, c * 128:(c + 1) * 128].rearrange("(t p) n -> p t n", p=128),
                      Oc[b2], reads=[("Oc", b2)], writes=[("OS", bb)])

    def phaseC(self, l):
        S, A, I = self.S, self.A, self.I
        self.new_phase()
        pre = "l%d_" % l
        ntile = NT if l == 0 else NTL
        Wo = A.bf(KC * D).rearrange("p (k n) -> p k n", k=KC)
        for h2_ in range(2):
            S.dma("pool", Wo[:, :, h2_ * 512:(h2_ + 1) * 512],
                  I[pre + "w_o"][:, h2_ * 512:(h2_ + 1) * 512].rearrange("(k p) n -> p k n", p=128), writes=["Wo"])
        WoL = A.bf(KC * D).rearrange("p (k n) -> p k n", k=KC)
        gtmp = A.f32(D)
        gain = A.f32(D)
        self.bc_row(gain, I[pre + "norm_ffn"], [], "gain")
        if l == 0:
            WoC = A.bf(KC * D).rearrange("p (k n) -> p k n", k=KC)
            self.bc_row(gtmp, self.MOD[l, 2, 2 * D:3 * D], [], "gtmp")
            for k in range(KC):
                S.op("dve", lambda e, k=k: e.tensor_tensor(WoC[:, k, :], Wo[:, k, :], gtmp, ALU.mult),
                     reads=["Wo", "gtmp"], writes=["WoC"])
            A2c, SH2c = self.mod_tiles(l, 2, 4, 3, gain, "c")
        else:
            Wr = A.f32(KC * NE).rearrange("p (k n) -> p k n", k=KC)
            S.dma("sp", Wr, I["l1_w_router"].rearrange("(k p) n -> p k n", p=128), writes=["Wr"])
            h2f = A.f32(D)
            h2fT = A.f32(KC * 128).rearrange("p (k n) -> p k n", k=KC)
            lg = A.f32(8 * NE)
            sm = A.f32(8)
        Ots = [A.bf(D) for _ in range(2)]
        xts = [A.f32(D) for _ in range(2)]
        OT = [A.bf(KC * 128).rearrange("p (k n) -> p k n", k=KC) for _ in range(2)]
        x1s = [A.f32(D) for _ in range(2)]
        junk = A.bf(D)
        stt = A.f32(4)
        t1 = A.f32(D)
        h2 = A.bf(D)
        h2T = [A.bf(KC * 128).rearrange("p (k n) -> p k n", k=KC) for _ in range(2)]
        pT = self.pbank_bf(0).rearrange("p (k n) -> p k n", k=KC)
        pY = [self.pbank(1), self.pbank(2)]
        pT2 = self.pbank_bf(3).rearrange("p (k n) -> p k n", k=KC)
        pF = self.psum[:, 4:6, :].rearrange("p a (j n) -> p (a j) n", n=128)
        pL = self.psum[:, 6, 0:NE]
        it = 0
        for bb in range(NB):
            mark = A.top
            self.bc_row(gtmp, self.MOD[l, bb, 2 * D:3 * D], [], "gtmp")
            for k in range(KC):
                S.op("dve", lambda e, k=k: e.tensor_tensor(WoL[:, k, :], Wo[:, k, :], gtmp, ALU.mult),
                     reads=["Wo", "gtmp"], writes=["WoL"])
            A2l, SH2l = self.mod_tiles(l, bb, 4, 3, gain, "l")
            for t in range(ntile):
                b2 = it % 2
                it += 1
                lat = t < NTL
                S.dma("sp", Ots[b2], self.OS[bb, t * 128:(t + 1) * 128, :], writes=[("Ot", b2)])
                src, srd = self.xsrc(l, bb, t)
                S.dma("sp", xts[b2], src, reads=srd, writes=[("xt", b2)])
                for k in range(KC):
                    S.tr(pT[:, k, :], Ots[b2][:, k * 128:(k + 1) * 128], self.ident, reads=[("Ot", b2), "ident"],
                         writes=["pT"])
                S.op("act", lambda e, b2=b2: e.copy(OT[b2], pT), reads=["pT"], writes=[("OT", b2)])
                W = WoL if lat else WoC
                wtok = "WoL" if lat else "WoC"
                for hf in range(2):
                    for k in range(KC):
                        S.mm(pY[hf], OT[b2][:, k, :], W[:, k, hf * 512:(hf + 1) * 512], k == 0, k == KC - 1,
                             reads=[("OT", b2), wtok], writes=[("pY", hf)])
                    S.op("dve", lambda e, hf=hf, b2=b2: e.tensor_tensor(
                        x1s[b2][:, hf * 512:(hf + 1) * 512], pY[hf], xts[b2][:, hf * 512:(hf + 1) * 512], ALU.add),
                        reads=[("pY", hf), ("xt", b2)], writes=[("x1", b2)])
                S.dma("sp", self.XR1[bb, t * 128:(t + 1) * 128, :], x1s[b2], reads=[("x1", b2)],
                      writes=[("XR1", bb, t)])
                if l == 0:
                    self.rms_mod(x1s[b2], ("x1", b2), A2l if lat else A2c, ("A", "l" if lat else "c"),
                                 SH2l if lat else SH2c, ("SH", "l" if lat else "c"), h2, "h2", junk, stt, "stC", t1)
                else:
                    self.rms_mod(x1s[b2], ("x1", b2), A2l, ("A", "l"), SH2l, ("SH", "l"), h2f, "h2f", junk, stt,
                                 "stC", t1)
                    S.op("act", lambda e: e.copy(h2, h2f), reads=["h2f"], writes=["h2"])
                for k in range(KC):
                    S.tr(pT2[:, k, :], h2[:, k * 128:(k + 1) * 128], self.ident, reads=["h2", "ident"], writes=["pT2"])
                S.op("act", lambda e, b2=b2: e.copy(h2T[b2], pT2), reads=["pT2"], writes=[("h2T", b2)])
                col = bb * T + t * 128
                S.dma("sp", self.H2T[:, :, col:col + 128].rearrange("k p n -> p k n"), h2T[b2],
                      reads=[("h2T", b2)], writes=[("H2T", bb, t)])
                if l == 1:
                    for k in range(KC):
                        S.tr(pF[:, k, :], h2f[:, k * 128:(k + 1) * 128], self.ident_f, reads=["h2f", "ident_f"],
                             writes=["pF"])
                    S.op("dve", lambda e: e.tensor_copy(h2fT, pF), reads=["pF"], writes=["h2fT"])
                    for k in range(KC):
                        S.mm(pL, h2fT[:, k, :], Wr[:, k, :], k == 0, k == KC - 1, reads=["h2fT", "Wr"], writes=["pL"])
                    L = lambda i: lg[:, i * NE:(i + 1) * NE]
                    V = lambda i: sm[:, i:i + 1]
                    S.op("dve", lambda e: e.tensor_copy(L(0), pL), reads=["pL"], writes=["g0"])
                    S.op("dve", lambda e: e.reduce_max(V(0), L(0), AX.X), reads=["g0"], writes=["m1"])
                    S.op("dve", lambda e: e.tensor_scalar(L(1), L(0), V(0), None, ALU.is_equal),
                         reads=["g0", "m1"], writes=["g1"])
                    S.op("dve", lambda e: e.scalar_tensor_tensor(L(2), L(1), -1e30, L(0), ALU.mult, ALU.add),
                         reads=["g0", "g1"], writes=["g2"])
                    S.op("dve", lambda e: e.reduce_max(V(1), L(2), AX.X), reads=["g2"], writes=["m2"])
                    S.op("dve", lambda e: e.tensor_scalar(L(3), L(0), V(1), None, ALU.is_ge),
                         reads=["g0", "m2"], writes=["g3"])
                    S.op("dve", lambda e: e.tensor_scalar(V(2), V(0), -1.0, None, ALU.mult), reads=["m1"], writes=["nm1"])
                    S.op("act", lambda e: e.activation(L(4), L(0), AF.Exp, bias=V(2), scale=1.0),
                         reads=["g0", "nm1"], writes=["g4"])
                    S.op("dve", lambda e: e.tensor_tensor(L(5), L(4), L(3), ALU.mult), reads=["g4", "g3"], writes=["g5"])
                    S.op("dve", lambda e: e.reduce_sum(V(3), L(5), AX.X), reads=["g5"], writes=["ssum"])
                    S.op("dve", lambda e: e.reciprocal(V(4), V(3)), reads=["ssum"], writes=["rsum"])
                    S.op("dve", lambda e: e.tensor_scalar(L(6), L(5), V(4), None, ALU.mult), reads=["g5", "rsum"],
                         writes=["g6"])
                    S.dma("sp", self.GAT[bb, t * 128:(t + 1) * 128, :], L(6), reads=["g6"], writes=[("GAT", bb, t)])
            A.top = mark

    def phaseD0(self):
        S, A, I = self.S, self.A, self.I
        self.new_phase()
        NCH = DFF // 128
        Wg = A.bf(KC * DFF).rearrange("p (k n) -> p k n", k=KC)
        Wu = A.bf(KC * DFF).rearrange("p (k n) -> p k n", k=KC)
        Wd = A.bf(NCH * D).rearrange("p (c n) -> p c n", c=NCH)
        for n0 in range(0, DFF, 512):
            n1 = min(n0 + 512, DFF)
            for W, nm in ((Wg, "l0_w_gate"), (Wu, "l0_w_up")):
                S.dma("pool", W[:, :, n0:n1], I[nm][:, n0:n1].rearrange("(k p) n -> p k n", p=128),
                      writes=[(nm, n0)])
        wd_src = I["l0_w_down"].rearrange("(c p) n -> p c n", p=128)
        for c0 in range(0, NCH, 4):
            c1 = min(c0 + 4, NCH)
            S.dma("pool", Wd[:, c0:c1, :], wd_src[:, c0:c1, :], writes=[("wd", c0)])
        wg_toks = [("l0_w_gate", n0) for n0 in range(0, DFF, 512)]
        wu_toks = [("l0_w_up", n0) for n0 in range(0, DFF, 512)]
        wd_toks = [("wd", c0) for c0 in range(0, NCH, 4)]
        G2c = A.f32(D)
        G2l = A.f32(D)
        self.bc_row(G2c, self.MOD[0, 2, 5 * D:6 * D], [], "G2c")
        hT = [A.bf(KC * 512).rearrange("p (k n) -> p k n", k=KC) for _ in range(2)]
        AT = A.bf(NCH * 512).rearrange("p (c n) -> p c n", c=NCH)
        sg = [A.bf(512) for _ in range(2)]
        xts = [A.f32(D) for _ in range(2)]
        x2s = [A.f32(D) for _ in range(2)]
        tmp = A.f32(512)
        pG = [self.pbank(0), self.pbank(1)]
        pU = [self.pbank(2), self.pbank(3)]
        pY = [self.pbank(4 + i) for i in range(4)]
        ngroups = NB * NT // 4
        ti = 0
        yi = 0
        cur_bb = -1
        for g in range(ngroups):
            hb = g % 2
            S.dma("sp", hT[hb], self.H2T[:, :, g * 512:(g + 1) * 512].rearrange("k p n -> p k n"),
                  writes=[("hT", hb)])
            for ci in range(NCH):
                pb = ci % 2
                for k in range(KC):
                    S.mm(pG[pb], Wg[:, k, ci * 128:(ci + 1) * 128], hT[hb][:, k, :], k == 0, k == KC - 1,
                         reads=[("hT", hb), wg_toks[ci // 4]], writes=[("pG", pb)])
                for k in range(KC):
                    S.mm(pU[pb], Wu[:, k, ci * 128:(ci + 1) * 128], hT[hb][:, k, :], k == 0, k == KC - 1,
                         reads=[("hT", hb), wu_toks[ci // 4]], writes=[("pU", pb)])
                S.op("act", lambda e, pb=pb: e.activation(sg[pb], pG[pb], AF.Silu), reads=[("pG", pb)],
                     writes=[("sg", pb)])
                S.op("dve", lambda e, pb=pb, ci=ci: e.tensor_tensor(AT[:, ci, :], sg[pb], pU[pb], ALU.mult),
                     reads=[("sg", pb), ("pU", pb)], writes=[("AT", ci)])
            at_toks = [("AT", ci) for ci in range(NCH)]
            for j in range(4):
                bb, t = divmod(4 * g + j, NT)
                lat = t < NTL
                if lat and bb != cur_bb:
                    cur_bb = bb
                    self.bc_row(G2l, self.MOD[0, bb, 5 * D:6 * D], [], "G2l")
                b2 = ti % 2
                ti += 1
                S.dma("sp", xts[b2], self.XR1[bb, t * 128:(t + 1) * 128, :], writes=[("xt", b2)])
                G2, gtok = (G2l, "G2l") if lat else (G2c, "G2c")
                for hf in range(2):
                    yb = yi % 4
                    yi += 1
                    for ci in range(NCH):
                        S.mm(pY[yb], AT[:, ci, j * 128:(j + 1) * 128], Wd[:, ci, hf * 512:(hf + 1) * 512],
                             ci == 0, ci == NCH - 1, reads=[("AT", ci), wd_toks[ci // 4]], writes=[("pY", yb)])
                    S.op("dve", lambda e, yb=yb, hf=hf, G2=G2: e.tensor_tensor(
                        tmp, pY[yb], G2[:, hf * 512:(hf + 1) * 512], ALU.mult),
                        reads=[("pY", yb), gtok], writes=["tmp"])
                    S.op("dve", lambda e, hf=hf, b2=b2: e.tensor_tensor(
                        x2s[b2][:, hf * 512:(hf + 1) * 512], tmp, xts[b2][:, hf * 512:(hf + 1) * 512], ALU.add),
                        reads=["tmp", ("xt", b2)], writes=[("x2", b2)])
                S.dma("sp", self.XR2[bb, t * 128:(t + 1) * 128, :], x2s[b2], reads=[("x2", b2)],
                      writes=[("XR2", bb, t)])

    def phaseB1(self):
        S, A, I = self.S, self.A, self.I
        self.new_phase()
        lam_init = 0.8 - 0.6 * math.exp(-0.3 * 1)
        lv = A.f32(4 * 64).rearrange("p (a n) -> p a n", a=4)
        for a, nm in enumerate(("q1", "k1", "q2", "k2")):
            self.bc_row(lv[:, a, :], I["l1_lambda_" + nm], [], ("lv", a))
        lp = A.f32(2 * 64).rearrange("p (a n) -> p a n", a=2)
        ls = A.f32(8)
        for a in range(2):
            S.op("dve", lambda e, a=a: e.tensor_tensor(lp[:, a, :], lv[:, 2 * a, :], lv[:, 2 * a + 1, :], ALU.mult),
                 reads=[("lv", 2 * a), ("lv", 2 * a + 1)], writes=[("lp", a)])
            S.op("dve", lambda e, a=a: e.reduce_sum(ls[:, a:a + 1], lp[:, a, :], AX.X), reads=[("lp", a)],
                 writes=[("ls", a)])
        S.op("act", lambda e: e.activation(ls[:, 2:4], ls[:, 0:2], AF.Exp), reads=[("ls", 0), ("ls", 1)],
             writes=["lexp"])
        S.op("dve", lambda e: e.tensor_tensor(ls[:, 4:5], ls[:, 3:4], ls[:, 2:3], ALU.subtract), reads=["lexp"],
             writes=["ldiff"])
        S.op("dve", lambda e: e.tensor_scalar(ls[:, 5:6], ls[:, 4:5], -lam_init, None, ALU.add), reads=["ldiff"],
             writes=["nlam"])
        nlam = ls[:, 5:6]
        SUB = A.f32(128)
        self.bc_row(SUB, I["l1_subln"], [], "SUB0")
        S.op("dve", lambda e: e.tensor_scalar(SUB, SUB, 1.0 - lam_init, None, ALU.mult), reads=["SUB0"], writes=["SUB"])
        QTh = [A.bf(SEQ) for _ in range(2)]
        KTh = [A.bf(T) for _ in range(2)]
        Vh = [A.bf(NT * 129).rearrange("p (t d) -> p t d", t=NT) for _ in range(2)]
        NEB = 4
        E = [A.bf(512) for _ in range(NEB)]
        accS = [A.f32(8 * 129) for _ in range(2)]
        ob = A.f32(4 * 128).rearrange("p (q n) -> p q n", q=4)
        o1 = A.f32(128)
        sq = A.f32(128)
        rr = A.f32(16)
        Oc = [A.bf(NTL * 128).rearrange("p (t n) -> p t n", t=NTL) for _ in range(2)]
        pS = [self.pbank(i) for i in range(4)]

        def acc(s_):
            b = 4 + s_ // 3
            off = (s_ % 3) * 129
            return self.psum[:, b, off:off + 129]

        it = 0
        si = 0
        ci = 0
        for bb in range(NB):
            for h in range(KC):
                b2 = it % 2
                it += 1
                S.dma("sp", QTh[b2], self.QT[bb, h, :, 0:SEQ], writes=[("QTh", b2)])
                S.dma("sp", KTh[b2], self.KT[bb, h], writes=[("KTh", b2)])
                S.dma("sp", Vh[b2], self.VS[bb, :, h * 129:(h + 1) * 129].rearrange("(t p) w -> p t w", p=128),
                      writes=[("Vh", b2)])
                for qc in range(4):
                    for kt in range(NT):
                        for m in range(2):
                            sb = si % 4
                            eb = si % NEB
                            si += 1
                            pr = slice(64 * m, 64 * m + 64)
                            S.mm(pS[sb], KTh[b2][pr, kt * 128:(kt + 1) * 128], QTh[b2][pr, qc * 512:(qc + 1) * 512],
                                 True, True, reads=[("KTh", b2), ("QTh", b2)], writes=[("pS", sb)])
                            S.op("act", lambda e, sb=sb, eb=eb: e.activation(E[eb], pS[sb], AF.Exp),
                                 reads=[("pS", sb)], writes=[("E", eb)])
                            for qt in range(4):
                                sl = m * 4 + qt
                                S.mm(acc(sl), E[eb][:, qt * 128:(qt + 1) * 128], Vh[b2][:, kt, :],
                                     kt == 0 and sl % 3 == 0, kt == NT - 1, reads=[("E", eb), ("Vh", b2)],
                                     writes=[("acc", sl)], skip_group_check=True)
                    ab = ci % 2
                    ci += 1
                    aS = accS[ab]
                    for bk in range(3):
                        n = 387 if bk < 2 else 258
                        S.op("dve", lambda e, bk=bk, n=n, aS=aS: e.tensor_copy(aS[:, bk * 387:bk * 387 + n],
                                                                             self.psum[:, 4 + bk, 0:n]),
                             reads=[("acc", 3 * bk + j) for j in range(3) if 3 * bk + j < 8], writes=[("accS", ab, bk)])
                    atoks = [("accS", ab, bk) for bk in range(3)]
                    a3 = aS.rearrange("p (s n) -> p s n", n=129)
                    S.op("dve", lambda e, a3=a3: e.reciprocal(rr[:, 0:8], a3[:, :, 128]), reads=atoks, writes=["rr"])
                    S.op("dve", lambda e: e.tensor_scalar(rr[:, 8:12], rr[:, 4:8], nlam, None, ALU.mult),
                         reads=["rr", "nlam"], writes=["rr2"])
                    for qt in range(4):
                        S.op("dve", lambda e, qt=qt, a3=a3: e.tensor_scalar(o1, a3[:, qt, 0:128], rr[:, qt:qt + 1], None,
                                                                        ALU.mult), reads=atoks + ["rr"], writes=["o1"])
                        S.op("dve", lambda e, qt=qt, a3=a3: e.scalar_tensor_tensor(
                            ob[:, qt, :], a3[:, 4 + qt, 0:128], rr[:, 8 + qt:9 + qt], o1, ALU.mult, ALU.add),
                            reads=atoks + ["rr2", "o1"], writes=[("ob", qt)])
                        S.op("dve", lambda e, qt=qt: e.tensor_tensor(sq, ob[:, qt, :], ob[:, qt, :], ALU.mult),
                             reads=[("ob", qt)], writes=["sq"])
                        S.op("dve", lambda e, qt=qt: e.reduce_sum(rr[:, 12 + qt:13 + qt], sq, AX.X), reads=["sq"],
                             writes=[("ss", qt)])
                    sst = [("ss", qt) for qt in range(4)]
                    S.op("act", lambda e: e.activation(ls[:, 6:10][:, 0:4] if False else rr[:, 12:16], rr[:, 12:16], AF.Ln,
                                                       bias=EPS, scale=1.0 / 128), reads=sst, writes=["lnv"])
                    S.op("act", lambda e: e.activation(rr[:, 12:16], rr[:, 12:16], AF.Exp, scale=-0.5), reads=["lnv"],
                         writes=["rstd"])
                    for qt in range(4):
                        S.op("dve", lambda e, qt=qt, b2=b2, qc=qc: e.scalar_tensor_tensor(
                            Oc[b2][:, qc * 4 + qt, :], ob[:, qt, :], rr[:, 12 + qt:13 + qt], SUB, ALU.mult, ALU.mult),
                            reads=[("ob", qt), "rstd", "SUB"], writes=[("Oc", b2)])
                S.dma("sp", self.OS[bb, 0:SEQ, h * 128:(h + 1) * 128].rearrange("(t p) n -> p t n", p=128),
                      Oc[b2], reads=[("Oc", b2)], writes=[("OS", bb)])

    def phaseD1(self):
        S, A, I = self.S, self.A, self.I
        self.new_phase()
        NG = DFE // 512
        hT = A.bf(KC * SEQ).rearrange("p (k n) -> p k n", k=KC)
        Y = A.f32(NTL * D).rearrange("p (t n) -> p t n", t=NTL)
        gat = A.f32(NTL * NE).rearrange("p (t e) -> p t e", t=NTL)
        wg = [A.bf(KC * 512).rearrange("p (k n) -> p k n", k=KC) for _ in range(2)]
        wu = [A.bf(KC * 512).rearrange("p (k n) -> p k n", k=KC) for _ in range(2)]
        wd = [A.bf(4 * D).rearrange("p (c n) -> p c n", c=4) for _ in range(2)]
        AT = [A.bf(4 * 512).rearrange("p (c n) -> p c n", c=4) for _ in range(2)]
        sg = [A.bf(512) for _ in range(2)]
        G2l = A.f32(D)
        fin = A.f32(D)
        self.bc_row(fin, I["final_norm"], [], "fin")
        xts = [A.f32(D) for _ in range(2)]
        x2s = [A.f32(D) for _ in range(2)]
        outs = [A.f32(D) for _ in range(2)]
        junk = A.bf(D)
        stt = A.f32(4)
        pG = [self.pbank(0), self.pbank(1)]
        pU = [self.pbank(2), self.pbank(3)]
        pY = [self.pbank(4 + i) for i in range(4)]
        wi = 0
        ai = 0
        yi = 0
        pi = 0
        ti = 0
        for bb in range(NB):
            S.dma("sp", hT, self.H2T[:, :, bb * T:bb * T + SEQ].rearrange("k p n -> p k n"), writes=["hT"])
            S.dma("sp", gat, self.GAT[bb].rearrange("(t p) e -> p t e", p=128), writes=["gat"])
            self.bc_row(G2l, self.MOD[1, bb, 5 * D:6 * D], [], "G2l")
            for ex in range(NE):
                for g in range(NG):
                    wb = wi % 2
                    wi += 1
                    S.dma("pool", wg[wb], I["l1_w_gate"][ex][:, g * 512:(g + 1) * 512].rearrange("(k p) n -> p k n", p=128),
                          writes=[("wg", wb)])
                    S.dma("pool", wu[wb], I["l1_w_up"][ex][:, g * 512:(g + 1) * 512].rearrange("(k p) n -> p k n", p=128),
                          writes=[("wu", wb)])
                    S.dma("pool", wd[wb], I["l1_w_down"][ex][g * 512:(g + 1) * 512, :].rearrange("(c p) n -> p c n", p=128),
                          writes=[("wd", wb)])
                    first = (ex == 0 and g == 0)
                    for tg in range(4):
                        ab = ai % 2
                        ai += 1
                        for ci in range(4):
                            pb = pi % 2
                            pi += 1
                            for k in range(KC):
                                S.mm(pG[pb], wg[wb][:, k, ci * 128:(ci + 1) * 128], hT[:, k, tg * 512:(tg + 1) * 512],
                                     k == 0, k == KC - 1, reads=["hT", ("wg", wb)], writes=[("pG", pb)])
                            for k in range(KC):
                                S.mm(pU[pb], wu[wb][:, k, ci * 128:(ci + 1) * 128], hT[:, k, tg * 512:(tg + 1) * 512],
                                     k == 0, k == KC - 1, reads=["hT", ("wu", wb)], writes=[("pU", pb)])
                            S.op("act", lambda e, pb=pb: e.activation(sg[pb], pG[pb], AF.Silu), reads=[("pG", pb)],
                                 writes=[("sg", pb)])
                            S.op("dve", lambda e, pb=pb, ci=ci, ab=ab: e.tensor_tensor(AT[ab][:, ci, :], sg[pb], pU[pb],
                                                                                   ALU.mult),
                                 reads=[("sg", pb), ("pU", pb)], writes=[("AT", ab)])
                        for j in range(4):
                            tile = tg * 4 + j
                            for hf in range(2):
                                yb = yi % 4
                                yi += 1
                                for ci in range(4):
                                    S.mm(pY[yb], AT[ab][:, ci, j * 128:(j + 1) * 128], wd[wb][:, ci, hf * 512:(hf + 1) * 512],
                                         ci == 0, ci == 3, reads=[("AT", ab), ("wd", wb)], writes=[("pY", yb)])
                                ysl = Y[:, tile, hf * 512:(hf + 1) * 512]
                                gsc = gat[:, tile, ex:ex + 1]
                                if first:
                                    S.op("dve", lambda e, ysl=ysl, gsc=gsc, yb=yb: e.tensor_scalar(ysl, pY[yb], gsc, None,
                                                                                                  ALU.mult),
                                         reads=[("pY", yb), "gat"], writes=[("Y", tile, hf)])
                                else:
                                    S.op("dve", lambda e, ysl=ysl, gsc=gsc, yb=yb: e.scalar_tensor_tensor(
                                        ysl, pY[yb], gsc, ysl, ALU.mult, ALU.add),
                                        reads=[("pY", yb), "gat", ("Y", tile, hf)], writes=[("Y", tile, hf)])
            for tile in range(NTL):
                b2 = ti % 2
                ti += 1
                S.dma("sp", xts[b2], self.XR1[bb, tile * 128:(tile + 1) * 128, :], writes=[("xt", b2)])
                S.op("dve", lambda e, tile=tile, b2=b2: e.tensor_tensor(x2s[b2], Y[:, tile, :], G2l, ALU.mult),
                     reads=[("Y", tile, 0), ("Y", tile, 1), "G2l"], writes=[("x2a", b2)])
                S.op("dve", lambda e, b2=b2: e.tensor_tensor(x2s[b2], x2s[b2], xts[b2], ALU.add),
                     reads=[("x2a", b2), ("xt", b2)], writes=[("x2", b2)])
                if self.dbg:
                    S.dma("sp", self.XR2[bb, tile * 128:(tile + 1) * 128, :], x2s[b2], reads=[("x2", b2)],
                          writes=[("XR2", bb, tile)])
                S.op("act", lambda e, b2=b2: e.activation(junk, x2s[b2], AF.Square, accum_out=stt[:, 0:1]),
                     reads=[("x2", b2)], writes=["junk", "ssq"])
                S.op("act", lambda e: e.activation(stt[:, 1:2], stt[:, 0:1], AF.Sqrt, bias=EPS, scale=1.0 / D),
                     reads=["ssq"], writes=["std"])
                S.op("dve", lambda e: e.reciprocal(stt[:, 2:3], stt[:, 1:2]), reads=["std"], writes=["rstd"])
                S.op("dve", lambda e, b2=b2: e.scalar_tensor_tensor(outs[b2], x2s[b2], stt[:, 2:3], fin, ALU.mult, ALU.mult),
                     reads=[("x2", b2), "rstd", "fin"], writes=[("out", b2)])
                S.dma("sp", self.out[bb, tile * 128:(tile + 1) * 128, :], outs[b2], reads=[("out", b2)],
                      writes=[("OUT", bb, tile)])


def _const_tables():
    ident = np.eye(128, dtype=np.float32)
    pos = np.arange(SEQ)
    row = (pos // GRID_W).astype(np.float64)
    col = (pos % GRID_W).astype(np.float64)
    inv = 10000.0 ** (-np.arange(0, 32, 2, dtype=np.float64) / 32.0)
    ar = row[:, None] * inv[None]
    ac = col[:, None] * inv[None]
    cs = np.concatenate([np.cos(ar), np.cos(ar), np.cos(ac), np.cos(ac)], -1)
    sn = np.concatenate([-np.sin(ar), np.sin(ar), -np.sin(ac), np.sin(ac)], -1)
    cs = cs.reshape(NTL, 128, 64).transpose(1, 0, 2).astype(np.float32)
    sn = sn.reshape(NTL, 128, 64).transpose(1, 0, 2).astype(np.float32)
    return ident, np.ascontiguousarray(cs), np.ascontiguousarray(sn)


NA_VAR_TILES = (0, 1, 5, 14, 15)


def _na_index():
    kp = np.arange(128)
    kr, kc = kp // 64, kp % 64
    qp = np.arange(128)
    qr, qc = qp // 64, qp % 64
    dr = np.zeros((5, 128, 5, 128), np.int64)
    dc = np.zeros((5, 128, 5, 128), np.int64)
    ok = np.zeros((5, 128, 5, 128), bool)
    for v, i in enumerate(NA_VAR_TILES):
        j0 = min(max(i - 2, 0), 11)
        for jj in range(5):
            rk = 2 * (j0 + jj) + kr
            r = 2 * i + qr
            rs = np.clip(r - 4, 0, 24)
            vrow = (rk[:, None] >= rs[None, :]) & (rk[:, None] < rs[None, :] + 8)
            ws = np.clip(qc - 8, 0, 48)
            vcol = (kc[:, None] >= ws[None, :]) & (kc[:, None] < ws[None, :] + 16)
            val = vrow & vcol
            ok[v, :, jj, :] = val
            dr[v, :, jj, :] = np.where(val, rk[:, None] - r[None, :] + 7, 0)
            dc[v, :, jj, :] = np.where(val, kc[:, None] - qc[None, :] + 15, 0)
    return dr, dc, ok


def _na_bias(rpb):
    dr, dc, ok = _na_index()
    tab = rpb[:, dr, dc]
    tab = np.where(ok[None], tab, np.float32(-30000.0))
    tab = tab.transpose(1, 0, 2, 3, 4).reshape(5, 16, 128, 640)
    return np.ascontiguousarray(tab.astype(np.float32))


def make_in_maps(inputs, n_cores=8):
    ident, cs, sn = _const_tables()
    nab = _na_bias(np.asarray(inputs["l0_rpb"], np.float32))
    shared = {}
    for k, v in inputs.items():
        if k in ("x", "c", "ctx", "l0_rpb"):
            continue
        shared[k] = np.ascontiguousarray(np.asarray(v, np.float32))
    shared["ident"] = ident
    shared["rope_cs"] = cs
    shared["rope_sn"] = sn
    shared["na_bias"] = nab
    maps = []
    for c in range(n_cores):
        m = dict(shared)
        m["x"] = np.ascontiguousarray(inputs["x"][c * NB:(c + 1) * NB])
        m["ctx"] = np.ascontiguousarray(inputs["ctx"][c * NB:(c + 1) * NB])
        m["c"] = np.ascontiguousarray(inputs["c"][c * NB:(c + 1) * NB])
        maps.append(m)
    return maps


_NC_CACHE = {}


def kernel(**inputs):
    if "nc" not in _NC_CACHE:
        _NC_CACHE["nc"] = Builder().build()
    nc = _NC_CACHE["nc"]
    maps = make_in_maps(inputs)
    res = run_bass_kernel_spmd(nc, maps, core_ids=list(range(8)))
    out = np.concatenate([np.asarray(r["out"]) for r in res.results], axis=0)
    return out.astype(np.float32, copy=False)
```

```python
import math
from contextlib import ExitStack

import numpy as np
import concourse.bass as bass
import concourse.mybir as mybir
from concourse.bass_utils import run_bass_kernel_spmd

F32 = mybir.dt.float32
BF16 = mybir.dt.bfloat16
I32 = mybir.dt.int32
AF = mybir.ActivationFunctionType
ALU = mybir.AluOpType
AX = mybir.AxisListType

ENGS = ("pe", "act", "dve", "pool", "sp")
N_DMA_SEMS = 12


class Op:
    __slots__ = ("eng", "fn", "deps", "is_dma", "signal", "sem", "val", "idx", "prev_dma")

    def __init__(self, eng, fn, is_dma, idx):
        self.eng = eng
        self.fn = fn
        self.deps = set()
        self.is_dma = is_dma
        self.signal = is_dma
        self.sem = None
        self.val = 0
        self.idx = idx
        self.prev_dma = None


class Sched:
    def __init__(self, nc):
        self.nc = nc
        self.ops = []
        self.last_w = {}
        self.readers = {}

    def op(self, eng, fn, reads=(), writes=(), dma=False):
        o = Op(eng, fn, dma, len(self.ops))
        self.ops.append(o)
        for t in reads:
            w = self.last_w.get(t)
            if w is not None:
                self._dep(o, w, raw=True)
        for t in writes:
            w = self.last_w.get(t)
            if w is not None:
                self._dep(o, w, raw=False)
            rd = self.readers.get(t)
            if rd:
                for k, r in rd.items():
                    if k == "dma":
                        for rr in r:
                            self._dep(o, rr, raw=False)
                    else:
                        self._dep(o, r, raw=False)
        for t in reads:
            rd = self.readers.setdefault(t, {})
            if dma:
                rd.setdefault("dma", []).append(o)
            else:
                rd[eng] = o
        for t in writes:
            self.last_w[t] = o
            self.readers[t] = {}
        return o

    def _dep(self, o, d, raw):
        if d is o:
            return
        if d.is_dma or o.is_dma:
            o.deps.add(d)
            d.signal = True
            return
        if d.eng == o.eng:
            if raw and o.eng != "pe":
                o.deps.add(d)
                d.signal = True
            return
        o.deps.add(d)
        d.signal = True

    def dma(self, q, out, in_, reads=(), writes=(), **kw):
        return self.op(q, lambda e: e.dma_start(out=out, in_=in_, **kw), reads, writes, dma=True)

    def mm(self, out, lhsT, rhs, start, stop, reads=(), writes=(), **kw):
        return self.op("pe", lambda e: e.matmul(out, lhsT, rhs, start=start, stop=stop, **kw),
                       reads, writes)

    def tr(self, out, in_, ident, reads=(), writes=()):
        return self.op("pe", lambda e: e.transpose(out, in_, ident), reads, writes)

    def emit(self, stack):
        nc = self.nc
        sems = {e: stack.enter_context(nc.semaphore("s_" + e)) for e in ENGS}
        dsems = {}
        dcount = {}
        dnext = {}
        dlast = {}
        cnt = {e: 0 for e in ENGS}
        per_eng = {e: [] for e in ENGS}
        for o in self.ops:
            per_eng[o.eng].append(o)
            if o.is_dma:
                q = o.eng
                if q not in dsems:
                    dsems[q] = [stack.enter_context(nc.semaphore("d_%s%d" % (q, i)))
                                for i in range(N_DMA_SEMS)]
                    dnext[q] = 0
                i = dnext[q]
                dnext[q] = (i + 1) % N_DMA_SEMS
                s = dsems[q][i]
                o.prev_dma = dlast.get(s)
                dlast[s] = o
                dcount[s] = dcount.get(s, 0) + 16
                o.sem = s
                o.val = dcount[s]
            elif o.signal:
                cnt[o.eng] += 1
                o.sem = sems[o.eng]
                o.val = cnt[o.eng]
        all_dma_final = dict(dcount)
        block = stack.enter_context(nc.Block())

        def run(eng_name):
            def body(e):
                waited = {}
                for o in per_eng[eng_name]:
                    need = {}
                    for d in o.deps:
                        if need.get(d.sem, 0) < d.val:
                            need[d.sem] = d.val
                    if o.prev_dma is not None:
                        p = o.prev_dma
                        if need.get(p.sem, 0) < p.val:
                            need[p.sem] = p.val
                    for s, v in need.items():
                        if waited.get(s, 0) < v:
                            e.wait_ge(s, v)
                            waited[s] = v
                    ins = o.fn(e)
                    if o.is_dma:
                        ins.then_inc(o.sem, 16)
                    elif o.signal:
                        ins.then_inc(o.sem, 1)
                if eng_name == "sp":
                    for s, v in all_dma_final.items():
                        if waited.get(s, 0) < v:
                            e.wait_ge(s, v)
            return body

        block.tensor(run("pe"))
        block.scalar(run("act"))
        block.vector(run("dve"))
        block.gpsimd(run("pool"))
        block.sync(run("sp"))

    def barrier(self):
        last = {}
        dmas = []
        for o in self.ops:
            if o.is_dma:
                dmas.append(o)
            else:
                last[o.eng] = o
        self._bar = list(last.values()) + dmas
        self._bar_seen = set()
        self.last_w = {}
        self.readers = {}

    _bar = None
    _bar_seen = None


_orig_op = Sched.op


def _op_with_barrier(self, eng, fn, reads=(), writes=(), dma=False):
    o = _orig_op(self, eng, fn, reads, writes, dma)
    if self._bar is not None and eng not in self._bar_seen:
        self._bar_seen.add(eng)
        for d in self._bar:
            if d is o:
                continue
            if d.is_dma or d.eng != eng:
                o.deps.add(d)
                d.signal = True
    return o


Sched.op = _op_with_barrier

B0_LA = 2
B0_NPS = 3
NB = 2
SEQ = 2048
CTX = 256
T = SEQ + CTX
NT = T // 128
NTL = SEQ // 128
D = 1024
KC = 8
DFF = 2816
NE = 8
DFE = 3584
EPS = 1e-6
GRID_W = 64
VW = 1040


class Arena:
    def __init__(self, ap, words):
        self.ap = ap
        self.words = words
        self.top = 0

    def f32(self, n):
        assert self.top + n <= self.words, "arena overflow %d" % (self.top + n)
        a = self.ap[:, self.top:self.top + n]
        self.top += n
        return a

    def bf(self, n):
        assert n % 2 == 0
        return self.f32(n // 2).bitcast(BF16)


class Builder:
    def __init__(self, dbg=False, phases=None):
        self.dbg = dbg
        self.phases = phases
        self.nc = bass.Bass("TRN2", target_bir_lowering=False)
        self.S = Sched(self.nc)
        self.inputs = {}

    def din(self, name, shape, dt=F32):
        t = self.nc.dram_tensor(name, list(shape), dt, kind="ExternalInput").ap()
        self.inputs[name] = t
        return t

    def dscr(self, name, shape, dt):
        kind = "ExternalOutput" if self.dbg else "Internal"
        return self.nc.dram_tensor(name, list(shape), dt, kind=kind).ap()

    def want(self, ph):
        return self.phases is None or ph in self.phases

    def build(self):
        nc = self.nc
        shapes = {"x": [NB, SEQ, D], "ctx": [NB, CTX, D], "c": [NB, D], "c_ctx": [D],
                  "l0_w_gate": [D, DFF], "l0_w_up": [D, DFF], "l0_w_down": [DFF, D],
                  "na_bias": [5, 16, 128, 640], "l1_subln": [128], "l1_w_router": [D, NE],
                  "l1_w_gate": [NE, D, DFE], "l1_w_up": [NE, D, DFE], "l1_w_down": [NE, DFE, D],
                  "final_norm": [D], "ident": [128, 128], "rope_cs": [128, NTL, 64], "rope_sn": [128, NTL, 64]}
        shapes.update({"l0_w_ada": [D, 6 * D], "l0_b_ada": [6 * D], "l0_norm_mix": [D], "l0_w_qkv": [D, 3 * D],
                       "l0_w_o": [D, D], "l0_norm_ffn": [D], "l1_w_ada": [D, 6 * D], "l1_b_ada": [6 * D],
                       "l1_norm_mix": [D], "l1_w_qkv": [D, 3 * D], "l1_w_o": [D, D], "l1_norm_ffn": [D]})
        for k in ("q1", "k1", "q2", "k2"):
            shapes["l1_lambda_" + k] = [64]
        bld = self

        class Lazy(dict):
            def __missing__(d, name):
                if bld.dbg:
                    d[name] = bld.din(name, shapes[name])
                    return d[name]
                raise KeyError(name)

        I = Lazy()
        if not self.dbg:
            for name in sorted(shapes):
                I[name] = self.din(name, shapes[name])
        self.I = I
        self.out = nc.dram_tensor("out", [NB, SEQ, D], F32, kind="ExternalOutput").ap()
        self.MOD = self.dscr("MOD", [2, 3, 6 * D], F32)
        self.QT = self.dscr("QT", [NB, KC, 128, T], BF16)
        self.KT = self.dscr("KT", [NB, KC, 128, T], BF16)
        self.VS = self.dscr("VS", [NB, T, VW], BF16)
        self.OS = self.dscr("OS", [NB, T, D], BF16)
        self.XR1 = self.dscr("XR1", [NB, T, D], F32)
        self.XR2 = self.dscr("XR2", [NB, T, D], F32)
        self.H2T = self.dscr("H2T", [KC, 128, NB * T], BF16)
        self.GAT = self.dscr("GAT", [NB, SEQ, NE], F32)

        with ExitStack() as st:
            AW = 51200
            arena_t = st.enter_context(nc.sbuf_tensor("arena", [128, AW], F32))
            self.psum = st.enter_context(nc.psum_tensor("psum", [128, 8, 512], F32))
            self.A = Arena(arena_t, AW)
            self.ident_f = self.A.f32(128)
            self.ident = self.A.bf(128)
            S = self.S
            S.dma("sp", self.ident_f, I["ident"], writes=["ident_f"])
            S.dma("pool", self.ident, I["ident"], writes=["ident"])
            self.base = self.A.top
            if self.want("0"):
                self.phase0()
            for l in range(2):
                if self.want("A%d" % l):
                    self.phaseA(l)
                if self.want("B%d" % l):
                    (self.phaseB0 if l == 0 else self.phaseB1)()
                if self.want("C%d" % l):
                    self.phaseC(l)
                if self.want("D%d" % l):
                    (self.phaseD0 if l == 0 else self.phaseD1)()
            S.emit(st)
        return nc

    def new_phase(self):
        self.S.barrier()
        self.A.top = self.base

    def pbank(self, b):
        return self.psum[:, b, :]

    def pbank_bf(self, b, nb=1):
        v = self.psum[:, b:b + nb, :].rearrange("p a b -> p (a b)").bitcast(BF16)
        return v

    def phase0(self):
        S, A, I = self.S, self.A, self.I
        self.new_phase()
        cT = A.f32(24).rearrange("p (k j) -> p k j", k=KC)
        cs = A.f32(24).rearrange("p (k j) -> p k j", k=KC)
        srcs = [I["c"][0], I["c"][1], I["c_ctx"]]
        for j, src in enumerate(srcs):
            S.dma("sp", cT[:, :, j], src.rearrange("(k p) -> p k", p=128), writes=["cT"],
                  allow_slow_non_contiguous=True)
        S.op("act", lambda e: e.activation(cs, cT, AF.Silu), reads=["cT"], writes=["cs"])
        bt = A.f32(6 * D)
        modv = A.f32(6 * D)
        wb = [A.f32(KC * 512).rearrange("p (k n) -> p k n", k=KC) for _ in range(2)]
        it = 0
        for l in range(2):
            S.dma("sp", bt[0:3, :], I["l%d_b_ada" % l].partition_broadcast(3), reads=[], writes=["bt"])
            for n in range(12):
                b = it % 2
                it += 1
                S.dma("sp", wb[b], I["l%d_w_ada" % l][:, n * 512:(n + 1) * 512].rearrange("(k p) n -> p k n", p=128),
                      writes=[("wb", b)])
                pb = self.psum[0:3, n % 2, :]
                for k in range(KC):
                    S.mm(pb, cs[:, k, :], wb[b][:, k, :], k == 0, k == KC - 1,
                         reads=["cs", ("wb", b)], writes=[("p0", n % 2)])
                S.op("dve", lambda e, pb=pb, n=n: e.tensor_tensor(modv[0:3, n * 512:(n + 1) * 512], pb,
                                                                 bt[0:3, n * 512:(n + 1) * 512], ALU.add),
                     reads=[("p0", n % 2), "bt"], writes=["modv"])
            S.dma("sp", self.MOD[l], modv[0:3, :], reads=["modv"], writes=[("MOD", l)])

    def bc_row(self, dst, row, reads, tok):
        self.S.dma("sp", dst, row.partition_broadcast(128), reads=reads, writes=[tok])

    def mod_tiles(self, l, j, idx_scale, idx_shift, gain, tag):
        S, A = self.S, self.A
        tmp = A.f32(D)
        At = A.f32(D)
        SH = A.f32(D)
        self.bc_row(tmp, self.MOD[l, j, idx_scale * D:(idx_scale + 1) * D], [("MOD", l)], ("mtmp", tag))
        self.bc_row(SH, self.MOD[l, j, idx_shift * D:(idx_shift + 1) * D], [("MOD", l)], ("SH", tag))
        S.op("dve", lambda e: e.scalar_tensor_tensor(At, tmp, 1.0, gain, ALU.add, ALU.mult),
             reads=[("mtmp", tag), "gain"], writes=[("A", tag)])
        return At, SH

    def xsrc(self, l, bb, t):
        if l == 0:
            if t < NTL:
                return self.I["x"][bb, t * 128:(t + 1) * 128, :], []
            return self.I["ctx"][bb, (t - NTL) * 128:(t - NTL + 1) * 128, :], []
        return self.XR2[bb, t * 128:(t + 1) * 128, :], [("XR2", bb, t)]

    def rms_mod(self, xt, xtok, At, Atok, SH, SHtok, h_out, h_tok, junk, st, sttok, t1):
        S = self.S
        S.op("act", lambda e: e.activation(junk, xt, AF.Square, accum_out=st[:, 0:1]),
             reads=[xtok], writes=["junk", (sttok, 0)])
        S.op("act", lambda e: e.activation(st[:, 1:2], st[:, 0:1], AF.Sqrt, bias=EPS, scale=1.0 / D),
             reads=[(sttok, 0)], writes=[(sttok, 1)])
        S.op("dve", lambda e: e.reciprocal(st[:, 2:3], st[:, 1:2]), reads=[(sttok, 1)], writes=[(sttok, 2)])
        S.op("dve", lambda e: e.scalar_tensor_tensor(t1, xt, st[:, 2:3], At, ALU.mult, ALU.mult),
             reads=[xtok, (sttok, 2), Atok], writes=["t1"])
        S.op("dve", lambda e: e.tensor_tensor(h_out, t1, SH, ALU.add), reads=["t1", SHtok], writes=[h_tok])

    def phaseA(self, l):
        S, A, I = self.S, self.A, self.I
        self.new_phase()
        H, dv = (16, 64) if l == 0 else (8, 128)
        vw = H * (dv + 1)
        pre = "l%d_" % l
        wqkv = A.bf(KC * 3 * D).rearrange("p (k n) -> p k n", k=KC)
        for n in range(6):
            S.dma("pool", wqkv[:, :, n * 512:(n + 1) * 512],
                  I[pre + "w_qkv"][:, n * 512:(n + 1) * 512].rearrange("(k p) n -> p k n", p=128),
                  writes=[("wqkv", n)])
        gain = A.f32(D)
        self.bc_row(gain, I[pre + "norm_mix"], [], "gain")
        Ac, SHc = self.mod_tiles(l, 2, 1, 0, gain, "c")
        if l == 1:
            cs_t = A.f32(NTL * 64).rearrange("p (t j) -> p t j", t=NTL)
            sn_t = A.f32(NTL * 64).rearrange("p (t j) -> p t j", t=NTL)
            S.dma("sp", cs_t, I["rope_cs"], writes=["rope_cs"])
            S.dma("sp", sn_t, I["rope_sn"], writes=["rope_sn"])
        xts = [A.f32(D) for _ in range(2)]
        junk = A.bf(D)
        stt = A.f32(4)
        t1 = A.f32(D)
        hb = A.bf(D)
        hT = [A.bf(KC * 128).rearrange("p (k n) -> p k n", k=KC) for _ in range(2)]
        if l == 1:
            qk32 = A.f32(2 * D)
            r1 = A.f32(2 * D)
            r2 = A.f32(2 * D)
        qkb = A.bf(2 * D)
        qkT = [A.bf(16 * 128).rearrange("p (k n) -> p k n", k=16) for _ in range(2)]
        vaug = [A.bf(vw) for _ in range(2)]
        for i in range(2):
            S.op("pool", lambda e, i=i: e.memset(vaug[i], 1.0), writes=[("vaug", i)])
        pT = self.pbank_bf(0).rearrange("p (k n) -> p k n", k=KC)
        pQT = self.pbank_bf(5, 2).rearrange("p (k n) -> p k n", k=16)
        it = 0
        for bb in range(NB):
            mark = A.top
            Al, SHl = self.mod_tiles(l, bb, 1, 0, gain, "l")
            for t in range(NT):
                b2 = it % 2
                it += 1
                lat = t < NTL
                src, srd = self.xsrc(l, bb, t)
                xt = xts[b2]
                S.dma("sp", xt, src, reads=srd, writes=[("xt", b2)])
                self.rms_mod(xt, ("xt", b2), Al if lat else Ac, ("A", "l" if lat else "c"),
                             SHl if lat else SHc, ("SH", "l" if lat else "c"), hb, "hb", junk, stt, "stA", t1)
                for k in range(KC):
                    S.tr(pT[:, k, :], hb[:, k * 128:(k + 1) * 128], self.ident, reads=["hb", "ident"], writes=["pT"])
                S.op("act", lambda e, b2=b2: e.copy(hT[b2], pT), reads=["pT"], writes=[("hT", b2)])
                va = vaug[b2].rearrange("p (h d) -> p h d", h=H)
                for n in range(6):
                    pq = self.pbank(1 + n % 4)
                    for k in range(KC):
                        S.mm(pq, hT[b2][:, k, :], wqkv[:, k, n * 512:(n + 1) * 512], k == 0, k == KC - 1,
                             reads=[("hT", b2), ("wqkv", n)], writes=[("pq", n % 4)])
                    if n < 4:
                        dst = (qk32 if l == 1 else qkb)[:, n * 512:(n + 1) * 512]
                        dtok = ("qk", n)
                        if n < 2:
                            S.op("act", lambda e, dst=dst, pq=pq: e.mul(dst, pq, 0.125),
                                 reads=[("pq", n % 4)], writes=[dtok])
                        else:
                            S.op("act", lambda e, dst=dst, pq=pq: e.copy(dst, pq),
                                 reads=[("pq", n % 4)], writes=[dtok])
                    else:
                        hpb = 512 // dv
                        h0 = (n - 4) * hpb
                        S.op("dve", lambda e, pq=pq, h0=h0, hpb=hpb, va=va: e.tensor_copy(
                            va[:, h0:h0 + hpb, 0:dv], pq.rearrange("p (h d) -> p h d", h=hpb)),
                            reads=[("pq", n % 4)], writes=[("vaug", b2)])
                qtoks = [("qk", n) for n in range(4)]
                if l == 1:
                    if lat:
                        xv = qk32.rearrange("p (g j) -> p g j", g=32)
                        csb = cs_t[:, t, :].unsqueeze(1).broadcast_to([128, 32, 64])
                        S.op("pool", lambda e, xv=xv, csb=csb: e.tensor_tensor(
                            r1.rearrange("p (g j) -> p g j", g=32), xv, csb, ALU.mult),
                            reads=qtoks + ["rope_cs"], writes=["r1"])
                        x5 = qk32.rearrange("p (g a h j) -> p g a h j", g=32, a=2, h=2)
                        o5 = r2.rearrange("p (g a h j) -> p g a h j", g=32, a=2, h=2)
                        s4 = sn_t[:, t, :].rearrange("p (a h j) -> p a h j", a=2, h=2)
                        for hh in range(2):
                            snb = s4[:, :, hh, :].unsqueeze(1).broadcast_to([128, 32, 2, 16])
                            S.op("dve", lambda e, hh=hh, snb=snb: e.tensor_tensor(
                                o5[:, :, :, hh, :], x5[:, :, :, 1 - hh, :], snb, ALU.mult),
                                reads=qtoks + ["rope_sn"], writes=[("r2", hh)])
                        S.op("dve", lambda e: e.tensor_tensor(qkb, r1, r2, ALU.add),
                             reads=["r1", ("r2", 0), ("r2", 1)], writes=["qkb"])
                    else:
                        S.op("dve", lambda e: e.tensor_copy(qkb, qk32), reads=qtoks, writes=["qkb"])
                    qkb_r = ["qkb"]
                else:
                    qkb_r = qtoks
                for j in range(16):
                    S.tr(pQT[:, j, :], qkb[:, j * 128:(j + 1) * 128], self.ident, reads=qkb_r + ["ident"],
                         writes=["pQT"])
                S.op("act", lambda e, b2=b2: e.copy(qkT[b2], pQT), reads=["pQT"], writes=[("qkT", b2)])
                S.dma("sp", self.QT[bb, :, :, t * 128:(t + 1) * 128].rearrange("c p n -> p c n"),
                      qkT[b2][:, 0:8, :], reads=[("qkT", b2)], writes=[("QT", bb)])
                S.dma("sp", self.KT[bb, :, :, t * 128:(t + 1) * 128].rearrange("c p n -> p c n"),
                      qkT[b2][:, 8:16, :], reads=[("qkT", b2)], writes=[("KT", bb)])
                S.dma("sp", self.VS[bb, t * 128:(t + 1) * 128, 0:vw], vaug[b2], reads=[("vaug", b2)],
                      writes=[("VS", bb)])
            A.top = mark

    def phaseB0(self):
        S, A, I = self.S, self.A, self.I
        self.new_phase()
        bt32 = A.f32(5 * 2 * 640)
        EB = A.bf(5 * 2 * 640)
        EBv = EB.rearrange("p (v h n) -> p v h n", v=5, h=2)
        QTc = [A.bf(T) for _ in range(2)]
        KTc = [A.bf(T) for _ in range(2)]
        Vc = [A.bf(NT * 130).rearrange("p (t h d) -> p t h d", t=NT, h=2) for _ in range(2)]
        Oc = [A.bf(NT * 128).rearrange("p (t n) -> p t n", t=NT) for _ in range(2)]
        NEB = 4
        E = [A.bf(8 * 128).rearrange("p (j n) -> p j n", j=8) for _ in range(NEB)]
        rec = A.f32(8)
        NPS = B0_NPS
        pS = [self.psum[:, 2 * i:2 * i + 2, :].rearrange("p a (j n) -> p (a j) n", n=128) for i in range(NPS)]
        NPO = 8 - 2 * NPS
        pO = self.psum[:, 2 * NPS:8, 0:128]
        var_of = {0: 0, 1: 1, 14: 3, 15: 4}
        it = 0
        qi = 0
        for c in range(KC):
            btv = bt32.rearrange("p (v h n) -> p v h n", v=5, h=2)
            for v5 in range(5):
                S.dma("sp", btv[:, v5], I["na_bias"][v5, 2 * c:2 * c + 2].rearrange("h p n -> p h n"),
                      writes=["bt32"])
            S.op("act", lambda e: e.activation(EB, bt32, AF.Exp), reads=["bt32"], writes=["EB"])
            for bb in range(NB):
                b2 = it % 2
                it += 1
                S.dma("sp", QTc[b2], self.QT[bb, c], writes=[("QTc", b2)])
                S.dma("sp", KTc[b2], self.KT[bb, c], writes=[("KTc", b2)])
                S.dma("sp", Vc[b2].rearrange("p t h d -> p t (h d)"),
                      self.VS[bb, :, 2 * c * 65:(2 * c + 2) * 65].rearrange("(t p) w -> p t w", p=128),
                      writes=[("Vc", b2)])
                LA = B0_LA
                items = [(hh, i) for hh in range(2) for i in range(NT)]
                meta = {}

                def stage1(hh, i, b2=b2):
                    nonlocal qi
                    pr = slice(64 * hh, 64 * hh + 64)
                    if i < NTL:
                        j0 = min(max(i - 2, 0), 11)
                        kts = [j0 + jj for jj in range(5)] + [16, 17]
                        v = var_of.get(i, 2)
                    else:
                        kts = [16, 17]
                        v = None
                    nk = len(kts)
                    sb = qi % NPS
                    eb = qi % NEB
                    slot = qi % NPO
                    qi += 1
                    meta[(hh, i)] = (kts, eb, slot)
                    for jj, kt in enumerate(kts):
                        S.mm(pS[sb][:, jj, :], KTc[b2][pr, kt * 128:(kt + 1) * 128],
                             QTc[b2][pr, i * 128:(i + 1) * 128], True, True,
                             reads=[("KTc", b2), ("QTc", b2)], writes=[("pS", sb)])
                    if nk > 4:
                        S.op("act", lambda e, sb=sb, eb=eb: e.activation(E[eb][:, 0:4, :], pS[sb][:, 0:4, :], AF.Exp),
                             reads=[("pS", sb)], writes=[("E", eb)])
                        S.op("act", lambda e, sb=sb, eb=eb, nk=nk: e.activation(E[eb][:, 4:nk, :], pS[sb][:, 4:nk, :], AF.Exp),
                             reads=[("pS", sb)], writes=[("E", eb)])
                    else:
                        S.op("act", lambda e, sb=sb, eb=eb, nk=nk: e.activation(E[eb][:, 0:nk, :], pS[sb][:, 0:nk, :], AF.Exp),
                             reads=[("pS", sb)], writes=[("E", eb)])
                    if v is not None:
                        S.op("dve", lambda e, eb=eb, v=v, hh=hh: e.tensor_tensor(
                            E[eb][:, 0:5, :], E[eb][:, 0:5, :],
                            EBv[:, v, hh, :].rearrange("p (j n) -> p j n", j=5), ALU.mult),
                            reads=[("E", eb), "EB"], writes=[("E", eb)])

                def stage2(hh, i, b2=b2):
                    kts, eb, slot = meta[(hh, i)]
                    nk = len(kts)
                    for jj, kt in enumerate(kts):
                        S.mm(pO[:, slot, 0:65], E[eb][:, jj, :], Vc[b2][:, kt, hh, :], jj == 0, jj == nk - 1,
                             reads=[("E", eb), ("Vc", b2)], writes=[("pO", slot)])
                    S.op("dve", lambda e, slot=slot: e.reciprocal(rec[:, slot:slot + 1], pO[:, slot, 64:65]),
                         reads=[("pO", slot)], writes=[("rec", slot)])
                    S.op("dve", lambda e, slot=slot, i=i, hh=hh, b2=b2: e.tensor_scalar(
                        Oc[b2][:, i, 64 * hh:64 * hh + 64], pO[:, slot, 0:64], rec[:, slot:slot + 1], None, ALU.mult),
                        reads=[("pO", slot), ("rec", slot)], writes=[("Oc", b2)])

                for idx in range(len(items) + LA):
                    if idx < len(items):
                        stage1(*items[idx])
                    if idx >= LA:
                        stage2(*items[idx - LA])
                S.dma("sp", self.OS[bb, :, c * 128:(c + 1) * 128].rearrange("(t p) n -> p t n", p=128),
                      Oc[b2], reads=[("Oc", b2)], writes=[("OS", bb)])

    def phaseC(self, l):
        S, A, I = self.S, self.A, self.I
        self.new_phase()
        pre = "l%d_" % l
        ntile = NT if l == 0 else NTL
        Wo = A.bf(KC * D).rearrange("p (k n) -> p k n", k=KC)
        for h2_ in range(2):
            S.dma("pool", Wo[:, :, h2_ * 512:(h2_ + 1) * 512],
                  I[pre + "w_o"][:, h2_ * 512:(h2_ + 1) * 512].rearrange("(k p) n -> p k n", p=128), writes=["Wo"])
        WoL = A.bf(KC * D).rearrange("p (k n) -> p k n", k=KC)
        gtmp = A.f32(D)
        gain = A.f32(D)
        self.bc_row(gain, I[pre + "norm_ffn"], [], "gain")
        if l == 0:
            WoC = A.bf(KC * D).rearrange("p (k n) -> p k n", k=KC)
            self.bc_row(gtmp, self.MOD[l, 2, 2 * D:3 * D], [], "gtmp")
            for k in range(KC):
                S.op("dve", lambda e, k=k: e.tensor_tensor(WoC[:, k, :], Wo[:, k, :], gtmp, ALU.mult),
                     reads=["Wo", "gtmp"], writes=["WoC"])
            A2c, SH2c = self.mod_tiles(l, 2, 4, 3, gain, "c")
        else:
            Wr = A.f32(KC * NE).rearrange("p (k n) -> p k n", k=KC)
            S.dma("sp", Wr, I["l1_w_router"].rearrange("(k p) n -> p k n", p=128), writes=["Wr"])
            h2f = A.f32(D)
            h2fT = A.f32(KC * 128).rearrange("p (k n) -> p k n", k=KC)
            lg = A.f32(8 * NE)
            sm = A.f32(8)
        Ots = [A.bf(D) for _ in range(2)]
        xts = [A.f32(D) for _ in range(2)]
        OT = [A.bf(KC * 128).rearrange("p (k n) -> p k n", k=KC) for _ in range(2)]
        x1s = [A.f32(D) for _ in range(2)]
        junk = A.bf(D)
        stt = A.f32(4)
        t1 = A.f32(D)
        h2 = A.bf(D)
        h2T = [A.bf(KC * 128).rearrange("p (k n) -> p k n", k=KC) for _ in range(2)]
        pT = self.pbank_bf(0).rearrange("p (k n) -> p k n", k=KC)
        pY = [self.pbank(1), self.pbank(2)]
        pT2 = self.pbank_bf(3).rearrange("p (k n) -> p k n", k=KC)
        pF = self.psum[:, 4:6, :].rearrange("p a (j n) -> p (a j) n", n=128)
        pL = self.psum[:, 6, 0:NE]
        it = 0
        for bb in range(NB):
            mark = A.top
            self.bc_row(gtmp, self.MOD[l, bb, 2 * D:3 * D], [], "gtmp")
            for k in range(KC):
                S.op("dve", lambda e, k=k: e.tensor_tensor(WoL[:, k, :], Wo[:, k, :], gtmp, ALU.mult),
                     reads=["Wo", "gtmp"], writes=["WoL"])
            A2l, SH2l = self.mod_tiles(l, bb, 4, 3, gain, "l")
            for t in range(ntile):
                b2 = it % 2
                it += 1
                lat = t < NTL
                S.dma("sp", Ots[b2], self.OS[bb, t * 128:(t + 1) * 128, :], writes=[("Ot", b2)])
                src, srd = self.xsrc(l, bb, t)
                S.dma("sp", xts[b2], src, reads=srd, writes=[("xt", b2)])
                for k in range(KC):
                    S.tr(pT[:, k, :], Ots[b2][:, k * 128:(k + 1) * 128], self.ident, reads=[("Ot", b2), "ident"],
                         writes=["pT"])
                S.op("act", lambda e, b2=b2: e.copy(OT[b2], pT), reads=["pT"], writes=[("OT", b2)])
                W = WoL if lat else WoC
                wtok = "WoL" if lat else "WoC"
                for hf in range(2):
                    for k in range(KC):
                        S.mm(pY[hf], OT[b2][:, k, :], W[:, k, hf * 512:(hf + 1) * 512], k == 0, k == KC - 1,
                             reads=[("OT", b2), wtok], writes=[("pY", hf)])
                    S.op("dve", lambda e, hf=hf, b2=b2: e.tensor_tensor(
                        x1s[b2][:, hf * 512:(hf + 1) * 512], pY[hf], xts[b2][:, hf * 512:(hf + 1) * 512], ALU.add),
                        reads=[("pY", hf), ("xt", b2)], writes=[("x1", b2)])
                S.dma("sp", self.XR1[bb, t * 128:(t + 1) * 128, :], x1s[b2], reads=[("x1", b2)],
                      writes=[("XR1", bb, t)])
                if l == 0:
                    self.rms_mod(x1s[b2], ("x1", b2), A2l if lat else A2c, ("A", "l" if lat else "c"),
                                 SH2l if lat else SH2c, ("SH", "l" if lat else "c"), h2, "h2", junk, stt, "stC", t1)
                else:
                    self.rms_mod(x1s[b2], ("x1", b2), A2l, ("A", "l"), SH2l, ("SH", "l"), h2f, "h2f", junk, stt,
                                 "stC", t1)
                    S.op("act", lambda e: e.copy(h2, h2f), reads=["h2f"], writes=["h2"])
                for k in range(KC):
                    S.tr(pT2[:, k, :], h2[:, k * 128:(k + 1) * 128], self.ident, reads=["h2", "ident"], writes=["pT2"])
                S.op("act", lambda e, b2=b2: e.copy(h2T[b2], pT2), reads=["pT2"], writes=[("h2T", b2)])
                col = bb * T + t * 128
                S.dma("sp", self.H2T[:, :, col:col + 128].rearrange("k p n -> p k n"), h2T[b2],
                      reads=[("h2T", b2)], writes=[("H2T", bb, t)])
                if l == 1:
                    for k in range(KC):
                        S.tr(pF[:, k, :], h2f[:, k * 128:(k + 1) * 128], self.ident_f, reads=["h2f", "ident_f"],
                             writes=["pF"])
                    S.op("dve", lambda e: e.tensor_copy(h2fT, pF), reads=["pF"], writes=["h2fT"])
                    for k in range(KC):
                        S.mm(pL, h2fT[:, k, :], Wr[:, k, :], k == 0, k == KC - 1, reads=["h2fT", "Wr"], writes=["pL"])
                    L = lambda i: lg[:, i * NE:(i + 1) * NE]
                    V = lambda i: sm[:, i:i + 1]
                    S.op("dve", lambda e: e.tensor_copy(L(0), pL), reads=["pL"], writes=["g0"])
                    S.op("dve", lambda e: e.reduce_max(V(0), L(0), AX.X), reads=["g0"], writes=["m1"])
                    S.op("dve", lambda e: e.tensor_scalar(L(1), L(0), V(0), None, ALU.is_equal),
                         reads=["g0", "m1"], writes=["g1"])
                    S.op("dve", lambda e: e.scalar_tensor_tensor(L(2), L(1), -1e30, L(0), ALU.mult, ALU.add),
                         reads=["g0", "g1"], writes=["g2"])
                    S.op("dve", lambda e: e.reduce_max(V(1), L(2), AX.X), reads=["g2"], writes=["m2"])
                    S.op("dve", lambda e: e.tensor_scalar(L(3), L(0), V(1), None, ALU.is_ge),
                         reads=["g0", "m2"], writes=["g3"])
                    S.op("dve", lambda e: e.tensor_scalar(V(2), V(0), -1.0, None, ALU.mult), reads=["m1"], writes=["nm1"])
                    S.op("act", lambda e: e.activation(L(4), L(0), AF.Exp, bias=V(2), scale=1.0),
                         reads=["g0", "nm1"], writes=["g4"])
                    S.op("dve", lambda e: e.tensor_tensor(L(5), L(4), L(3), ALU.mult), reads=["g4", "g3"], writes=["g5"])
                    S.op("dve", lambda e: e.reduce_sum(V(3), L(5), AX.X), reads=["g5"], writes=["ssum"])
                    S.op("dve", lambda e: e.reciprocal(V(4), V(3)), reads=["ssum"], writes=["rsum"])
                    S.op("dve", lambda e: e.tensor_scalar(L(6), L(5), V(4), None, ALU.mult), reads=["g5", "rsum"],
                         writes=["g6"])
                    S.dma("sp", self.GAT[bb, t * 128:(t + 1) * 128, :], L(6), reads=["g6"], writes=[("GAT", bb, t)])
            A.top = mark

    def phaseD0(self):
        S, A, I = self.S, self.A, self.I
        self.new_phase()
        NCH = DFF // 128
        Wg = A.bf(KC * DFF).rearrange("p (k n) -> p k n", k=KC)
        Wu = A.bf(KC * DFF).rearrange("p (k n) -> p k n", k=KC)
        Wd = A.bf(NCH * D).rearrange("p (c n) -> p c n", c=NCH)
        for n0 in range(0, DFF, 512):
            n1 = min(n0 + 512, DFF)
            for W, nm in ((Wg, "l0_w_gate"), (Wu, "l0_w_up")):
                S.dma("pool", W[:, :, n0:n1], I[nm][:, n0:n1].rearrange("(k p) n -> p k n", p=128),
                      writes=[(nm, n0)])
        wd_src = I["l0_w_down"].rearrange("(c p) n -> p c n", p=128)
        for c0 in range(0, NCH, 4):
            c1 = min(c0 + 4, NCH)
            S.dma("pool", Wd[:, c0:c1, :], wd_src[:, c0:c1, :], writes=[("wd", c0)])
        wg_toks = [("l0_w_gate", n0) for n0 in range(0, DFF, 512)]
        wu_toks = [("l0_w_up", n0) for n0 in range(0, DFF, 512)]
        wd_toks = [("wd", c0) for c0 in range(0, NCH, 4)]
        G2c = A.f32(D)
        G2l = A.f32(D)
        self.bc_row(G2c, self.MOD[0, 2, 5 * D:6 * D], [], "G2c")
        hT = [A.bf(KC * 512).rearrange("p (k n) -> p k n", k=KC) for _ in range(2)]
        AT = A.bf(NCH * 512).rearrange("p (c n) -> p c n", c=NCH)
        sg = [A.bf(512) for _ in range(2)]
        xts = [A.f32(D) for _ in range(2)]
        x2s = [A.f32(D) for _ in range(2)]
        tmp = A.f32(512)
        pG = [self.pbank(0), self.pbank(1)]
        pU = [self.pbank(2), self.pbank(3)]
        pY = [self.pbank(4 + i) for i in range(4)]
        ngroups = NB * NT // 4
        ti = 0
        yi = 0
        cur_bb = -1
        for g in range(ngroups):
            hb = g % 2
            S.dma("sp", hT[hb], self.H2T[:, :, g * 512:(g + 1) * 512].rearrange("k p n -> p k n"),
                  writes=[("hT", hb)])
            for ci in range(NCH):
                pb = ci % 2
                for k in range(KC):
                    S.mm(pG[pb], Wg[:, k, ci * 128:(ci + 1) * 128], hT[hb][:, k, :], k == 0, k == KC - 1,
                         reads=[("hT", hb), wg_toks[ci // 4]], writes=[("pG", pb)])
                for k in range(KC):
                    S.mm(pU[pb], Wu[:, k, ci * 128:(ci + 1) * 128], hT[hb][:, k, :], k == 0, k == KC - 1,
                         reads=[("hT", hb), wu_toks[ci // 4]], writes=[("pU", pb)])
                S.op("act", lambda e, pb=pb: e.activation(sg[pb], pG[pb], AF.Silu), reads=[("pG", pb)],
                     writes=[("sg", pb)])
                S.op("dve", lambda e, pb=pb, ci=ci: e.tensor_tensor(AT[:, ci, :], sg[pb], pU[pb], ALU.mult),
                     reads=[("sg", pb), ("pU", pb)], writes=[("AT", ci)])
            at_toks = [("AT", ci) for ci in range(NCH)]
            for j in range(4):
                bb, t = divmod(4 * g + j, NT)
                lat = t < NTL
                if lat and bb != cur_bb:
                    cur_bb = bb
                    self.bc_row(G2l, self.MOD[0, bb, 5 * D:6 * D], [], "G2l")
                b2 = ti % 2
                ti += 1
                S.dma("sp", xts[b2], self.XR1[bb, t * 128:(t + 1) * 128, :], writes=[("xt", b2)])
                G2, gtok = (G2l, "G2l") if lat else (G2c, "G2c")
                for hf in range(2):
                    yb = yi % 4
                    yi += 1
                    for ci in range(NCH):
                        S.mm(pY[yb], AT[:, ci, j * 128:(j + 1) * 128], Wd[:, ci, hf * 512:(hf + 1) * 512],
                             ci == 0, ci == NCH - 1, reads=[("AT", ci), wd_toks[ci // 4]], writes=[("pY", yb)])
                    S.op("dve", lambda e, yb=yb, hf=hf, G2=G2: e.tensor_tensor(
                        tmp, pY[yb], G2[:, hf * 512:(hf + 1) * 512], ALU.mult),
                        reads=[("pY", yb), gtok], writes=["tmp"])
                    S.op("dve", lambda e, hf=hf, b2=b2: e.tensor_tensor(
                        x2s[b2][:, hf * 512:(hf + 1) * 512], tmp, xts[b2][:, hf * 512:(hf + 1) * 512], ALU.add),
                        reads=["tmp", ("xt", b2)], writes=[("x2", b2)])
                S.dma("sp", self.XR2[bb, t * 128:(t + 1) * 128, :], x2s[b2], reads=[("x2", b2)],
                      writes=[("XR2", bb, t)])

    def phaseB1(self):
        S, A, I = self.S, self.A, self.I
        self.new_phase()
        lam_init = 0.8 - 0.6 * math.exp(-0.3 * 1)
        lv = A.f32(4 * 64).rearrange("p (a n) -> p a n", a=4)
        for a, nm in enumerate(("q1", "k1", "q2", "k2")):
            self.bc_row(lv[:, a, :], I["l1_lambda_" + nm], [], ("lv", a))
        lp = A.f32(2 * 64).rearrange("p (a n) -> p a n", a=2)
        ls = A.f32(8)
        for a in range(2):
            S.op("dve", lambda e, a=a: e.tensor_tensor(lp[:, a, :], lv[:, 2 * a, :], lv[:, 2 * a + 1, :], ALU.mult),
                 reads=[("lv", 2 * a), ("lv", 2 * a + 1)], writes=[("lp", a)])
            S.op("dve", lambda e, a=a: e.reduce_sum(ls[:, a:a + 1], lp[:, a, :], AX.X), reads=[("lp", a)],
                 writes=[("ls", a)])
        S.op("act", lambda e: e.activation(ls[:, 2:4], ls[:, 0:2], AF.Exp), reads=[("ls", 0), ("ls", 1)],
             writes=["lexp"])
        S.op("dve", lambda e: e.tensor_tensor(ls[:, 4:5], ls[:, 3:4], ls[:, 2:3], ALU.subtract), reads=["lexp"],
             writes=["ldiff"])
        S.op("dve", lambda e: e.tensor_scalar(ls[:, 5:6], ls[:, 4:5], -lam_init, None, ALU.add), reads=["ldiff"],
             writes=["nlam"])
        nlam = ls[:, 5:6]
        SUB = A.f32(128)
        self.bc_row(SUB, I["l1_subln"], [], "SUB0")
        S.op("dve", lambda e: e.tensor_scalar(SUB, SUB, 1.0 - lam_init, None, ALU.mult), reads=["SUB0"], writes=["SUB"])
        QTh = [A.bf(SEQ) for _ in range(2)]
        KTh = [A.bf(T) for _ in range(2)]
        Vh = [A.bf(NT * 129).rearrange("p (t d) -> p t d", t=NT) for _ in range(2)]
        NEB = 4
        E = [A.bf(512) for _ in range(NEB)]
        accS = [A.f32(8 * 129) for _ in range(2)]
        ob = A.f32(4 * 128).rearrange("p (q n) -> p q n", q=4)
        o1 = A.f32(128)
        sq = A.f32(128)
        rr = A.f32(16)
        Oc = [A.bf(NTL * 128).rearrange("p (t n) -> p t n", t=NTL) for _ in range(2)]
        pS = [self.pbank(i) for i in range(4)]

        def acc(s_):
            b = 4 + s_ // 3
            off = (s_ % 3) * 129
            return self.psum[:, b, off:off + 129]

        it = 0
        si = 0
        ci = 0
        for bb in range(NB):
            for h in range(KC):
                b2 = it % 2
                it += 1
                S.dma("sp", QTh[b2], self.QT[bb, h, :, 0:SEQ], writes=[("QTh", b2)])
                S.dma("sp", KTh[b2], self.KT[bb, h], writes=[("KTh", b2)])
                S.dma("sp", Vh[b2], self.VS[bb, :, h * 129:(h + 1) * 129].rearrange("(t p) w -> p t w", p=128),
                      writes=[("Vh", b2)])
                LA = 2
                steps = [(qc, kt, m) for qc in range(4) for kt in range(NT) for m in range(2)]
                meta = {}

                def st1(qc, kt, m, b2=b2):
                    nonlocal si
                    sb = si % 4
                    eb = si % NEB
                    si += 1
                    meta[(qc, kt, m)] = eb
                    pr = slice(64 * m, 64 * m + 64)
                    S.mm(pS[sb], KTh[b2][pr, kt * 128:(kt + 1) * 128], QTh[b2][pr, qc * 512:(qc + 1) * 512],
                         True, True, reads=[("KTh", b2), ("QTh", b2)], writes=[("pS", sb)])
                    S.op("act", lambda e, sb=sb, eb=eb: e.activation(E[eb], pS[sb], AF.Exp),
                         reads=[("pS", sb)], writes=[("E", eb)])

                def st2(qc, kt, m, b2=b2):
                    eb = meta[(qc, kt, m)]
                    for qt in range(4):
                        sl = m * 4 + qt
                        S.mm(acc(sl), E[eb][:, qt * 128:(qt + 1) * 128], Vh[b2][:, kt, :],
                             kt == 0 and sl % 3 == 0, kt == NT - 1, reads=[("E", eb), ("Vh", b2)],
                             writes=[("acc", sl)], skip_group_check=True)
                    if kt == NT - 1 and m == 1:
                        epilogue(qc, b2)

                def epilogue(qc, b2):
                    nonlocal ci
                    ab = ci % 2
                    ci += 1
                    aS = accS[ab]
                    for bk in range(3):
                        n = 387 if bk < 2 else 258
                        S.op("dve", lambda e, bk=bk, n=n, aS=aS: e.tensor_copy(aS[:, bk * 387:bk * 387 + n],
                                                                             self.psum[:, 4 + bk, 0:n]),
                             reads=[("acc", 3 * bk + j) for j in range(3) if 3 * bk + j < 8], writes=[("accS", ab, bk)])
                    atoks = [("accS", ab, bk) for bk in range(3)]
                    a3 = aS.rearrange("p (s n) -> p s n", n=129)
                    S.op("dve", lambda e, a3=a3: e.reciprocal(rr[:, 0:8], a3[:, :, 128]), reads=atoks, writes=["rr"])
                    S.op("dve", lambda e: e.tensor_scalar(rr[:, 8:12], rr[:, 4:8], nlam, None, ALU.mult),
                         reads=["rr", "nlam"], writes=["rr2"])
                    for qt in range(4):
                        S.op("dve", lambda e, qt=qt, a3=a3: e.tensor_scalar(o1, a3[:, qt, 0:128], rr[:, qt:qt + 1], None,
                                                                        ALU.mult), reads=atoks + ["rr"], writes=["o1"])
                        S.op("dve", lambda e, qt=qt, a3=a3: e.scalar_tensor_tensor(
                            ob[:, qt, :], a3[:, 4 + qt, 0:128], rr[:, 8 + qt:9 + qt], o1, ALU.mult, ALU.add),
                            reads=atoks + ["rr2", "o1"], writes=[("ob", qt)])
                        S.op("dve", lambda e, qt=qt: e.tensor_tensor(sq, ob[:, qt, :], ob[:, qt, :], ALU.mult),
                             reads=[("ob", qt)], writes=["sq"])
                        S.op("dve", lambda e, qt=qt: e.reduce_sum(rr[:, 12 + qt:13 + qt], sq, AX.X), reads=["sq"],
                             writes=[("ss", qt)])
                    sst = [("ss", qt) for qt in range(4)]
                    S.op("act", lambda e: e.activation(rr[:, 12:16], rr[:, 12:16], AF.Ln, bias=EPS, scale=1.0 / 128),
                         reads=sst, writes=["lnv"])
                    S.op("act", lambda e: e.activation(rr[:, 12:16], rr[:, 12:16], AF.Exp, scale=-0.5), reads=["lnv"],
                         writes=["rstd"])
                    for qt in range(4):
                        S.op("dve", lambda e, qt=qt, b2=b2, qc=qc: e.scalar_tensor_tensor(
                            Oc[b2][:, qc * 4 + qt, :], ob[:, qt, :], rr[:, 12 + qt:13 + qt], SUB, ALU.mult, ALU.mult),
                            reads=[("ob", qt), "rstd", "SUB"], writes=[("Oc", b2)])

                for idx in range(len(steps) + LA):
                    if idx < len(steps):
                        st1(*steps[idx])
                    if idx >= LA:
                        st2(*steps[idx - LA])
                S.dma("sp", self.OS[bb, 0:SEQ, h * 128:(h + 1) * 128].rearrange("(t p) n -> p t n", p=128),
                      Oc[b2], reads=[("Oc", b2)], writes=[("OS", bb)])

    def phaseD1(self):
        S, A, I = self.S, self.A, self.I
        self.new_phase()
        NG = DFE // 512
        hT = A.bf(KC * SEQ).rearrange("p (k n) -> p k n", k=KC)
        Y = A.f32(NTL * D).rearrange("p (t n) -> p t n", t=NTL)
        gat = A.f32(NTL * NE).rearrange("p (t e) -> p t e", t=NTL)
        wg = [A.bf(KC * 512).rearrange("p (k n) -> p k n", k=KC) for _ in range(2)]
        wu = [A.bf(KC * 512).rearrange("p (k n) -> p k n", k=KC) for _ in range(2)]
        wd = [A.bf(4 * D).rearrange("p (c n) -> p c n", c=4) for _ in range(2)]
        AT = [A.bf(4 * 512).rearrange("p (c n) -> p c n", c=4) for _ in range(2)]
        sg = [A.bf(512) for _ in range(2)]
        G2l = A.f32(D)
        fin = A.f32(D)
        self.bc_row(fin, I["final_norm"], [], "fin")
        xts = [A.f32(D) for _ in range(2)]
        x2s = [A.f32(D) for _ in range(2)]
        outs = [A.f32(D) for _ in range(2)]
        junk = A.bf(D)
        stt = A.f32(4)
        pG = [self.pbank(0), self.pbank(1)]
        pU = [self.pbank(2), self.pbank(3)]
        pY = [self.pbank(4 + i) for i in range(4)]
        wi = 0
        ai = 0
        yi = 0
        pi = 0
        ti = 0
        for bb in range(NB):
            S.dma("sp", hT, self.H2T[:, :, bb * T:bb * T + SEQ].rearrange("k p n -> p k n"), writes=["hT"])
            S.dma("sp", gat, self.GAT[bb].rearrange("(t p) e -> p t e", p=128), writes=["gat"])
            self.bc_row(G2l, self.MOD[1, bb, 5 * D:6 * D], [], "G2l")
            for ex in range(NE):
                for g in range(NG):
                    wb = wi % 2
                    wi += 1
                    S.dma("pool", wg[wb], I["l1_w_gate"][ex][:, g * 512:(g + 1) * 512].rearrange("(k p) n -> p k n", p=128),
                          writes=[("wg", wb)])
                    S.dma("pool", wu[wb], I["l1_w_up"][ex][:, g * 512:(g + 1) * 512].rearrange("(k p) n -> p k n", p=128),
                          writes=[("wu", wb)])
                    S.dma("pool", wd[wb], I["l1_w_down"][ex][g * 512:(g + 1) * 512, :].rearrange("(c p) n -> p c n", p=128),
                          writes=[("wd", wb)])
                    first = (ex == 0 and g == 0)
                    for tg in range(4):
                        ab = ai % 2
                        ai += 1
                        for ci in range(4):
                            pb = pi % 2
                            pi += 1
                            for k in range(KC):
                                S.mm(pG[pb], wg[wb][:, k, ci * 128:(ci + 1) * 128], hT[:, k, tg * 512:(tg + 1) * 512],
                                     k == 0, k == KC - 1, reads=["hT", ("wg", wb)], writes=[("pG", pb)])
                            for k in range(KC):
                                S.mm(pU[pb], wu[wb][:, k, ci * 128:(ci + 1) * 128], hT[:, k, tg * 512:(tg + 1) * 512],
                                     k == 0, k == KC - 1, reads=["hT", ("wu", wb)], writes=[("pU", pb)])
                            S.op("act", lambda e, pb=pb: e.activation(sg[pb], pG[pb], AF.Silu), reads=[("pG", pb)],
                                 writes=[("sg", pb)])
                            S.op("dve", lambda e, pb=pb, ci=ci, ab=ab: e.tensor_tensor(AT[ab][:, ci, :], sg[pb], pU[pb],
                                                                                   ALU.mult),
                                 reads=[("sg", pb), ("pU", pb)], writes=[("AT", ab)])
                        for j in range(4):
                            tile = tg * 4 + j
                            for hf in range(2):
                                yb = yi % 4
                                yi += 1
                                for ci in range(4):
                                    S.mm(pY[yb], AT[ab][:, ci, j * 128:(j + 1) * 128], wd[wb][:, ci, hf * 512:(hf + 1) * 512],
                                         ci == 0, ci == 3, reads=[("AT", ab), ("wd", wb)], writes=[("pY", yb)])
                                ysl = Y[:, tile, hf * 512:(hf + 1) * 512]
                                gsc = gat[:, tile, ex:ex + 1]
                                if first:
                                    S.op("dve", lambda e, ysl=ysl, gsc=gsc, yb=yb: e.tensor_scalar(ysl, pY[yb], gsc, None,
                                                                                                  ALU.mult),
                                         reads=[("pY", yb), "gat"], writes=[("Y", tile, hf)])
                                else:
                                    S.op("dve", lambda e, ysl=ysl, gsc=gsc, yb=yb: e.scalar_tensor_tensor(
                                        ysl, pY[yb], gsc, ysl, ALU.mult, ALU.add),
                                        reads=[("pY", yb), "gat", ("Y", tile, hf)], writes=[("Y", tile, hf)])
            for tile in range(NTL):
                b2 = ti % 2
                ti += 1
                S.dma("sp", xts[b2], self.XR1[bb, tile * 128:(tile + 1) * 128, :], writes=[("xt", b2)])
                S.op("dve", lambda e, tile=tile, b2=b2: e.tensor_tensor(x2s[b2], Y[:, tile, :], G2l, ALU.mult),
                     reads=[("Y", tile, 0), ("Y", tile, 1), "G2l"], writes=[("x2a", b2)])
                S.op("dve", lambda e, b2=b2: e.tensor_tensor(x2s[b2], x2s[b2], xts[b2], ALU.add),
                     reads=[("x2a", b2), ("xt", b2)], writes=[("x2", b2)])
                if self.dbg:
                    S.dma("sp", self.XR2[bb, tile * 128:(tile + 1) * 128, :], x2s[b2], reads=[("x2", b2)],
                          writes=[("XR2", bb, tile)])
                S.op("act", lambda e, b2=b2: e.activation(junk, x2s[b2], AF.Square, accum_out=stt[:, 0:1]),
                     reads=[("x2", b2)], writes=["junk", "ssq"])
                S.op("act", lambda e: e.activation(stt[:, 1:2], stt[:, 0:1], AF.Sqrt, bias=EPS, scale=1.0 / D),
                     reads=["ssq"], writes=["std"])
                S.op("dve", lambda e: e.reciprocal(stt[:, 2:3], stt[:, 1:2]), reads=["std"], writes=["rstd"])
                S.op("dve", lambda e, b2=b2: e.scalar_tensor_tensor(outs[b2], x2s[b2], stt[:, 2:3], fin, ALU.mult, ALU.mult),
                     reads=[("x2", b2), "rstd", "fin"], writes=[("out", b2)])
                S.dma("sp", self.out[bb, tile * 128:(tile + 1) * 128, :], outs[b2], reads=[("out", b2)],
                      writes=[("OUT", bb, tile)])


def _const_tables():
    ident = np.eye(128, dtype=np.float32)
    pos = np.arange(SEQ)
    row = (pos // GRID_W).astype(np.float64)
    col = (pos % GRID_W).astype(np.float64)
    inv = 10000.0 ** (-np.arange(0, 32, 2, dtype=np.float64) / 32.0)
    ar = row[:, None] * inv[None]
    ac = col[:, None] * inv[None]
    cs = np.concatenate([np.cos(ar), np.cos(ar), np.cos(ac), np.cos(ac)], -1)
    sn = np.concatenate([-np.sin(ar), np.sin(ar), -np.sin(ac), np.sin(ac)], -1)
    cs = cs.reshape(NTL, 128, 64).transpose(1, 0, 2).astype(np.float32)
    sn = sn.reshape(NTL, 128, 64).transpose(1, 0, 2).astype(np.float32)
    return ident, np.ascontiguousarray(cs), np.ascontiguousarray(sn)


NA_VAR_TILES = (0, 1, 5, 14, 15)


def _na_index():
    kp = np.arange(128)
    kr, kc = kp // 64, kp % 64
    qp = np.arange(128)
    qr, qc = qp // 64, qp % 64
    dr = np.zeros((5, 128, 5, 128), np.int64)
    dc = np.zeros((5, 128, 5, 128), np.int64)
    ok = np.zeros((5, 128, 5, 128), bool)
    for v, i in enumerate(NA_VAR_TILES):
        j0 = min(max(i - 2, 0), 11)
        for jj in range(5):
            rk = 2 * (j0 + jj) + kr
            r = 2 * i + qr
            rs = np.clip(r - 4, 0, 24)
            vrow = (rk[:, None] >= rs[None, :]) & (rk[:, None] < rs[None, :] + 8)
            ws = np.clip(qc - 8, 0, 48)
            vcol = (kc[:, None] >= ws[None, :]) & (kc[:, None] < ws[None, :] + 16)
            val = vrow & vcol
            ok[v, :, jj, :] = val
            dr[v, :, jj, :] = np.where(val, rk[:, None] - r[None, :] + 7, 0)
            dc[v, :, jj, :] = np.where(val, kc[:, None] - qc[None, :] + 15, 0)
    return dr, dc, ok


def _na_bias(rpb):
    dr, dc, ok = _na_index()
    tab = rpb[:, dr, dc]
    tab = np.where(ok[None], tab, np.float32(-30000.0))
    tab = tab.transpose(1, 0, 2, 3, 4).reshape(5, 16, 128, 640)
    return np.ascontiguousarray(tab.astype(np.float32))


def make_in_maps(inputs, n_cores=8):
    ident, cs, sn = _const_tables()
    nab = _na_bias(np.asarray(inputs["l0_rpb"], np.float32))
    shared = {}
    for k, v in inputs.items():
        if k in ("x", "c", "ctx", "l0_rpb"):
            continue
        shared[k] = np.ascontiguousarray(np.asarray(v, np.float32))
    shared["ident"] = ident
    shared["rope_cs"] = cs
    shared["rope_sn"] = sn
    shared["na_bias"] = nab
    maps = []
    for c in range(n_cores):
        m = dict(shared)
        m["x"] = np.ascontiguousarray(inputs["x"][c * NB:(c + 1) * NB])
        m["ctx"] = np.ascontiguousarray(inputs["ctx"][c * NB:(c + 1) * NB])
        m["c"] = np.ascontiguousarray(inputs["c"][c * NB:(c + 1) * NB])
        maps.append(m)
    return maps


_NC_CACHE = {}


def kernel(**inputs):
    if "nc" not in _NC_CACHE:
        _NC_CACHE["nc"] = Builder().build()
    nc = _NC_CACHE["nc"]
    maps = make_in_maps(inputs)
    res = run_bass_kernel_spmd(nc, maps, core_ids=list(range(8)))
    out = np.concatenate([np.asarray(r["out"]) for r in res.results], axis=0)
    return out.astype(np.float32, copy=False)
```

```python
import math
from contextlib import ExitStack

import numpy as np
import concourse.bass as bass
import concourse.mybir as mybir
from concourse.bass_utils import run_bass_kernel_spmd

F32 = mybir.dt.float32
BF16 = mybir.dt.bfloat16
I32 = mybir.dt.int32
AF = mybir.ActivationFunctionType
ALU = mybir.AluOpType
AX = mybir.AxisListType

ENGS = ("pe", "act", "dve", "pool", "sp")
N_DMA_SEMS = 12


class Op:
    __slots__ = ("eng", "fn", "deps", "is_dma", "signal", "sem", "val", "idx", "prev_dma")

    def __init__(self, eng, fn, is_dma, idx):
        self.eng = eng
        self.fn = fn
        self.deps = set()
        self.is_dma = is_dma
        self.signal = is_dma
        self.sem = None
        self.val = 0
        self.idx = idx
        self.prev_dma = None


class Sched:
    def __init__(self, nc):
        self.nc = nc
        self.ops = []
        self.last_w = {}
        self.readers = {}

    def op(self, eng, fn, reads=(), writes=(), dma=False):
        o = Op(eng, fn, dma, len(self.ops))
        self.ops.append(o)
        for t in reads:
            w = self.last_w.get(t)
            if w is not None:
                self._dep(o, w, raw=True)
        for t in writes:
            w = self.last_w.get(t)
            if w is not None:
                self._dep(o, w, raw=False)
            rd = self.readers.get(t)
            if rd:
                for k, r in rd.items():
                    if k == "dma":
                        for rr in r:
                            self._dep(o, rr, raw=False)
                    else:
                        self._dep(o, r, raw=False)
        for t in reads:
            rd = self.readers.setdefault(t, {})
            if dma:
                rd.setdefault("dma", []).append(o)
            else:
                rd[eng] = o
        for t in writes:
            self.last_w[t] = o
            self.readers[t] = {}
        return o

    def _dep(self, o, d, raw):
        if d is o:
            return
        if d.is_dma or o.is_dma:
            o.deps.add(d)
            d.signal = True
            return
        if d.eng == o.eng:
            if raw and o.eng != "pe":
                o.deps.add(d)
                d.signal = True
            return
        o.deps.add(d)
        d.signal = True

    def dma(self, q, out, in_, reads=(), writes=(), **kw):
        return self.op(q, lambda e: e.dma_start(out=out, in_=in_, **kw), reads, writes, dma=True)

    def mm(self, out, lhsT, rhs, start, stop, reads=(), writes=(), **kw):
        return self.op("pe", lambda e: e.matmul(out, lhsT, rhs, start=start, stop=stop, **kw),
                       reads, writes)

    def tr(self, out, in_, ident, reads=(), writes=()):
        return self.op("pe", lambda e: e.transpose(out, in_, ident), reads, writes)

    def emit(self, stack):
        nc = self.nc
        sems = {e: stack.enter_context(nc.semaphore("s_" + e)) for e in ENGS}
        dsems = {}
        dcount = {}
        dnext = {}
        dlast = {}
        cnt = {e: 0 for e in ENGS}
        per_eng = {e: [] for e in ENGS}
        for o in self.ops:
            per_eng[o.eng].append(o)
            if o.is_dma:
                q = o.eng
                if q not in dsems:
                    dsems[q] = [stack.enter_context(nc.semaphore("d_%s%d" % (q, i)))
                                for i in range(N_DMA_SEMS)]
                    dnext[q] = 0
                i = dnext[q]
                dnext[q] = (i + 1) % N_DMA_SEMS
                s = dsems[q][i]
                o.prev_dma = dlast.get(s)
                dlast[s] = o
                dcount[s] = dcount.get(s, 0) + 16
                o.sem = s
                o.val = dcount[s]
            elif o.signal:
                cnt[o.eng] += 1
                o.sem = sems[o.eng]
                o.val = cnt[o.eng]
        all_dma_final = dict(dcount)
        block = stack.enter_context(nc.Block())

        def run(eng_name):
            def body(e):
                waited = {}
                for o in per_eng[eng_name]:
                    need = {}
                    for d in o.deps:
                        if need.get(d.sem, 0) < d.val:
                            need[d.sem] = d.val
                    if o.prev_dma is not None:
                        p = o.prev_dma
                        if need.get(p.sem, 0) < p.val:
                            need[p.sem] = p.val
                    for s, v in need.items():
                        if waited.get(s, 0) < v:
                            e.wait_ge(s, v)
                            waited[s] = v
                    ins = o.fn(e)
                    if o.is_dma:
                        ins.then_inc(o.sem, 16)
                    elif o.signal:
                        ins.then_inc(o.sem, 1)
                if eng_name == "sp":
                    for s, v in all_dma_final.items():
                        if waited.get(s, 0) < v:
                            e.wait_ge(s, v)
            return body

        block.tensor(run("pe"))
        block.scalar(run("act"))
        block.vector(run("dve"))
        block.gpsimd(run("pool"))
        block.sync(run("sp"))

    def barrier(self):
        last = {}
        dmas = []
        for o in self.ops:
            if o.is_dma:
                dmas.append(o)
            else:
                last[o.eng] = o
        self._bar = list(last.values()) + dmas
        self._bar_seen = set()
        self.last_w = {}
        self.readers = {}

    _bar = None
    _bar_seen = None


_orig_op = Sched.op


def _op_with_barrier(self, eng, fn, reads=(), writes=(), dma=False):
    o = _orig_op(self, eng, fn, reads, writes, dma)
    if self._bar is not None and eng not in self._bar_seen:
        self._bar_seen.add(eng)
        for d in self._bar:
            if d is o:
                continue
            if d.is_dma or d.eng != eng:
                o.deps.add(d)
                d.signal = True
    return o


Sched.op = _op_with_barrier

B0_LA = 2
B0_NPS = 3
NB = 2
SEQ = 2048
CTX = 256
T = SEQ + CTX
NT = T // 128
NTL = SEQ // 128
D = 1024
KC = 8
DFF = 2816
NE = 8
DFE = 3584
EPS = 1e-6
GRID_W = 64
VW = 1040


class Arena:
    def __init__(self, ap, words):
        self.ap = ap
        self.words = words
        self.top = 0

    def f32(self, n):
        assert self.top + n <= self.words, "arena overflow %d" % (self.top + n)
        a = self.ap[:, self.top:self.top + n]
        self.top += n
        return a

    def bf(self, n):
        assert n % 2 == 0
        return self.f32(n // 2).bitcast(BF16)


class Builder:
    def __init__(self, dbg=False, phases=None):
        self.dbg = dbg
        self.phases = phases
        self.nc = bass.Bass("TRN2", target_bir_lowering=False)
        self.S = Sched(self.nc)
        self.inputs = {}

    def din(self, name, shape, dt=F32):
        t = self.nc.dram_tensor(name, list(shape), dt, kind="ExternalInput").ap()
        self.inputs[name] = t
        return t

    def dscr(self, name, shape, dt):
        kind = "ExternalOutput" if self.dbg else "Internal"
        return self.nc.dram_tensor(name, list(shape), dt, kind=kind).ap()

    def want(self, ph):
        return self.phases is None or ph in self.phases

    def build(self):
        nc = self.nc
        shapes = {"x": [NB, SEQ, D], "ctx": [NB, CTX, D], "c": [NB, D], "c_ctx": [D],
                  "l0_w_gate": [D, DFF], "l0_w_up": [D, DFF], "l0_w_down": [DFF, D],
                  "na_bias": [5, 16, 128, 640], "l1_subln": [128], "l1_w_router": [D, NE],
                  "l1_w_gate": [NE, D, DFE], "l1_w_up": [NE, D, DFE], "l1_w_down": [NE, DFE, D],
                  "final_norm": [D], "ident": [128, 128], "rope_cs": [128, NTL, 64], "rope_sn": [128, NTL, 64]}
        shapes.update({"l0_w_ada": [D, 6 * D], "l0_b_ada": [6 * D], "l0_norm_mix": [D], "l0_w_qkv": [D, 3 * D],
                       "l0_w_o": [D, D], "l0_norm_ffn": [D], "l1_w_ada": [D, 6 * D], "l1_b_ada": [6 * D],
                       "l1_norm_mix": [D], "l1_w_qkv": [D, 3 * D], "l1_w_o": [D, D], "l1_norm_ffn": [D]})
        for k in ("q1", "k1", "q2", "k2"):
            shapes["l1_lambda_" + k] = [64]
        bld = self

        class Lazy(dict):
            def __missing__(d, name):
                if bld.dbg:
                    d[name] = bld.din(name, shapes[name])
                    return d[name]
                raise KeyError(name)

        I = Lazy()
        if not self.dbg:
            for name in sorted(shapes):
                I[name] = self.din(name, shapes[name])
        self.I = I
        self.out = nc.dram_tensor("out", [NB, SEQ, D], F32, kind="ExternalOutput").ap()
        self.MOD = self.dscr("MOD", [2, 3, 6 * D], F32)
        self.QT = self.dscr("QT", [NB, KC, 128, T], BF16)
        self.KT = self.dscr("KT", [NB, KC, 128, T], BF16)
        self.VS = self.dscr("VS", [NB, T, VW], BF16)
        self.OS = self.dscr("OS", [NB, T, D], BF16)
        self.XR1 = self.dscr("XR1", [NB, T, D], F32)
        self.XR2 = self.dscr("XR2", [NB, T, D], F32)
        self.H2T = self.dscr("H2T", [KC, 128, NB * T], BF16)
        self.GAT = self.dscr("GAT", [NB, SEQ, NE], F32)

        with ExitStack() as st:
            AW = 51200
            arena_t = st.enter_context(nc.sbuf_tensor("arena", [128, AW], F32))
            self.psum = st.enter_context(nc.psum_tensor("psum", [128, 8, 512], F32))
            self.A = Arena(arena_t, AW)
            self.ident_f = self.A.f32(128)
            self.ident = self.A.bf(128)
            S = self.S
            S.dma("sp", self.ident_f, I["ident"], writes=["ident_f"])
            S.dma("pool", self.ident, I["ident"], writes=["ident"])
            self.base = self.A.top
            if self.want("0"):
                self.phase0()
            for l in range(2):
                if self.want("A%d" % l):
                    self.phaseA(l)
                if self.want("B%d" % l):
                    (self.phaseB0 if l == 0 else self.phaseB1)()
                if self.want("C%d" % l):
                    self.phaseC(l)
                if self.want("D%d" % l):
                    (self.phaseD0 if l == 0 else self.phaseD1)()
            S.emit(st)
        return nc

    def new_phase(self):
        self.S.barrier()
        self.A.top = self.base

    def pbank(self, b):
        return self.psum[:, b, :]

    def pbank_bf(self, b, nb=1):
        v = self.psum[:, b:b + nb, :].rearrange("p a b -> p (a b)").bitcast(BF16)
        return v

    def phase0(self):
        S, A, I = self.S, self.A, self.I
        self.new_phase()
        cT = A.f32(24).rearrange("p (k j) -> p k j", k=KC)
        cs = A.f32(24).rearrange("p (k j) -> p k j", k=KC)
        srcs = [I["c"][0], I["c"][1], I["c_ctx"]]
        for j, src in enumerate(srcs):
            S.dma("sp", cT[:, :, j], src.rearrange("(k p) -> p k", p=128), writes=["cT"],
                  allow_slow_non_contiguous=True)
        S.op("act", lambda e: e.activation(cs, cT, AF.Silu), reads=["cT"], writes=["cs"])
        bt = A.f32(6 * D)
        modv = A.f32(6 * D)
        wb = [A.f32(KC * 512).rearrange("p (k n) -> p k n", k=KC) for _ in range(2)]
        it = 0
        for l in range(2):
            S.dma("sp", bt[0:3, :], I["l%d_b_ada" % l].partition_broadcast(3), reads=[], writes=["bt"])
            for n in range(12):
                b = it % 2
                it += 1
                S.dma("sp", wb[b], I["l%d_w_ada" % l][:, n * 512:(n + 1) * 512].rearrange("(k p) n -> p k n", p=128),
                      writes=[("wb", b)])
                pb = self.psum[0:3, n % 2, :]
                for k in range(KC):
                    S.mm(pb, cs[:, k, :], wb[b][:, k, :], k == 0, k == KC - 1,
                         reads=["cs", ("wb", b)], writes=[("p0", n % 2)])
                S.op("dve", lambda e, pb=pb, n=n: e.tensor_tensor(modv[0:3, n * 512:(n + 1) * 512], pb,
                                                                 bt[0:3, n * 512:(n + 1) * 512], ALU.add),
                     reads=[("p0", n % 2), "bt"], writes=["modv"])
            S.dma("sp", self.MOD[l], modv[0:3, :], reads=["modv"], writes=[("MOD", l)])

    def bc_row(self, dst, row, reads, tok):
        self.S.dma("sp", dst, row.partition_broadcast(128), reads=reads, writes=[tok])

    def mod_tiles(self, l, j, idx_scale, idx_shift, gain, tag):
        S, A = self.S, self.A
        tmp = A.f32(D)
        At = A.f32(D)
        SH = A.f32(D)
        self.bc_row(tmp, self.MOD[l, j, idx_scale * D:(idx_scale + 1) * D], [("MOD", l)], ("mtmp", tag))
        self.bc_row(SH, self.MOD[l, j, idx_shift * D:(idx_shift + 1) * D], [("MOD", l)], ("SH", tag))
        S.op("dve", lambda e: e.scalar_tensor_tensor(At, tmp, 1.0, gain, ALU.add, ALU.mult),
             reads=[("mtmp", tag), "gain"], writes=[("A", tag)])
        return At, SH

    def xsrc(self, l, bb, t):
        if l == 0:
            if t < NTL:
                return self.I["x"][bb, t * 128:(t + 1) * 128, :], []
            return self.I["ctx"][bb, (t - NTL) * 128:(t - NTL + 1) * 128, :], []
        return self.XR2[bb, t * 128:(t + 1) * 128, :], [("XR2", bb, t)]

    def rms_mod(self, xt, xtok, At, Atok, SH, SHtok, h_out, h_tok, junk, st, sttok, t1):
        S = self.S
        S.op("act", lambda e: e.activation(junk, xt, AF.Square, accum_out=st[:, 0:1]),
             reads=[xtok], writes=["junk", (sttok, 0)])
        S.op("act", lambda e: e.activation(st[:, 1:2], st[:, 0:1], AF.Sqrt, bias=EPS, scale=1.0 / D),
             reads=[(sttok, 0)], writes=[(sttok, 1)])
        S.op("dve", lambda e: e.reciprocal(st[:, 2:3], st[:, 1:2]), reads=[(sttok, 1)], writes=[(sttok, 2)])
        S.op("dve", lambda e: e.scalar_tensor_tensor(t1, xt, st[:, 2:3], At, ALU.mult, ALU.mult),
             reads=[xtok, (sttok, 2), Atok], writes=["t1"])
        S.op("dve", lambda e: e.tensor_tensor(h_out, t1, SH, ALU.add), reads=["t1", SHtok], writes=[h_tok])

    def pipeline(self, n, order):
        lo = -max(off for _, off in order)
        hi = n - min(off for _, off in order)
        for s_ in range(lo, hi):
            for fn, off in order:
                t = s_ + off
                if 0 <= t < n:
                    fn(t)

    def phaseA(self, l):
        S, A, I = self.S, self.A, self.I
        self.new_phase()
        H, dv = (16, 64) if l == 0 else (8, 128)
        vw = H * (dv + 1)
        pre = "l%d_" % l
        wqkv = A.bf(KC * 3 * D).rearrange("p (k n) -> p k n", k=KC)
        for n in range(6):
            S.dma("pool", wqkv[:, :, n * 512:(n + 1) * 512],
                  I[pre + "w_qkv"][:, n * 512:(n + 1) * 512].rearrange("(k p) n -> p k n", p=128),
                  writes=[("wqkv", n)])
        gain = A.f32(D)
        self.bc_row(gain, I[pre + "norm_mix"], [], "gain")
        Ac, SHc = self.mod_tiles(l, 2, 1, 0, gain, "c")
        lat_mods = [self.mod_tiles(l, bb, 1, 0, gain, "l%d" % bb) for bb in range(NB)]
        if l == 1:
            cs_t = A.f32(NTL * 64).rearrange("p (t j) -> p t j", t=NTL)
            sn_t = A.f32(NTL * 64).rearrange("p (t j) -> p t j", t=NTL)
            S.dma("sp", cs_t, I["rope_cs"], writes=["rope_cs"])
            S.dma("sp", sn_t, I["rope_sn"], writes=["rope_sn"])
            qk32 = A.f32(2 * D)
            r1 = A.f32(2 * D)
            r2 = A.f32(2 * D)
        xts = [A.f32(D) for _ in range(3)]
        junk = A.bf(D)
        stt = A.f32(4)
        t1 = A.f32(D)
        hb = A.bf(D)
        hT = [A.bf(KC * 128).rearrange("p (k n) -> p k n", k=KC) for _ in range(2)]
        qkbs = [A.bf(2 * D) for _ in range(2)]
        qkT = [A.bf(16 * 128).rearrange("p (k n) -> p k n", k=16) for _ in range(2)]
        vaug = [A.bf(vw) for _ in range(2)]
        for i in range(2):
            S.op("pool", lambda e, i=i: e.memset(vaug[i], 1.0), writes=[("vaug", i)])
        pT = self.pbank_bf(0).rearrange("p (k n) -> p k n", k=KC)
        pQT = self.pbank_bf(5, 2).rearrange("p (k n) -> p k n", k=16)
        ntot = NB * NT

        def info(ti):
            bb, t = divmod(ti, NT)
            return bb, t, t < NTL

        def stX(ti):
            bb, t, lat = info(ti)
            src, srd = self.xsrc(l, bb, t)
            S.dma("sp", xts[ti % 3], src, reads=srd, writes=[("xt", ti % 3)])

        def stN(ti):
            bb, t, lat = info(ti)
            Al, SHl = lat_mods[bb]
            tg = "l%d" % bb
            self.rms_mod(xts[ti % 3], ("xt", ti % 3), Al if lat else Ac, ("A", tg if lat else "c"),
                         SHl if lat else SHc, ("SH", tg if lat else "c"), hb, "hb", junk, stt, "stA", t1)

        def stTh(ti):
            b2 = ti % 2
            for k in range(KC):
                S.tr(pT[:, k, :], hb[:, k * 128:(k + 1) * 128], self.ident, reads=["hb", "ident"], writes=["pT"])
            S.op("act", lambda e, b2=b2: e.copy(hT[b2], pT), reads=["pT"], writes=[("hT", b2)])

        def stQ(ti):
            bb, t, lat = info(ti)
            b2 = ti % 2
            qkb = qkbs[b2]
            va = vaug[b2].rearrange("p (h d) -> p h d", h=H)
            for n in range(6):
                pq = self.pbank(1 + n % 4)
                for k in range(KC):
                    S.mm(pq, hT[b2][:, k, :], wqkv[:, k, n * 512:(n + 1) * 512], k == 0, k == KC - 1,
                         reads=[("hT", b2), ("wqkv", n)], writes=[("pq", n % 4)])
                if n < 4:
                    if l == 1:
                        dst, dtok = qk32[:, n * 512:(n + 1) * 512], ("qk32", n)
                    else:
                        dst, dtok = qkb[:, n * 512:(n + 1) * 512], ("qkb", b2)
                    if n < 2:
                        S.op("act", lambda e, dst=dst, pq=pq: e.mul(dst, pq, 0.125),
                             reads=[("pq", n % 4)], writes=[dtok])
                    else:
                        S.op("act", lambda e, dst=dst, pq=pq: e.copy(dst, pq),
                             reads=[("pq", n % 4)], writes=[dtok])
                else:
                    hpb = 512 // dv
                    h0 = (n - 4) * hpb
                    S.op("dve", lambda e, pq=pq, h0=h0, hpb=hpb, va=va: e.tensor_copy(
                        va[:, h0:h0 + hpb, 0:dv], pq.rearrange("p (h d) -> p h d", h=hpb)),
                        reads=[("pq", n % 4)], writes=[("vaug", b2)])
            if l == 1:
                qtoks = [("qk32", n) for n in range(4)]
                if lat:
                    xv = qk32.rearrange("p (g j) -> p g j", g=32)
                    csb = cs_t[:, t, :].unsqueeze(1).broadcast_to([128, 32, 64])
                    S.op("pool", lambda e, xv=xv, csb=csb: e.tensor_tensor(
                        r1.rearrange("p (g j) -> p g j", g=32), xv, csb, ALU.mult),
                        reads=qtoks + ["rope_cs"], writes=["r1"])
                    x5 = qk32.rearrange("p (g a h j) -> p g a h j", g=32, a=2, h=2)
                    o5 = r2.rearrange("p (g a h j) -> p g a h j", g=32, a=2, h=2)
                    s4 = sn_t[:, t, :].rearrange("p (a h j) -> p a h j", a=2, h=2)
                    for hh in range(2):
                        snb = s4[:, :, hh, :].unsqueeze(1).broadcast_to([128, 32, 2, 16])
                        S.op("dve", lambda e, hh=hh, snb=snb: e.tensor_tensor(
                            o5[:, :, :, hh, :], x5[:, :, :, 1 - hh, :], snb, ALU.mult),
                            reads=qtoks + ["rope_sn"], writes=[("r2", hh)])
                    S.op("dve", lambda e, qkb=qkb: e.tensor_tensor(qkb, r1, r2, ALU.add),
                         reads=["r1", ("r2", 0), ("r2", 1)], writes=[("qkb", b2)])
                else:
                    S.op("dve", lambda e, qkb=qkb: e.tensor_copy(qkb, qk32), reads=qtoks, writes=[("qkb", b2)])

        def stTq(ti):
            bb, t, lat = info(ti)
            b2 = ti % 2
            qkb = qkbs[b2]
            for j in range(16):
                S.tr(pQT[:, j, :], qkb[:, j * 128:(j + 1) * 128], self.ident, reads=[("qkb", b2), "ident"],
                     writes=["pQT"])
            S.op("act", lambda e, b2=b2: e.copy(qkT[b2], pQT), reads=["pQT"], writes=[("qkT", b2)])
            S.dma("sp", self.QT[bb, :, :, t * 128:(t + 1) * 128].rearrange("c p n -> p c n"),
                  qkT[b2][:, 0:8, :], reads=[("qkT", b2)], writes=[("QT", bb)])
            S.dma("sp", self.KT[bb, :, :, t * 128:(t + 1) * 128].rearrange("c p n -> p c n"),
                  qkT[b2][:, 8:16, :], reads=[("qkT", b2)], writes=[("KT", bb)])
            S.dma("sp", self.VS[bb, t * 128:(t + 1) * 128, 0:vw], vaug[b2], reads=[("vaug", b2)],
                  writes=[("VS", bb)])

        self.pipeline(ntot, [(stX, 2), (stN, 1), (stQ, 0), (stTh, 1), (stTq, -1)])

    def phaseB0(self):
        S, A, I = self.S, self.A, self.I
        self.new_phase()
        bt32 = A.f32(5 * 2 * 640)
        EB = A.bf(5 * 2 * 640)
        EBv = EB.rearrange("p (v h n) -> p v h n", v=5, h=2)
        QTc = [A.bf(T) for _ in range(2)]
        KTc = [A.bf(T) for _ in range(2)]
        Vc = [A.bf(NT * 130).rearrange("p (t h d) -> p t h d", t=NT, h=2) for _ in range(2)]
        Oc = [A.bf(NT * 128).rearrange("p (t n) -> p t n", t=NT) for _ in range(2)]
        NEB = 4
        E = [A.bf(8 * 128).rearrange("p (j n) -> p j n", j=8) for _ in range(NEB)]
        rec = A.f32(8)
        NPS = B0_NPS
        pS = [self.psum[:, 2 * i:2 * i + 2, :].rearrange("p a (j n) -> p (a j) n", n=128) for i in range(NPS)]
        NPO = 8 - 2 * NPS
        pO = self.psum[:, 2 * NPS:8, 0:128]
        var_of = {0: 0, 1: 1, 14: 3, 15: 4}
        it = 0
        qi = 0
        for c in range(KC):
            btv = bt32.rearrange("p (v h n) -> p v h n", v=5, h=2)
            for v5 in range(5):
                S.dma("sp", btv[:, v5], I["na_bias"][v5, 2 * c:2 * c + 2].rearrange("h p n -> p h n"),
                      writes=["bt32"])
            S.op("act", lambda e: e.activation(EB, bt32, AF.Exp), reads=["bt32"], writes=["EB"])
            for bb in range(NB):
                b2 = it % 2
                it += 1
                S.dma("sp", QTc[b2], self.QT[bb, c], writes=[("QTc", b2)])
                S.dma("sp", KTc[b2], self.KT[bb, c], writes=[("KTc", b2)])
                S.dma("sp", Vc[b2].rearrange("p t h d -> p t (h d)"),
                      self.VS[bb, :, 2 * c * 65:(2 * c + 2) * 65].rearrange("(t p) w -> p t w", p=128),
                      writes=[("Vc", b2)])
                LA = B0_LA
                items = [(hh, i) for hh in range(2) for i in range(NT)]
                meta = {}

                def stage1(hh, i, b2=b2):
                    nonlocal qi
                    pr = slice(64 * hh, 64 * hh + 64)
                    if i < NTL:
                        j0 = min(max(i - 2, 0), 11)
                        kts = [j0 + jj for jj in range(5)] + [16, 17]
                        v = var_of.get(i, 2)
                    else:
                        kts = [16, 17]
                        v = None
                    nk = len(kts)
                    sb = qi % NPS
                    eb = qi % NEB
                    slot = qi % NPO
                    qi += 1
                    meta[(hh, i)] = (kts, eb, slot)
                    for jj, kt in enumerate(kts):
                        S.mm(pS[sb][:, jj, :], KTc[b2][pr, kt * 128:(kt + 1) * 128],
                             QTc[b2][pr, i * 128:(i + 1) * 128], True, True,
                             reads=[("KTc", b2), ("QTc", b2)], writes=[("pS", sb)])
                    if nk > 4:
                        S.op("act", lambda e, sb=sb, eb=eb: e.activation(E[eb][:, 0:4, :], pS[sb][:, 0:4, :], AF.Exp),
                             reads=[("pS", sb)], writes=[("E", eb)])
                        S.op("act", lambda e, sb=sb, eb=eb, nk=nk: e.activation(E[eb][:, 4:nk, :], pS[sb][:, 4:nk, :], AF.Exp),
                             reads=[("pS", sb)], writes=[("E", eb)])
                    else:
                        S.op("act", lambda e, sb=sb, eb=eb, nk=nk: e.activation(E[eb][:, 0:nk, :], pS[sb][:, 0:nk, :], AF.Exp),
                             reads=[("pS", sb)], writes=[("E", eb)])
                    if v is not None:
                        S.op("dve", lambda e, eb=eb, v=v, hh=hh: e.tensor_tensor(
                            E[eb][:, 0:5, :], E[eb][:, 0:5, :],
                            EBv[:, v, hh, :].rearrange("p (j n) -> p j n", j=5), ALU.mult),
                            reads=[("E", eb), "EB"], writes=[("E", eb)])

                def stage2(hh, i, b2=b2):
                    kts, eb, slot = meta[(hh, i)]
                    nk = len(kts)
                    for jj, kt in enumerate(kts):
                        S.mm(pO[:, slot, 0:65], E[eb][:, jj, :], Vc[b2][:, kt, hh, :], jj == 0, jj == nk - 1,
                             reads=[("E", eb), ("Vc", b2)], writes=[("pO", slot)])
                    S.op("dve", lambda e, slot=slot: e.reciprocal(rec[:, slot:slot + 1], pO[:, slot, 64:65]),
                         reads=[("pO", slot)], writes=[("rec", slot)])
                    S.op("dve", lambda e, slot=slot, i=i, hh=hh, b2=b2: e.tensor_scalar(
                        Oc[b2][:, i, 64 * hh:64 * hh + 64], pO[:, slot, 0:64], rec[:, slot:slot + 1], None, ALU.mult),
                        reads=[("pO", slot), ("rec", slot)], writes=[("Oc", b2)])

                for idx in range(len(items) + LA):
                    if idx < len(items):
                        stage1(*items[idx])
                    if idx >= LA:
                        stage2(*items[idx - LA])
                S.dma("sp", self.OS[bb, :, c * 128:(c + 1) * 128].rearrange("(t p) n -> p t n", p=128),
                      Oc[b2], reads=[("Oc", b2)], writes=[("OS", bb)])

    def phaseC(self, l):
        S, A, I = self.S, self.A, self.I
        self.new_phase()
        pre = "l%d_" % l
        ntile = NT if l == 0 else NTL
        Wo = A.bf(KC * D).rearrange("p (k n) -> p k n", k=KC)
        for h2_ in range(2):
            S.dma("pool", Wo[:, :, h2_ * 512:(h2_ + 1) * 512],
                  I[pre + "w_o"][:, h2_ * 512:(h2_ + 1) * 512].rearrange("(k p) n -> p k n", p=128), writes=["Wo"])
        WoL = A.bf(KC * D).rearrange("p (k n) -> p k n", k=KC)
        gtmp = A.f32(D)
        gain = A.f32(D)
        self.bc_row(gain, I[pre + "norm_ffn"], [], "gain")
        if l == 0:
            WoC = A.bf(KC * D).rearrange("p (k n) -> p k n", k=KC)
            self.bc_row(gtmp, self.MOD[l, 2, 2 * D:3 * D], [], "gtmp")
            for k in range(KC):
                S.op("dve", lambda e, k=k: e.tensor_tensor(WoC[:, k, :], Wo[:, k, :], gtmp, ALU.mult),
                     reads=["Wo", "gtmp"], writes=["WoC"])
            A2c, SH2c = self.mod_tiles(l, 2, 4, 3, gain, "c")
        else:
            Wr = A.f32(KC * NE).rearrange("p (k n) -> p k n", k=KC)
            S.dma("sp", Wr, I["l1_w_router"].rearrange("(k p) n -> p k n", p=128), writes=["Wr"])
            h2f = A.f32(D)
            h2fT = A.f32(KC * 128).rearrange("p (k n) -> p k n", k=KC)
            lg = A.f32(8 * NE)
            sm = A.f32(8)
        Ots = [A.bf(D) for _ in range(3)]
        xts = [A.f32(D) for _ in range(3)]
        OT = [A.bf(KC * 128).rearrange("p (k n) -> p k n", k=KC) for _ in range(2)]
        x1s = [A.f32(D) for _ in range(3)]
        junk = A.bf(D)
        stt = A.f32(4)
        t1 = A.f32(D)
        h2s = [A.bf(D) for _ in range(2)]
        if l == 1:
            h2fs = [A.f32(D) for _ in range(2)]
        h2T = [A.bf(KC * 128).rearrange("p (k n) -> p k n", k=KC) for _ in range(2)]
        pT = self.pbank_bf(0).rearrange("p (k n) -> p k n", k=KC)
        pY = [self.pbank(1), self.pbank(2)]
        pT2 = self.pbank_bf(3).rearrange("p (k n) -> p k n", k=KC)
        pF = self.psum[:, 4:6, :].rearrange("p a (j n) -> p (a j) n", n=128)
        pL = self.psum[:, 6, 0:NE]
        WoLs = [WoL, A.bf(KC * D).rearrange("p (k n) -> p k n", k=KC)]
        mods = []
        for bb in range(NB):
            self.bc_row(gtmp, self.MOD[l, bb, 2 * D:3 * D], [], "gtmp")
            for k in range(KC):
                S.op("dve", lambda e, k=k, bb=bb: e.tensor_tensor(WoLs[bb][:, k, :], Wo[:, k, :], gtmp, ALU.mult),
                     reads=["Wo", "gtmp"], writes=[("WoL", bb)])
            mods.append(self.mod_tiles(l, bb, 4, 3, gain, "l%d" % bb))
        ntot = NB * ntile

        def info(ti):
            bb, t = divmod(ti, ntile)
            return bb, t, t < NTL

        def stL(ti):
            bb, t, lat = info(ti)
            S.dma("sp", Ots[ti % 3], self.OS[bb, t * 128:(t + 1) * 128, :], writes=[("Ot", ti % 3)])
            src, srd = self.xsrc(l, bb, t)
            S.dma("sp", xts[ti % 3], src, reads=srd, writes=[("xt", ti % 3)])

        def stTo(ti):
            b2 = ti % 2
            for k in range(KC):
                S.tr(pT[:, k, :], Ots[ti % 3][:, k * 128:(k + 1) * 128], self.ident, reads=[("Ot", ti % 3), "ident"],
                     writes=["pT"])
            S.op("act", lambda e, b2=b2: e.copy(OT[b2], pT), reads=["pT"], writes=[("OT", b2)])

        def stY(ti):
            bb, t, lat = info(ti)
            b2 = ti % 2
            b3 = ti % 3
            W = WoLs[bb] if lat else WoC
            wtok = ("WoL", bb) if lat else "WoC"
            for hf in range(2):
                for k in range(KC):
                    S.mm(pY[hf], OT[b2][:, k, :], W[:, k, hf * 512:(hf + 1) * 512], k == 0, k == KC - 1,
                         reads=[("OT", b2), wtok], writes=[("pY", hf)])
                S.op("dve", lambda e, hf=hf, b3=b3: e.tensor_tensor(
                    x1s[b3][:, hf * 512:(hf + 1) * 512], pY[hf], xts[b3][:, hf * 512:(hf + 1) * 512], ALU.add),
                    reads=[("pY", hf), ("xt", b3)], writes=[("x1", b3)])
            S.dma("sp", self.XR1[bb, t * 128:(t + 1) * 128, :], x1s[b3], reads=[("x1", b3)],
                  writes=[("XR1", bb, t)])

        def stN(ti):
            bb, t, lat = info(ti)
            b2 = ti % 2
            b3 = ti % 3
            A2l, SH2l = mods[bb]
            tg = "l%d" % bb
            if l == 0:
                self.rms_mod(x1s[b3], ("x1", b3), A2l if lat else A2c, ("A", tg if lat else "c"),
                             SH2l if lat else SH2c, ("SH", tg if lat else "c"), h2s[b2], ("h2", b2), junk, stt, "stC", t1)
            else:
                self.rms_mod(x1s[b3], ("x1", b3), A2l, ("A", tg), SH2l, ("SH", tg), h2fs[b2], ("h2f", b2), junk, stt,
                             "stC", t1)
                S.op("pool", lambda e, b2=b2: e.tensor_copy(h2s[b2], h2fs[b2]), reads=[("h2f", b2)], writes=[("h2", b2)])

        def stTh(ti):
            bb, t, lat = info(ti)
            b2 = ti % 2
            h2 = h2s[b2]
            for k in range(KC):
                S.tr(pT2[:, k, :], h2[:, k * 128:(k + 1) * 128], self.ident, reads=[("h2", b2), "ident"], writes=["pT2"])
            S.op("act", lambda e, b2=b2: e.copy(h2T[b2], pT2), reads=["pT2"], writes=[("h2T", b2)])
            col = bb * T + t * 128
            S.dma("sp", self.H2T[:, :, col:col + 128].rearrange("k p n -> p k n"), h2T[b2],
                  reads=[("h2T", b2)], writes=[("H2T", bb, t)])
            if l == 1:
                h2f = h2fs[b2]
                for k in range(KC):
                    S.tr(pF[:, k, :], h2f[:, k * 128:(k + 1) * 128], self.ident_f, reads=[("h2f", b2), "ident_f"],
                         writes=["pF"])
                S.op("dve", lambda e: e.tensor_copy(h2fT, pF), reads=["pF"], writes=["h2fT"])
                for k in range(KC):
                    S.mm(pL, h2fT[:, k, :], Wr[:, k, :], k == 0, k == KC - 1, reads=["h2fT", "Wr"], writes=["pL"])
                L = lambda i: lg[:, i * NE:(i + 1) * NE]
                V = lambda i: sm[:, i:i + 1]
                S.op("dve", lambda e: e.tensor_copy(L(0), pL), reads=["pL"], writes=["g0"])
                S.op("dve", lambda e: e.reduce_max(V(0), L(0), AX.X), reads=["g0"], writes=["m1"])
                S.op("dve", lambda e: e.tensor_scalar(L(1), L(0), V(0), None, ALU.is_equal),
                     reads=["g0", "m1"], writes=["g1"])
                S.op("dve", lambda e: e.scalar_tensor_tensor(L(2), L(1), -1e30, L(0), ALU.mult, ALU.add),
                     reads=["g0", "g1"], writes=["g2"])
                S.op("dve", lambda e: e.reduce_max(V(1), L(2), AX.X), reads=["g2"], writes=["m2"])
                S.op("dve", lambda e: e.tensor_scalar(L(3), L(0), V(1), None, ALU.is_ge),
                     reads=["g0", "m2"], writes=["g3"])
                S.op("dve", lambda e: e.tensor_scalar(V(2), V(0), -1.0, None, ALU.mult), reads=["m1"], writes=["nm1"])
                S.op("act", lambda e: e.activation(L(4), L(0), AF.Exp, bias=V(2), scale=1.0),
                     reads=["g0", "nm1"], writes=["g4"])
                S.op("dve", lambda e: e.tensor_tensor(L(5), L(4), L(3), ALU.mult), reads=["g4", "g3"], writes=["g5"])
                S.op("dve", lambda e: e.reduce_sum(V(3), L(5), AX.X), reads=["g5"], writes=["ssum"])
                S.op("dve", lambda e: e.reciprocal(V(4), V(3)), reads=["ssum"], writes=["rsum"])
                S.op("dve", lambda e: e.tensor_scalar(L(6), L(5), V(4), None, ALU.mult), reads=["g5", "rsum"],
                     writes=["g6"])
                S.dma("sp", self.GAT[bb, t * 128:(t + 1) * 128, :], L(6), reads=["g6"], writes=[("GAT", bb, t)])

        self.pipeline(ntot, [(stL, 2), (stN, -1), (stY, 0), (stTo, 1), (stTh, -2)])

    def phaseD0(self):
        S, A, I = self.S, self.A, self.I
        self.new_phase()
        NCH = DFF // 128
        Wg = A.bf(KC * DFF).rearrange("p (k n) -> p k n", k=KC)
        Wu = A.bf(KC * DFF).rearrange("p (k n) -> p k n", k=KC)
        Wd = A.bf(NCH * D).rearrange("p (c n) -> p c n", c=NCH)
        for n0 in range(0, DFF, 512):
            n1 = min(n0 + 512, DFF)
            for W, nm in ((Wg, "l0_w_gate"), (Wu, "l0_w_up")):
                S.dma("pool", W[:, :, n0:n1], I[nm][:, n0:n1].rearrange("(k p) n -> p k n", p=128),
                      writes=[(nm, n0)])
        wd_src = I["l0_w_down"].rearrange("(c p) n -> p c n", p=128)
        for c0 in range(0, NCH, 4):
            c1 = min(c0 + 4, NCH)
            S.dma("pool", Wd[:, c0:c1, :], wd_src[:, c0:c1, :], writes=[("wd", c0)])
        wg_toks = [("l0_w_gate", n0) for n0 in range(0, DFF, 512)]
        wu_toks = [("l0_w_up", n0) for n0 in range(0, DFF, 512)]
        wd_toks = [("wd", c0) for c0 in range(0, NCH, 4)]
        G2c = A.f32(D)
        G2l = A.f32(D)
        self.bc_row(G2c, self.MOD[0, 2, 5 * D:6 * D], [], "G2c")
        hT = [A.bf(KC * 512).rearrange("p (k n) -> p k n", k=KC) for _ in range(2)]
        AT = A.bf(NCH * 512).rearrange("p (c n) -> p c n", c=NCH)
        sg = [A.bf(512) for _ in range(2)]
        xts = [A.f32(D) for _ in range(2)]
        x2s = [A.f32(D) for _ in range(2)]
        tmp = A.f32(512)
        pG = [self.pbank(0), self.pbank(1)]
        pU = [self.pbank(2), self.pbank(3)]
        pY = [self.pbank(4 + i) for i in range(4)]
        ngroups = NB * NT // 4
        ti = 0
        yi = 0
        cur_bb = -1
        for g in range(ngroups):
            hb = g % 2
            S.dma("sp", hT[hb], self.H2T[:, :, g * 512:(g + 1) * 512].rearrange("k p n -> p k n"),
                  writes=[("hT", hb)])
            for ci in range(NCH):
                pb = ci % 2
                for k in range(KC):
                    S.mm(pG[pb], Wg[:, k, ci * 128:(ci + 1) * 128], hT[hb][:, k, :], k == 0, k == KC - 1,
                         reads=[("hT", hb), wg_toks[ci // 4]], writes=[("pG", pb)])
                for k in range(KC):
                    S.mm(pU[pb], Wu[:, k, ci * 128:(ci + 1) * 128], hT[hb][:, k, :], k == 0, k == KC - 1,
                         reads=[("hT", hb), wu_toks[ci // 4]], writes=[("pU", pb)])
                S.op("act", lambda e, pb=pb: e.activation(sg[pb], pG[pb], AF.Silu), reads=[("pG", pb)],
                     writes=[("sg", pb)])
                S.op("dve", lambda e, pb=pb, ci=ci: e.tensor_tensor(AT[:, ci, :], sg[pb], pU[pb], ALU.mult),
                     reads=[("sg", pb), ("pU", pb)], writes=[("AT", ci)])
            at_toks = [("AT", ci) for ci in range(NCH)]
            for j in range(4):
                bb, t = divmod(4 * g + j, NT)
                lat = t < NTL
                if lat and bb != cur_bb:
                    cur_bb = bb
                    self.bc_row(G2l, self.MOD[0, bb, 5 * D:6 * D], [], "G2l")
                b2 = ti % 2
                ti += 1
                S.dma("sp", xts[b2], self.XR1[bb, t * 128:(t + 1) * 128, :], writes=[("xt", b2)])
                G2, gtok = (G2l, "G2l") if lat else (G2c, "G2c")
                for hf in range(2):
                    yb = yi % 4
                    yi += 1
                    for ci in range(NCH):
                        S.mm(pY[yb], AT[:, ci, j * 128:(j + 1) * 128], Wd[:, ci, hf * 512:(hf + 1) * 512],
                             ci == 0, ci == NCH - 1, reads=[("AT", ci), wd_toks[ci // 4]], writes=[("pY", yb)])
                    S.op("dve", lambda e, yb=yb, hf=hf, G2=G2: e.tensor_tensor(
                        tmp, pY[yb], G2[:, hf * 512:(hf + 1) * 512], ALU.mult),
                        reads=[("pY", yb), gtok], writes=["tmp"])
                    S.op("dve", lambda e, hf=hf, b2=b2: e.tensor_tensor(
                        x2s[b2][:, hf * 512:(hf + 1) * 512], tmp, xts[b2][:, hf * 512:(hf + 1) * 512], ALU.add),
                        reads=["tmp", ("xt", b2)], writes=[("x2", b2)])
                S.dma("sp", self.XR2[bb, t * 128:(t + 1) * 128, :], x2s[b2], reads=[("x2", b2)],
                      writes=[("XR2", bb, t)])

    def phaseB1(self):
        S, A, I = self.S, self.A, self.I
        self.new_phase()
        lam_init = 0.8 - 0.6 * math.exp(-0.3 * 1)
        lv = A.f32(4 * 64).rearrange("p (a n) -> p a n", a=4)
        for a, nm in enumerate(("q1", "k1", "q2", "k2")):
            self.bc_row(lv[:, a, :], I["l1_lambda_" + nm], [], ("lv", a))
        lp = A.f32(2 * 64).rearrange("p (a n) -> p a n", a=2)
        ls = A.f32(8)
        for a in range(2):
            S.op("dve", lambda e, a=a: e.tensor_tensor(lp[:, a, :], lv[:, 2 * a, :], lv[:, 2 * a + 1, :], ALU.mult),
                 reads=[("lv", 2 * a), ("lv", 2 * a + 1)], writes=[("lp", a)])
            S.op("dve", lambda e, a=a: e.reduce_sum(ls[:, a:a + 1], lp[:, a, :], AX.X), reads=[("lp", a)],
                 writes=[("ls", a)])
        S.op("act", lambda e: e.activation(ls[:, 2:4], ls[:, 0:2], AF.Exp), reads=[("ls", 0), ("ls", 1)],
             writes=["lexp"])
        S.op("dve", lambda e: e.tensor_tensor(ls[:, 4:5], ls[:, 3:4], ls[:, 2:3], ALU.subtract), reads=["lexp"],
             writes=["ldiff"])
        S.op("dve", lambda e: e.tensor_scalar(ls[:, 5:6], ls[:, 4:5], -lam_init, None, ALU.add), reads=["ldiff"],
             writes=["nlam"])
        nlam = ls[:, 5:6]
        SUB = A.f32(128)
        self.bc_row(SUB, I["l1_subln"], [], "SUB0")
        S.op("dve", lambda e: e.tensor_scalar(SUB, SUB, 1.0 - lam_init, None, ALU.mult), reads=["SUB0"], writes=["SUB"])
        QTh = [A.bf(SEQ) for _ in range(2)]
        KTh = [A.bf(T) for _ in range(2)]
        Vh = [A.bf(NT * 129).rearrange("p (t d) -> p t d", t=NT) for _ in range(2)]
        NEB = 4
        E = [A.bf(512) for _ in range(NEB)]
        accS = [A.f32(8 * 129) for _ in range(2)]
        ob = A.f32(4 * 128).rearrange("p (q n) -> p q n", q=4)
        o1 = A.f32(128)
        sq = A.f32(128)
        rr = A.f32(16)
        Oc = [A.bf(NTL * 128).rearrange("p (t n) -> p t n", t=NTL) for _ in range(2)]
        pS = [self.pbank(i) for i in range(4)]

        def acc(s_):
            b = 4 + s_ // 3
            off = (s_ % 3) * 129
            return self.psum[:, b, off:off + 129]

        it = 0
        si = 0
        ci = 0
        for bb in range(NB):
            for h in range(KC):
                b2 = it % 2
                it += 1
                S.dma("sp", QTh[b2], self.QT[bb, h, :, 0:SEQ], writes=[("QTh", b2)])
                S.dma("sp", KTh[b2], self.KT[bb, h], writes=[("KTh", b2)])
                S.dma("sp", Vh[b2], self.VS[bb, :, h * 129:(h + 1) * 129].rearrange("(t p) w -> p t w", p=128),
                      writes=[("Vh", b2)])
                LA = 2
                steps = [(qc, kt, m) for qc in range(4) for kt in range(NT) for m in range(2)]
                meta = {}

                def st1(qc, kt, m, b2=b2):
                    nonlocal si
                    sb = si % 4
                    eb = si % NEB
                    si += 1
                    meta[(qc, kt, m)] = eb
                    pr = slice(64 * m, 64 * m + 64)
                    S.mm(pS[sb], KTh[b2][pr, kt * 128:(kt + 1) * 128], QTh[b2][pr, qc * 512:(qc + 1) * 512],
                         True, True, reads=[("KTh", b2), ("QTh", b2)], writes=[("pS", sb)])
                    S.op("act", lambda e, sb=sb, eb=eb: e.activation(E[eb], pS[sb], AF.Exp),
                         reads=[("pS", sb)], writes=[("E", eb)])

                def st2(qc, kt, m, b2=b2):
                    eb = meta[(qc, kt, m)]
                    for qt in range(4):
                        sl = m * 4 + qt
                        S.mm(acc(sl), E[eb][:, qt * 128:(qt + 1) * 128], Vh[b2][:, kt, :],
                             kt == 0 and sl % 3 == 0, kt == NT - 1, reads=[("E", eb), ("Vh", b2)],
                             writes=[("acc", sl)], skip_group_check=True)
                    if kt == NT - 1 and m == 1:
                        epilogue(qc, b2)

                def epilogue(qc, b2):
                    nonlocal ci
                    ab = ci % 2
                    ci += 1
                    aS = accS[ab]
                    for bk in range(3):
                        n = 387 if bk < 2 else 258
                        S.op("dve", lambda e, bk=bk, n=n, aS=aS: e.tensor_copy(aS[:, bk * 387:bk * 387 + n],
                                                                             self.psum[:, 4 + bk, 0:n]),
                             reads=[("acc", 3 * bk + j) for j in range(3) if 3 * bk + j < 8], writes=[("accS", ab, bk)])
                    atoks = [("accS", ab, bk) for bk in range(3)]
                    a3 = aS.rearrange("p (s n) -> p s n", n=129)
                    S.op("dve", lambda e, a3=a3: e.reciprocal(rr[:, 0:8], a3[:, :, 128]), reads=atoks, writes=["rr"])
                    S.op("dve", lambda e: e.tensor_scalar(rr[:, 8:12], rr[:, 4:8], nlam, None, ALU.mult),
                         reads=["rr", "nlam"], writes=["rr2"])
                    for qt in range(4):
                        S.op("dve", lambda e, qt=qt, a3=a3: e.tensor_scalar(o1, a3[:, qt, 0:128], rr[:, qt:qt + 1], None,
                                                                        ALU.mult), reads=atoks + ["rr"], writes=["o1"])
                        S.op("dve", lambda e, qt=qt, a3=a3: e.scalar_tensor_tensor(
                            ob[:, qt, :], a3[:, 4 + qt, 0:128], rr[:, 8 + qt:9 + qt], o1, ALU.mult, ALU.add),
                            reads=atoks + ["rr2", "o1"], writes=[("ob", qt)])
                        S.op("dve", lambda e, qt=qt: e.tensor_tensor(sq, ob[:, qt, :], ob[:, qt, :], ALU.mult),
                             reads=[("ob", qt)], writes=["sq"])
                        S.op("dve", lambda e, qt=qt: e.reduce_sum(rr[:, 12 + qt:13 + qt], sq, AX.X), reads=["sq"],
                             writes=[("ss", qt)])
                    sst = [("ss", qt) for qt in range(4)]
                    S.op("act", lambda e: e.activation(rr[:, 12:16], rr[:, 12:16], AF.Ln, bias=EPS, scale=1.0 / 128),
                         reads=sst, writes=["lnv"])
                    S.op("act", lambda e: e.activation(rr[:, 12:16], rr[:, 12:16], AF.Exp, scale=-0.5), reads=["lnv"],
                         writes=["rstd"])
                    for qt in range(4):
                        S.op("dve", lambda e, qt=qt, b2=b2, qc=qc: e.scalar_tensor_tensor(
                            Oc[b2][:, qc * 4 + qt, :], ob[:, qt, :], rr[:, 12 + qt:13 + qt], SUB, ALU.mult, ALU.mult),
                            reads=[("ob", qt), "rstd", "SUB"], writes=[("Oc", b2)])

                for idx in range(len(steps) + LA):
                    if idx < len(steps):
                        st1(*steps[idx])
                    if idx >= LA:
                        st2(*steps[idx - LA])
                S.dma("sp", self.OS[bb, 0:SEQ, h * 128:(h + 1) * 128].rearrange("(t p) n -> p t n", p=128),
                      Oc[b2], reads=[("Oc", b2)], writes=[("OS", bb)])

    def phaseD1(self):
        S, A, I = self.S, self.A, self.I
        self.new_phase()
        NG = DFE // 512
        hT = A.bf(KC * SEQ).rearrange("p (k n) -> p k n", k=KC)
        Y = A.f32(NTL * D).rearrange("p (t n) -> p t n", t=NTL)
        gat = A.f32(NTL * NE).rearrange("p (t e) -> p t e", t=NTL)
        wg = [A.bf(KC * 512).rearrange("p (k n) -> p k n", k=KC) for _ in range(2)]
        wu = [A.bf(KC * 512).rearrange("p (k n) -> p k n", k=KC) for _ in range(2)]
        wd = [A.bf(4 * D).rearrange("p (c n) -> p c n", c=4) for _ in range(2)]
        AT = [A.bf(4 * 512).rearrange("p (c n) -> p c n", c=4) for _ in range(2)]
        sg = [A.bf(512) for _ in range(2)]
        G2l = A.f32(D)
        fin = A.f32(D)
        self.bc_row(fin, I["final_norm"], [], "fin")
        xts = [A.f32(D) for _ in range(2)]
        x2s = [A.f32(D) for _ in range(2)]
        outs = [A.f32(D) for _ in range(2)]
        junk = A.bf(D)
        stt = A.f32(4)
        pG = [self.pbank(0), self.pbank(1)]
        pU = [self.pbank(2), self.pbank(3)]
        pY = [self.pbank(4 + i) for i in range(4)]
        wi = 0
        ai = 0
        yi = 0
        pi = 0
        ti = 0
        for bb in range(NB):
            S.dma("sp", hT, self.H2T[:, :, bb * T:bb * T + SEQ].rearrange("k p n -> p k n"), writes=["hT"])
            S.dma("sp", gat, self.GAT[bb].rearrange("(t p) e -> p t e", p=128), writes=["gat"])
            self.bc_row(G2l, self.MOD[1, bb, 5 * D:6 * D], [], "G2l")
            for ex in range(NE):
                for g in range(NG):
                    wb = wi % 2
                    wi += 1
                    S.dma("pool", wg[wb], I["l1_w_gate"][ex][:, g * 512:(g + 1) * 512].rearrange("(k p) n -> p k n", p=128),
                          writes=[("wg", wb)])
                    S.dma("pool", wu[wb], I["l1_w_up"][ex][:, g * 512:(g + 1) * 512].rearrange("(k p) n -> p k n", p=128),
                          writes=[("wu", wb)])
                    S.dma("pool", wd[wb], I["l1_w_down"][ex][g * 512:(g + 1) * 512, :].rearrange("(c p) n -> p c n", p=128),
                          writes=[("wd", wb)])
                    first = (ex == 0 and g == 0)
                    for tg in range(4):
                        ab = ai % 2
                        ai += 1
                        for ci in range(4):
                            pb = pi % 2
                            pi += 1
                            for k in range(KC):
                                S.mm(pG[pb], wg[wb][:, k, ci * 128:(ci + 1) * 128], hT[:, k, tg * 512:(tg + 1) * 512],
                                     k == 0, k == KC - 1, reads=["hT", ("wg", wb)], writes=[("pG", pb)])
                            for k in range(KC):
                                S.mm(pU[pb], wu[wb][:, k, ci * 128:(ci + 1) * 128], hT[:, k, tg * 512:(tg + 1) * 512],
                                     k == 0, k == KC - 1, reads=["hT", ("wu", wb)], writes=[("pU", pb)])
                            S.op("act", lambda e, pb=pb: e.activation(sg[pb], pG[pb], AF.Silu), reads=[("pG", pb)],
                                 writes=[("sg", pb)])
                            S.op("dve", lambda e, pb=pb, ci=ci, ab=ab: e.tensor_tensor(AT[ab][:, ci, :], sg[pb], pU[pb],
                                                                                   ALU.mult),
                                 reads=[("sg", pb), ("pU", pb)], writes=[("AT", ab)])
                        for j in range(4):
                            tile = tg * 4 + j
                            for hf in range(2):
                                yb = yi % 4
                                yi += 1
                                for ci in range(4):
                                    S.mm(pY[yb], AT[ab][:, ci, j * 128:(j + 1) * 128], wd[wb][:, ci, hf * 512:(hf + 1) * 512],
                                         ci == 0, ci == 3, reads=[("AT", ab), ("wd", wb)], writes=[("pY", yb)])
                                ysl = Y[:, tile, hf * 512:(hf + 1) * 512]
                                gsc = gat[:, tile, ex:ex + 1]
                                if first:
                                    S.op("dve", lambda e, ysl=ysl, gsc=gsc, yb=yb: e.tensor_scalar(ysl, pY[yb], gsc, None,
                                                                                                  ALU.mult),
                                         reads=[("pY", yb), "gat"], writes=[("Y", tile, hf)])
                                else:
                                    S.op("dve", lambda e, ysl=ysl, gsc=gsc, yb=yb: e.scalar_tensor_tensor(
                                        ysl, pY[yb], gsc, ysl, ALU.mult, ALU.add),
                                        reads=[("pY", yb), "gat", ("Y", tile, hf)], writes=[("Y", tile, hf)])
            for tile in range(NTL):
                b2 = ti % 2
                ti += 1
                S.dma("sp", xts[b2], self.XR1[bb, tile * 128:(tile + 1) * 128, :], writes=[("xt", b2)])
                S.op("dve", lambda e, tile=tile, b2=b2: e.tensor_tensor(x2s[b2], Y[:, tile, :], G2l, ALU.mult),
                     reads=[("Y", tile, 0), ("Y", tile, 1), "G2l"], writes=[("x2a", b2)])
                S.op("dve", lambda e, b2=b2: e.tensor_tensor(x2s[b2], x2s[b2], xts[b2], ALU.add),
                     reads=[("x2a", b2), ("xt", b2)], writes=[("x2", b2)])
                if self.dbg:
                    S.dma("sp", self.XR2[bb, tile * 128:(tile + 1) * 128, :], x2s[b2], reads=[("x2", b2)],
                          writes=[("XR2", bb, tile)])
                S.op("act", lambda e, b2=b2: e.activation(junk, x2s[b2], AF.Square, accum_out=stt[:, 0:1]),
                     reads=[("x2", b2)], writes=["junk", "ssq"])
                S.op("act", lambda e: e.activation(stt[:, 1:2], stt[:, 0:1], AF.Sqrt, bias=EPS, scale=1.0 / D),
                     reads=["ssq"], writes=["std"])
                S.op("dve", lambda e: e.reciprocal(stt[:, 2:3], stt[:, 1:2]), reads=["std"], writes=["rstd"])
                S.op("dve", lambda e, b2=b2: e.scalar_tensor_tensor(outs[b2], x2s[b2], stt[:, 2:3], fin, ALU.mult, ALU.mult),
                     reads=[("x2", b2), "rstd", "fin"], writes=[("out", b2)])
                S.dma("sp", self.out[bb, tile * 128:(tile + 1) * 128, :], outs[b2], reads=[("out", b2)],
                      writes=[("OUT", bb, tile)])


def _const_tables():
    ident = np.eye(128, dtype=np.float32)
    pos = np.arange(SEQ)
    row = (pos // GRID_W).astype(np.float64)
    col = (pos % GRID_W).astype(np.float64)
    inv = 10000.0 ** (-np.arange(0, 32, 2, dtype=np.float64) / 32.0)
    ar = row[:, None] * inv[None]
    ac = col[:, None] * inv[None]
    cs = np.concatenate([np.cos(ar), np.cos(ar), np.cos(ac), np.cos(ac)], -1)
    sn = np.concatenate([-np.sin(ar), np.sin(ar), -np.sin(ac), np.sin(ac)], -1)
    cs = cs.reshape(NTL, 128, 64).transpose(1, 0, 2).astype(np.float32)
    sn = sn.reshape(NTL, 128, 64).transpose(1, 0, 2).astype(np.float32)
    return ident, np.ascontiguousarray(cs), np.ascontiguousarray(sn)


NA_VAR_TILES = (0, 1, 5, 14, 15)


def _na_index():
    kp = np.arange(128)
    kr, kc = kp // 64, kp % 64
    qp = np.arange(128)
    qr, qc = qp // 64, qp % 64
    dr = np.zeros((5, 128, 5, 128), np.int64)
    dc = np.zeros((5, 128, 5, 128), np.int64)
    ok = np.zeros((5, 128, 5, 128), bool)
    for v, i in enumerate(NA_VAR_TILES):
        j0 = min(max(i - 2, 0), 11)
        for jj in range(5):
            rk = 2 * (j0 + jj) + kr
            r = 2 * i + qr
            rs = np.clip(r - 4, 0, 24)
            vrow = (rk[:, None] >= rs[None, :]) & (rk[:, None] < rs[None, :] + 8)
            ws = np.clip(qc - 8, 0, 48)
            vcol = (kc[:, None] >= ws[None, :]) & (kc[:, None] < ws[None, :] + 16)
            val = vrow & vcol
            ok[v, :, jj, :] = val
            dr[v, :, jj, :] = np.where(val, rk[:, None] - r[None, :] + 7, 0)
            dc[v, :, jj, :] = np.where(val, kc[:, None] - qc[None, :] + 15, 0)
    return dr, dc, ok


def _na_bias(rpb):
    dr, dc, ok = _na_index()
    tab = rpb[:, dr, dc]
    tab = np.where(ok[None], tab, np.float32(-30000.0))
    tab = tab.transpose(1, 0, 2, 3, 4).reshape(5, 16, 128, 640)
    return np.ascontiguousarray(tab.astype(np.float32))


def make_in_maps(inputs, n_cores=8):
    ident, cs, sn = _const_tables()
    nab = _na_bias(np.asarray(inputs["l0_rpb"], np.float32))
    shared = {}
    for k, v in inputs.items():
        if k in ("x", "c", "ctx", "l0_rpb"):
            continue
        shared[k] = np.ascontiguousarray(np.asarray(v, np.float32))
    shared["ident"] = ident
    shared["rope_cs"] = cs
    shared["rope_sn"] = sn
    shared["na_bias"] = nab
    maps = []
    for c in range(n_cores):
        m = dict(shared)
        m["x"] = np.ascontiguousarray(inputs["x"][c * NB:(c + 1) * NB])
        m["ctx"] = np.ascontiguousarray(inputs["ctx"][c * NB:(c + 1) * NB])
        m["c"] = np.ascontiguousarray(inputs["c"][c * NB:(c + 1) * NB])
        maps.append(m)
    return maps


_NC_CACHE = {}


def kernel(**inputs):
    if "nc" not in _NC_CACHE:
        _NC_CACHE["nc"] = Builder().build()
    nc = _NC_CACHE["nc"]
    maps = make_in_maps(inputs)
    res = run_bass_kernel_spmd(nc, maps, core_ids=list(range(8)))
    out = np.concatenate([np.asarray(r["out"]) for r in res.results], axis=0)
    return out.astype(np.float32, copy=False)
```

```python
import math
from contextlib import ExitStack

import numpy as np
import concourse.bass as bass
import concourse.mybir as mybir
from concourse.bass_utils import run_bass_kernel_spmd

F32 = mybir.dt.float32
BF16 = mybir.dt.bfloat16
I32 = mybir.dt.int32
AF = mybir.ActivationFunctionType
ALU = mybir.AluOpType
AX = mybir.AxisListType

ENGS = ("pe", "act", "dve", "pool", "sp")
N_DMA_SEMS = 12


class Op:
    __slots__ = ("eng", "fn", "deps", "is_dma", "signal", "sem", "val", "idx", "prev_dma", "guard")

    def __init__(self, eng, fn, is_dma, idx):
        self.eng = eng
        self.fn = fn
        self.deps = set()
        self.is_dma = is_dma
        self.signal = is_dma
        self.sem = None
        self.val = 0
        self.idx = idx
        self.prev_dma = None
        self.guard = None


class Sched:
    def __init__(self, nc):
        self.nc = nc
        self.ops = []
        self.last_w = {}
        self.readers = {}
        self.cur_guard = None
        self.nguard = 0
        self.regs = {}
        self.nregs = 0

    def op(self, eng, fn, reads=(), writes=(), dma=False):
        o = Op(eng, fn, dma, len(self.ops))
        if self.cur_guard is not None and not dma and eng in self.cur_guard[2]:
            o.guard = self.cur_guard
        self.ops.append(o)
        for t in reads:
            w = self.last_w.get(t)
            if w is not None:
                self._dep(o, w, raw=True)
        for t in writes:
            w = self.last_w.get(t)
            if w is not None:
                self._dep(o, w, raw=False)
            rd = self.readers.get(t)
            if rd:
                for k, r in rd.items():
                    if k == "dma":
                        for rr in r:
                            self._dep(o, rr, raw=False)
                    else:
                        self._dep(o, r, raw=False)
        for t in reads:
            rd = self.readers.setdefault(t, {})
            if dma:
                rd.setdefault("dma", []).append(o)
            else:
                rd[eng] = o
        for t in writes:
            self.last_w[t] = o
            self.readers[t] = {}
        return o

    def _dep(self, o, d, raw):
        if d is o:
            return
        if d.is_dma or o.is_dma:
            o.deps.add(d)
            d.signal = True
            return
        if d.eng == o.eng:
            if raw and o.eng != "pe":
                o.deps.add(d)
                d.signal = True
            return
        o.deps.add(d)
        d.signal = True

    def dma(self, q, out, in_, reads=(), writes=(), **kw):
        return self.op(q, lambda e: e.dma_start(out=out, in_=in_, **kw), reads, writes, dma=True)

    def mm(self, out, lhsT, rhs, start, stop, reads=(), writes=(), **kw):
        return self.op("pe", lambda e: e.matmul(out, lhsT, rhs, start=start, stop=stop, **kw),
                       reads, writes)

    def tr(self, out, in_, ident, reads=(), writes=()):
        return self.op("pe", lambda e: e.transpose(out, in_, ident), reads, writes)

    def emit(self, stack):
        nc = self.nc
        sems = {e: stack.enter_context(nc.semaphore("s_" + e)) for e in ENGS}
        dsems = {}
        dcount = {}
        dnext = {}
        dlast = {}
        cnt = {e: 0 for e in ENGS}
        per_eng = {e: [] for e in ENGS}
        for o in self.ops:
            per_eng[o.eng].append(o)
            if o.is_dma:
                q = o.eng
                if q not in dsems:
                    dsems[q] = [stack.enter_context(nc.semaphore("d_%s%d" % (q, i)))
                                for i in range(N_DMA_SEMS)]
                    dnext[q] = 0
                i = dnext[q]
                dnext[q] = (i + 1) % N_DMA_SEMS
                s = dsems[q][i]
                o.prev_dma = dlast.get(s)
                dlast[s] = o
                dcount[s] = dcount.get(s, 0) + 16
                o.sem = s
                o.val = dcount[s]
            elif o.signal:
                cnt[o.eng] += 1
                o.sem = sems[o.eng]
                o.val = cnt[o.eng]
        all_dma_final = dict(dcount)
        block = stack.enter_context(nc.Block())

        def run(eng_name):
            def body(e):
                waited = {}
                self.regs[eng_name] = [e.alloc_register("cnt%d_%s" % (r_, eng_name)) for r_ in range(self.nregs)] if eng_name in ("pe", "act", "dve") else []

                def need_of(o, need):
                    for d in o.deps:
                        if need.get(d.sem, 0) < d.val:
                            need[d.sem] = d.val
                    if o.prev_dma is not None:
                        p = o.prev_dma
                        if need.get(p.sem, 0) < p.val:
                            need[p.sem] = p.val

                def do_waits(need):
                    for s_, v in need.items():
                        if waited.get(s_, 0) < v:
                            e.wait_ge(s_, v)
                            waited[s_] = v

                def issue(o):
                    ins = o.fn(e)
                    if o.is_dma:
                        ins.then_inc(o.sem, 16)
                    elif o.signal:
                        ins.then_inc(o.sem, 1)

                lst = per_eng[eng_name]
                i = 0
                while i < len(lst):
                    o = lst[i]
                    if o.guard is None:
                        need = {}
                        need_of(o, need)
                        do_waits(need)
                        issue(o)
                        i += 1
                        continue
                    j = i
                    while j < len(lst) and lst[j].guard is o.guard:
                        j += 1
                    grp = lst[i:j]
                    need = {}
                    for g_ in grp:
                        need_of(g_, need)
                    inner = set(id(g_) for g_ in grp)
                    need_out = {}
                    for g_ in grp:
                        for d in g_.deps:
                            if d.guard is o.guard:
                                continue
                            if id(d) not in inner and need_out.get(d.sem, 0) < d.val:
                                need_out[d.sem] = d.val
                        if g_.prev_dma is not None:
                            p = g_.prev_dma
                            if need_out.get(p.sem, 0) < p.val:
                                need_out[p.sem] = p.val
                    do_waits(need_out)
                    nsig = sum(1 for g_ in grp if g_.signal)
                    thr = o.guard[1]
                    with e.If_lt(self.regs[eng_name][o.guard[3]], thr + 1):
                        if nsig:
                            e.sem_inc(sems[eng_name], nsig)
                        else:
                            e.nop()
                    saved = dict(waited)
                    with e.Else():
                        for g_ in grp:
                            nd = {}
                            need_of(g_, nd)
                            do_waits(nd)
                            issue(g_)
                    waited.clear()
                    waited.update(saved)
                    i = j
                if eng_name == "sp":
                    for s_, v in all_dma_final.items():
                        if waited.get(s_, 0) < v:
                            e.wait_ge(s_, v)
            return body

        block.tensor(run("pe"))
        block.scalar(run("act"))
        block.vector(run("dve"))
        block.gpsimd(run("pool"))
        block.sync(run("sp"))

    def guard(self, thr, ridx, engines=("pe", "act", "dve")):
        self.nguard += 1
        self.cur_guard = (self.nguard, thr, tuple(engines), ridx)

    def end_guard(self):
        self.cur_guard = None

    def load_counts(self, ap, n, reads, engines=("pe", "act", "dve")):
        self.nregs = n
        for en in engines:
            self.op(en, lambda e, en=en: e.reg_load(self.regs[en], ap), reads=reads)

    def barrier(self):
        last = {}
        dmas = []
        for o in self.ops:
            if o.is_dma:
                dmas.append(o)
            else:
                last[o.eng] = o
        self._bar = list(last.values()) + dmas
        self._bar_seen = set()
        self.last_w = {}
        self.readers = {}

    _bar = None
    _bar_seen = None


_orig_op = Sched.op


def _op_with_barrier(self, eng, fn, reads=(), writes=(), dma=False):
    o = _orig_op(self, eng, fn, reads, writes, dma)
    if self._bar is not None and eng not in self._bar_seen:
        self._bar_seen.add(eng)
        for d in self._bar:
            if d is o:
                continue
            if d.is_dma or d.eng != eng:
                o.deps.add(d)
                d.signal = True
    return o


Sched.op = _op_with_barrier

B0_LA = 2
B0_NPS = 3
NB = 2
SEQ = 2048
CTX = 256
T = SEQ + CTX
NT = T // 128
NTL = SEQ // 128
D = 1024
KC = 8
DFF = 2816
NE = 8
DFE = 3584
EPS = 1e-6
GRID_W = 64
VW = 1040
SKIP_E = False
MOE_GS = 512
CAP = 2048


class Arena:
    def __init__(self, ap, words):
        self.ap = ap
        self.words = words
        self.top = 0

    def f32(self, n):
        assert self.top + n <= self.words, "arena overflow %d" % (self.top + n)
        a = self.ap[:, self.top:self.top + n]
        self.top += n
        return a

    def bf(self, n):
        assert n % 2 == 0
        return self.f32(n // 2).bitcast(BF16)


class Builder:
    def __init__(self, dbg=False, phases=None):
        self.dbg = dbg
        self.phases = phases
        self.nc = bass.Bass("TRN2", target_bir_lowering=False)
        self.S = Sched(self.nc)
        self.inputs = {}

    def din(self, name, shape, dt=F32):
        t = self.nc.dram_tensor(name, list(shape), dt, kind="ExternalInput").ap()
        self.inputs[name] = t
        return t

    def dscr(self, name, shape, dt):
        kind = "ExternalOutput" if self.dbg else "Internal"
        return self.nc.dram_tensor(name, list(shape), dt, kind=kind).ap()

    def want(self, ph):
        return self.phases is None or ph in self.phases

    def build(self):
        nc = self.nc
        shapes = {"x": [NB, SEQ, D], "ctx": [NB, CTX, D], "c": [NB, D], "c_ctx": [D],
                  "l0_w_gate": [D, DFF], "l0_w_up": [D, DFF], "l0_w_down": [DFF, D],
                  "na_bias": [5, 16, 128, 640], "l1_subln": [128], "l1_w_router": [D, NE],
                  "l1_w_gate": [NE, D, DFE], "l1_w_up": [NE, D, DFE], "l1_w_down": [NE, DFE, D],
                  "final_norm": [D], "ident": [128, 128], "ltri": [128, 128], "ecap": [128, NE], "rope_cs": [128, NTL, 64], "rope_sn": [128, NTL, 64]}
        shapes.update({"l0_w_ada": [D, 6 * D], "l0_b_ada": [6 * D], "l0_norm_mix": [D], "l0_w_qkv": [D, 3 * D],
                       "l0_w_o": [D, D], "l0_norm_ffn": [D], "l1_w_ada": [D, 6 * D], "l1_b_ada": [6 * D],
                       "l1_norm_mix": [D], "l1_w_qkv": [D, 3 * D], "l1_w_o": [D, D], "l1_norm_ffn": [D]})
        for k in ("q1", "k1", "q2", "k2"):
            shapes["l1_lambda_" + k] = [64]
        bld = self

        class Lazy(dict):
            def __missing__(d, name):
                if bld.dbg:
                    d[name] = bld.din(name, shapes[name])
                    return d[name]
                raise KeyError(name)

        I = Lazy()
        if not self.dbg:
            for name in sorted(shapes):
                I[name] = self.din(name, shapes[name])
        self.I = I
        self.out = nc.dram_tensor("out", [NB, SEQ, D], F32, kind="ExternalOutput").ap()
        self.MOD = self.dscr("MOD", [2, 3, 6 * D], F32)
        self.QT = self.dscr("QT", [NB, KC, 128, T], BF16)
        self.KT = self.dscr("KT", [NB, KC, 128, T], BF16)
        self.VS = self.dscr("VS", [NB, T, VW], BF16)
        self.OS = self.dscr("OS", [NB, T, D], BF16)
        self.XR1 = self.dscr("XR1", [NB, T, D], F32)
        self.XR2 = self.dscr("XR2", [NB, T, D], F32)
        self.H2T = self.dscr("H2T", [KC, 128, NB * T], BF16)
        self.GAT = self.dscr("GAT", [NB, SEQ, NE], F32)
        self.H2S = [self.dscr("H2S%d" % i, [NE * CAP, D], BF16) for i in range(NB)]
        self.SLI = self.dscr("SLI", [NB, SEQ, 2], I32)
        self.SLW = self.dscr("SLW", [NB, SEQ, 2], F32)
        self.CNT = self.dscr("CNT", [NB, NE], I32)
        self.OUTS = [self.dscr("OUTS%d" % i, [NE * CAP, D], F32) for i in range(NB)]

        with ExitStack() as st:
            AW = 51200
            arena_t = st.enter_context(nc.sbuf_tensor("arena", [128, AW], F32))
            self.psum = st.enter_context(nc.psum_tensor("psum", [128, 8, 512], F32))
            self.A = Arena(arena_t, AW)
            self.ident_f = self.A.f32(128)
            self.ident = self.A.bf(128)
            S = self.S
            S.dma("sp", self.ident_f, I["ident"], writes=["ident_f"])
            S.dma("pool", self.ident, I["ident"], writes=["ident"])
            self.base = self.A.top
            if self.want("0"):
                self.phase0()
            for l in range(2):
                if self.want("A%d" % l):
                    self.phaseA(l)
                if self.want("B%d" % l):
                    (self.phaseB0 if l == 0 else self.phaseB1)()
                if self.want("C%d" % l):
                    self.phaseC(l)
                if self.want("D%d" % l):
                    (self.phaseD0 if l == 0 else self.phaseD1)()
            S.emit(st)
        return nc

    def new_phase(self):
        self.S.barrier()
        self.A.top = self.base

    def pbank(self, b):
        return self.psum[:, b, :]

    def pbank_bf(self, b, nb=1):
        v = self.psum[:, b:b + nb, :].rearrange("p a b -> p (a b)").bitcast(BF16)
        return v

    def phase0(self):
        S, A, I = self.S, self.A, self.I
        self.new_phase()
        cT = A.f32(24).rearrange("p (k j) -> p k j", k=KC)
        cs = A.f32(24).rearrange("p (k j) -> p k j", k=KC)
        srcs = [I["c"][0], I["c"][1], I["c_ctx"]]
        for j, src in enumerate(srcs):
            S.dma("sp", cT[:, :, j], src.rearrange("(k p) -> p k", p=128), writes=["cT"],
                  allow_slow_non_contiguous=True)
        S.op("act", lambda e: e.activation(cs, cT, AF.Silu), reads=["cT"], writes=["cs"])
        bt = A.f32(6 * D)
        modv = A.f32(6 * D)
        wb = [A.f32(KC * 512).rearrange("p (k n) -> p k n", k=KC) for _ in range(2)]
        it = 0
        for l in range(2):
            S.dma("sp", bt[0:3, :], I["l%d_b_ada" % l].partition_broadcast(3), reads=[], writes=["bt"])
            for n in range(12):
                b = it % 2
                it += 1
                S.dma("sp", wb[b], I["l%d_w_ada" % l][:, n * 512:(n + 1) * 512].rearrange("(k p) n -> p k n", p=128),
                      writes=[("wb", b)])
                pb = self.psum[0:3, n % 2, :]
                for k in range(KC):
                    S.mm(pb, cs[:, k, :], wb[b][:, k, :], k == 0, k == KC - 1,
                         reads=["cs", ("wb", b)], writes=[("p0", n % 2)])
                S.op("dve", lambda e, pb=pb, n=n: e.tensor_tensor(modv[0:3, n * 512:(n + 1) * 512], pb,
                                                                 bt[0:3, n * 512:(n + 1) * 512], ALU.add),
                     reads=[("p0", n % 2), "bt"], writes=["modv"])
            S.dma("sp", self.MOD[l], modv[0:3, :], reads=["modv"], writes=[("MOD", l)])

    def bc_row(self, dst, row, reads, tok):
        self.S.dma("sp", dst, row.partition_broadcast(128), reads=reads, writes=[tok])

    def mod_tiles(self, l, j, idx_scale, idx_shift, gain, tag):
        S, A = self.S, self.A
        tmp = A.f32(D)
        At = A.f32(D)
        SH = A.f32(D)
        self.bc_row(tmp, self.MOD[l, j, idx_scale * D:(idx_scale + 1) * D], [("MOD", l)], ("mtmp", tag))
        self.bc_row(SH, self.MOD[l, j, idx_shift * D:(idx_shift + 1) * D], [("MOD", l)], ("SH", tag))
        S.op("dve", lambda e: e.scalar_tensor_tensor(At, tmp, 1.0, gain, ALU.add, ALU.mult),
             reads=[("mtmp", tag), "gain"], writes=[("A", tag)])
        return At, SH

    def xsrc(self, l, bb, t):
        if l == 0:
            if t < NTL:
                return self.I["x"][bb, t * 128:(t + 1) * 128, :], []
            return self.I["ctx"][bb, (t - NTL) * 128:(t - NTL + 1) * 128, :], []
        return self.XR2[bb, t * 128:(t + 1) * 128, :], [("XR2", bb, t)]

    def rms_mod(self, xt, xtok, At, Atok, SH, SHtok, h_out, h_tok, junk, st, sttok, t1):
        S = self.S
        S.op("act", lambda e: e.activation(junk, xt, AF.Square, accum_out=st[:, 0:1]),
             reads=[xtok], writes=["junk", (sttok, 0)])
        S.op("act", lambda e: e.activation(st[:, 1:2], st[:, 0:1], AF.Sqrt, bias=EPS, scale=1.0 / D),
             reads=[(sttok, 0)], writes=[(sttok, 1)])
        S.op("dve", lambda e: e.reciprocal(st[:, 2:3], st[:, 1:2]), reads=[(sttok, 1)], writes=[(sttok, 2)])
        S.op("dve", lambda e: e.scalar_tensor_tensor(t1, xt, st[:, 2:3], At, ALU.mult, ALU.mult),
             reads=[xtok, (sttok, 2), Atok], writes=["t1"])
        S.op("dve", lambda e: e.tensor_tensor(h_out, t1, SH, ALU.add), reads=["t1", SHtok], writes=[h_tok])

    def pipeline(self, n, order):
        lo = -max(off for _, off in order)
        hi = n - min(off for _, off in order)
        for s_ in range(lo, hi):
            for fn, off in order:
                t = s_ + off
                if 0 <= t < n:
                    fn(t)

    def phaseA(self, l):
        S, A, I = self.S, self.A, self.I
        self.new_phase()
        H, dv = (16, 64) if l == 0 else (8, 128)
        vw = H * (dv + 1)
        pre = "l%d_" % l
        wqkv = A.bf(KC * 3 * D).rearrange("p (k n) -> p k n", k=KC)
        for n in range(6):
            S.dma("pool", wqkv[:, :, n * 512:(n + 1) * 512],
                  I[pre + "w_qkv"][:, n * 512:(n + 1) * 512].rearrange("(k p) n -> p k n", p=128),
                  writes=[("wqkv", n)])
        gain = A.f32(D)
        self.bc_row(gain, I[pre + "norm_mix"], [], "gain")
        Ac, SHc = self.mod_tiles(l, 2, 1, 0, gain, "c")
        lat_mods = [self.mod_tiles(l, bb, 1, 0, gain, "l%d" % bb) for bb in range(NB)]
        if l == 1:
            cs_t = A.f32(NTL * 64).rearrange("p (t j) -> p t j", t=NTL)
            sn_t = A.f32(NTL * 64).rearrange("p (t j) -> p t j", t=NTL)
            S.dma("sp", cs_t, I["rope_cs"], writes=["rope_cs"])
            S.dma("sp", sn_t, I["rope_sn"], writes=["rope_sn"])
            qk32 = A.f32(2 * D)
            r1 = A.f32(2 * D)
            r2 = A.f32(2 * D)
        xts = [A.f32(D) for _ in range(3)]
        junk = A.bf(D)
        stt = A.f32(4)
        t1 = A.f32(D)
        hb = A.bf(D)
        hT = [A.bf(KC * 128).rearrange("p (k n) -> p k n", k=KC) for _ in range(2)]
        qkbs = [A.bf(2 * D) for _ in range(2)]
        qkT = [A.bf(16 * 128).rearrange("p (k n) -> p k n", k=16) for _ in range(2)]
        vaug = [A.bf(vw) for _ in range(2)]
        for i in range(2):
            S.op("pool", lambda e, i=i: e.memset(vaug[i], 1.0), writes=[("vaug", i)])
        pT = self.pbank_bf(0).rearrange("p (k n) -> p k n", k=KC)
        pQT = self.pbank_bf(5, 2).rearrange("p (k n) -> p k n", k=16)
        ntot = NB * NT

        def info(ti):
            bb, t = divmod(ti, NT)
            return bb, t, t < NTL

        def stX(ti):
            bb, t, lat = info(ti)
            src, srd = self.xsrc(l, bb, t)
            S.dma("sp", xts[ti % 3], src, reads=srd, writes=[("xt", ti % 3)])

        def stN(ti):
            bb, t, lat = info(ti)
            Al, SHl = lat_mods[bb]
            tg = "l%d" % bb
            self.rms_mod(xts[ti % 3], ("xt", ti % 3), Al if lat else Ac, ("A", tg if lat else "c"),
                         SHl if lat else SHc, ("SH", tg if lat else "c"), hb, "hb", junk, stt, "stA", t1)

        def stTh(ti):
            b2 = ti % 2
            for k in range(KC):
                S.tr(pT[:, k, :], hb[:, k * 128:(k + 1) * 128], self.ident, reads=["hb", "ident"], writes=["pT"])
            S.op("act", lambda e, b2=b2: e.copy(hT[b2], pT), reads=["pT"], writes=[("hT", b2)])

        def stQ(ti):
            bb, t, lat = info(ti)
            b2 = ti % 2
            qkb = qkbs[b2]
            va = vaug[b2].rearrange("p (h d) -> p h d", h=H)
            for n in range(6):
                pq = self.pbank(1 + n % 4)
                for k in range(KC):
                    S.mm(pq, hT[b2][:, k, :], wqkv[:, k, n * 512:(n + 1) * 512], k == 0, k == KC - 1,
                         reads=[("hT", b2), ("wqkv", n)], writes=[("pq", n % 4)])
                if n < 4:
                    if l == 1:
                        dst, dtok = qk32[:, n * 512:(n + 1) * 512], ("qk32", n)
                    else:
                        dst, dtok = qkb[:, n * 512:(n + 1) * 512], ("qkb", b2)
                    if n < 2:
                        S.op("act", lambda e, dst=dst, pq=pq: e.mul(dst, pq, 0.125),
                             reads=[("pq", n % 4)], writes=[dtok])
                    else:
                        S.op("act", lambda e, dst=dst, pq=pq: e.copy(dst, pq),
                             reads=[("pq", n % 4)], writes=[dtok])
                else:
                    hpb = 512 // dv
                    h0 = (n - 4) * hpb
                    S.op("dve", lambda e, pq=pq, h0=h0, hpb=hpb, va=va: e.tensor_copy(
                        va[:, h0:h0 + hpb, 0:dv], pq.rearrange("p (h d) -> p h d", h=hpb)),
                        reads=[("pq", n % 4)], writes=[("vaug", b2)])
            if l == 1:
                qtoks = [("qk32", n) for n in range(4)]
                if lat:
                    xv = qk32.rearrange("p (g j) -> p g j", g=32)
                    csb = cs_t[:, t, :].unsqueeze(1).broadcast_to([128, 32, 64])
                    S.op("pool", lambda e, xv=xv, csb=csb: e.tensor_tensor(
                        r1.rearrange("p (g j) -> p g j", g=32), xv, csb, ALU.mult),
                        reads=qtoks + ["rope_cs"], writes=["r1"])
                    x5 = qk32.rearrange("p (g a h j) -> p g a h j", g=32, a=2, h=2)
                    o5 = r2.rearrange("p (g a h j) -> p g a h j", g=32, a=2, h=2)
                    s4 = sn_t[:, t, :].rearrange("p (a h j) -> p a h j", a=2, h=2)
                    for hh in range(2):
                        snb = s4[:, :, hh, :].unsqueeze(1).broadcast_to([128, 32, 2, 16])
                        S.op("dve", lambda e, hh=hh, snb=snb: e.tensor_tensor(
                            o5[:, :, :, hh, :], x5[:, :, :, 1 - hh, :], snb, ALU.mult),
                            reads=qtoks + ["rope_sn"], writes=[("r2", hh)])
                    S.op("dve", lambda e, qkb=qkb: e.tensor_tensor(qkb, r1, r2, ALU.add),
                         reads=["r1", ("r2", 0), ("r2", 1)], writes=[("qkb", b2)])
                else:
                    S.op("dve", lambda e, qkb=qkb: e.tensor_copy(qkb, qk32), reads=qtoks, writes=[("qkb", b2)])

        def stTq(ti):
            bb, t, lat = info(ti)
            b2 = ti % 2
            qkb = qkbs[b2]
            for j in range(16):
                S.tr(pQT[:, j, :], qkb[:, j * 128:(j + 1) * 128], self.ident, reads=[("qkb", b2), "ident"],
                     writes=["pQT"])
            S.op("act", lambda e, b2=b2: e.copy(qkT[b2], pQT), reads=["pQT"], writes=[("qkT", b2)])
            S.dma("sp", self.QT[bb, :, :, t * 128:(t + 1) * 128].rearrange("c p n -> p c n"),
                  qkT[b2][:, 0:8, :], reads=[("qkT", b2)], writes=[("QT", bb)])
            S.dma("sp", self.KT[bb, :, :, t * 128:(t + 1) * 128].rearrange("c p n -> p c n"),
                  qkT[b2][:, 8:16, :], reads=[("qkT", b2)], writes=[("KT", bb)])
            S.dma("sp", self.VS[bb, t * 128:(t + 1) * 128, 0:vw], vaug[b2], reads=[("vaug", b2)],
                  writes=[("VS", bb)])

        self.pipeline(ntot, [(stX, 2), (stN, 1), (stQ, 0), (stTh, 1), (stTq, -1)])

    def phaseB0(self):
        S, A, I = self.S, self.A, self.I
        self.new_phase()
        bt32 = A.f32(5 * 2 * 640)
        EB = A.bf(5 * 2 * 640)
        EBv = EB.rearrange("p (v h n) -> p v h n", v=5, h=2)
        QTc = [A.bf(T) for _ in range(2)]
        KTc = [A.bf(T) for _ in range(2)]
        Vc = [A.bf(NT * 130).rearrange("p (t h d) -> p t h d", t=NT, h=2) for _ in range(2)]
        Oc = [A.bf(NT * 128).rearrange("p (t n) -> p t n", t=NT) for _ in range(2)]
        NEB = 4
        E = [A.bf(8 * 128).rearrange("p (j n) -> p j n", j=8) for _ in range(NEB)]
        rec = A.f32(8)
        NPS = B0_NPS
        pS = [self.psum[:, 2 * i:2 * i + 2, :].rearrange("p a (j n) -> p (a j) n", n=128) for i in range(NPS)]
        NPO = 8 - 2 * NPS
        pO = self.psum[:, 2 * NPS:8, 0:128]
        var_of = {0: 0, 1: 1, 14: 3, 15: 4}
        it = 0
        qi = 0
        for c in range(KC):
            btv = bt32.rearrange("p (v h n) -> p v h n", v=5, h=2)
            for v5 in range(5):
                S.dma("sp", btv[:, v5], I["na_bias"][v5, 2 * c:2 * c + 2].rearrange("h p n -> p h n"),
                      writes=["bt32"])
            S.op("act", lambda e: e.activation(EB, bt32, AF.Exp), reads=["bt32"], writes=["EB"])
            for bb in range(NB):
                b2 = it % 2
                it += 1
                S.dma("sp", QTc[b2], self.QT[bb, c], writes=[("QTc", b2)])
                S.dma("sp", KTc[b2], self.KT[bb, c], writes=[("KTc", b2)])
                S.dma("sp", Vc[b2].rearrange("p t h d -> p t (h d)"),
                      self.VS[bb, :, 2 * c * 65:(2 * c + 2) * 65].rearrange("(t p) w -> p t w", p=128),
                      writes=[("Vc", b2)])
                LA = B0_LA
                items = [(hh, i) for hh in range(2) for i in range(NT)]
                meta = {}

                def stage1(hh, i, b2=b2):
                    nonlocal qi
                    pr = slice(64 * hh, 64 * hh + 64)
                    if i < NTL:
                        j0 = min(max(i - 2, 0), 11)
                        kts = [j0 + jj for jj in range(5)] + [16, 17]
                        v = var_of.get(i, 2)
                    else:
                        kts = [16, 17]
                        v = None
                    nk = len(kts)
                    sb = qi % NPS
                    eb = qi % NEB
                    slot = qi % NPO
                    qi += 1
                    meta[(hh, i)] = (kts, eb, slot)
                    for jj, kt in enumerate(kts):
                        S.mm(pS[sb][:, jj, :], KTc[b2][pr, kt * 128:(kt + 1) * 128],
                             QTc[b2][pr, i * 128:(i + 1) * 128], True, True,
                             reads=[("KTc", b2), ("QTc", b2)], writes=[("pS", sb)])
                    if nk > 4:
                        S.op("act", lambda e, sb=sb, eb=eb: e.activation(E[eb][:, 0:4, :], pS[sb][:, 0:4, :], AF.Exp),
                             reads=[("pS", sb)], writes=[("E", eb)])
                        S.op("act", lambda e, sb=sb, eb=eb, nk=nk: e.activation(E[eb][:, 4:nk, :], pS[sb][:, 4:nk, :], AF.Exp),
                             reads=[("pS", sb)], writes=[("E", eb)])
                    else:
                        S.op("act", lambda e, sb=sb, eb=eb, nk=nk: e.activation(E[eb][:, 0:nk, :], pS[sb][:, 0:nk, :], AF.Exp),
                             reads=[("pS", sb)], writes=[("E", eb)])
                    if v is not None:
                        S.op("dve", lambda e, eb=eb, v=v, hh=hh: e.tensor_tensor(
                            E[eb][:, 0:5, :], E[eb][:, 0:5, :],
                            EBv[:, v, hh, :].rearrange("p (j n) -> p j n", j=5), ALU.mult),
                            reads=[("E", eb), "EB"], writes=[("E", eb)])

                def stage2(hh, i, b2=b2):
                    kts, eb, slot = meta[(hh, i)]
                    nk = len(kts)
                    for jj, kt in enumerate(kts):
                        S.mm(pO[:, slot, 0:65], E[eb][:, jj, :], Vc[b2][:, kt, hh, :], jj == 0, jj == nk - 1,
                             reads=[("E", eb), ("Vc", b2)], writes=[("pO", slot)])
                    S.op("dve", lambda e, slot=slot: e.reciprocal(rec[:, slot:slot + 1], pO[:, slot, 64:65]),
                         reads=[("pO", slot)], writes=[("rec", slot)])
                    S.op("dve", lambda e, slot=slot, i=i, hh=hh, b2=b2: e.tensor_scalar(
                        Oc[b2][:, i, 64 * hh:64 * hh + 64], pO[:, slot, 0:64], rec[:, slot:slot + 1], None, ALU.mult),
                        reads=[("pO", slot), ("rec", slot)], writes=[("Oc", b2)])

                for idx in range(len(items) + LA):
                    if idx < len(items):
                        stage1(*items[idx])
                    if idx >= LA:
                        stage2(*items[idx - LA])
                S.dma("sp", self.OS[bb, :, c * 128:(c + 1) * 128].rearrange("(t p) n -> p t n", p=128),
                      Oc[b2], reads=[("Oc", b2)], writes=[("OS", bb)])

    def phaseC(self, l):
        S, A, I = self.S, self.A, self.I
        self.new_phase()
        pre = "l%d_" % l
        ntile = NT if l == 0 else NTL
        Wo = A.bf(KC * D).rearrange("p (k n) -> p k n", k=KC)
        for h2_ in range(2):
            S.dma("pool", Wo[:, :, h2_ * 512:(h2_ + 1) * 512],
                  I[pre + "w_o"][:, h2_ * 512:(h2_ + 1) * 512].rearrange("(k p) n -> p k n", p=128), writes=["Wo"])
        WoL = A.bf(KC * D).rearrange("p (k n) -> p k n", k=KC)
        gtmp = A.f32(D)
        gain = A.f32(D)
        self.bc_row(gain, I[pre + "norm_ffn"], [], "gain")
        if l == 0:
            WoC = A.bf(KC * D).rearrange("p (k n) -> p k n", k=KC)
            self.bc_row(gtmp, self.MOD[l, 2, 2 * D:3 * D], [], "gtmp")
            for k in range(KC):
                S.op("dve", lambda e, k=k: e.tensor_tensor(WoC[:, k, :], Wo[:, k, :], gtmp, ALU.mult),
                     reads=["Wo", "gtmp"], writes=["WoC"])
            A2c, SH2c = self.mod_tiles(l, 2, 4, 3, gain, "c")
        else:
            Wr = A.f32(KC * NE).rearrange("p (k n) -> p k n", k=KC)
            S.dma("sp", Wr, I["l1_w_router"].rearrange("(k p) n -> p k n", p=128), writes=["Wr"])
            h2f = A.f32(D)
            h2fT = A.f32(KC * 128).rearrange("p (k n) -> p k n", k=KC)
            lg = A.f32(8 * NE)
            sm = A.f32(8)
            LT = A.f32(128)
            ONES = A.f32(128)
            ecap = A.f32(NE)
            S.dma("sp", LT, I["ltri"], writes=["LT"])
            S.dma("sp", ecap, I["ecap"], writes=["ecap"])
            S.op("pool", lambda e: e.memset(ONES, 1.0), writes=["ONES"])
            rt = A.f32(4 * NE)
            cnt = A.f32(NE)
            cnti = A.f32(NE).bitcast(I32)
            slw = [A.f32(4) for _ in range(2)]
            idx = [A.f32(2).bitcast(I32) for _ in range(2)]
        Ots = [A.bf(D) for _ in range(3)]
        xts = [A.f32(D) for _ in range(3)]
        OT = [A.bf(KC * 128).rearrange("p (k n) -> p k n", k=KC) for _ in range(2)]
        x1s = [A.f32(D) for _ in range(3)]
        junk = A.bf(D)
        stt = A.f32(4)
        t1 = A.f32(D)
        h2s = [A.bf(D) for _ in range(2)]
        if l == 1:
            h2fs = [A.f32(D) for _ in range(2)]
        h2T = [A.bf(KC * 128).rearrange("p (k n) -> p k n", k=KC) for _ in range(2)]
        pT = self.pbank_bf(0).rearrange("p (k n) -> p k n", k=KC)
        pY = [self.pbank(1), self.pbank(2)]
        pT2 = self.pbank_bf(3).rearrange("p (k n) -> p k n", k=KC)
        pF = self.psum[:, 4:6, :].rearrange("p a (j n) -> p (a j) n", n=128)
        pL = self.psum[:, 6, 0:NE]
        WoLs = [WoL, A.bf(KC * D).rearrange("p (k n) -> p k n", k=KC)]
        mods = []
        for bb in range(NB):
            self.bc_row(gtmp, self.MOD[l, bb, 2 * D:3 * D], [], "gtmp")
            for k in range(KC):
                S.op("dve", lambda e, k=k, bb=bb: e.tensor_tensor(WoLs[bb][:, k, :], Wo[:, k, :], gtmp, ALU.mult),
                     reads=["Wo", "gtmp"], writes=[("WoL", bb)])
            mods.append(self.mod_tiles(l, bb, 4, 3, gain, "l%d" % bb))
        ntot = NB * ntile

        def info(ti):
            bb, t = divmod(ti, ntile)
            return bb, t, t < NTL

        def stL(ti):
            bb, t, lat = info(ti)
            S.dma("sp", Ots[ti % 3], self.OS[bb, t * 128:(t + 1) * 128, :], writes=[("Ot", ti % 3)])
            src, srd = self.xsrc(l, bb, t)
            S.dma("sp", xts[ti % 3], src, reads=srd, writes=[("xt", ti % 3)])

        def stTo(ti):
            b2 = ti % 2
            for k in range(KC):
                S.tr(pT[:, k, :], Ots[ti % 3][:, k * 128:(k + 1) * 128], self.ident, reads=[("Ot", ti % 3), "ident"],
                     writes=["pT"])
            S.op("act", lambda e, b2=b2: e.copy(OT[b2], pT), reads=["pT"], writes=[("OT", b2)])

        def stY(ti):
            bb, t, lat = info(ti)
            b2 = ti % 2
            b3 = ti % 3
            W = WoLs[bb] if lat else WoC
            wtok = ("WoL", bb) if lat else "WoC"
            for hf in range(2):
                for k in range(KC):
                    S.mm(pY[hf], OT[b2][:, k, :], W[:, k, hf * 512:(hf + 1) * 512], k == 0, k == KC - 1,
                         reads=[("OT", b2), wtok], writes=[("pY", hf)])
                S.op("dve", lambda e, hf=hf, b3=b3: e.tensor_tensor(
                    x1s[b3][:, hf * 512:(hf + 1) * 512], pY[hf], xts[b3][:, hf * 512:(hf + 1) * 512], ALU.add),
                    reads=[("pY", hf), ("xt", b3)], writes=[("x1", b3)])
            S.dma("sp", self.XR1[bb, t * 128:(t + 1) * 128, :], x1s[b3], reads=[("x1", b3)],
                  writes=[("XR1", bb, t)])

        def stN(ti):
            bb, t, lat = info(ti)
            b2 = ti % 2
            b3 = ti % 3
            A2l, SH2l = mods[bb]
            tg = "l%d" % bb
            if l == 0:
                self.rms_mod(x1s[b3], ("x1", b3), A2l if lat else A2c, ("A", tg if lat else "c"),
                             SH2l if lat else SH2c, ("SH", tg if lat else "c"), h2s[b2], ("h2", b2), junk, stt, "stC", t1)
            else:
                self.rms_mod(x1s[b3], ("x1", b3), A2l, ("A", tg), SH2l, ("SH", tg), h2fs[b2], ("h2f", b2), junk, stt,
                             "stC", t1)
                S.op("pool", lambda e, b2=b2: e.tensor_copy(h2s[b2], h2fs[b2]), reads=[("h2f", b2)], writes=[("h2", b2)])

        def stTh(ti):
            bb, t, lat = info(ti)
            b2 = ti % 2
            h2 = h2s[b2]
            for k in range(KC):
                S.tr(pT2[:, k, :], h2[:, k * 128:(k + 1) * 128], self.ident, reads=[("h2", b2), "ident"], writes=["pT2"])
            S.op("act", lambda e, b2=b2: e.copy(h2T[b2], pT2), reads=["pT2"], writes=[("h2T", b2)])
            col = bb * T + t * 128
            S.dma("sp", self.H2T[:, :, col:col + 128].rearrange("k p n -> p k n"), h2T[b2],
                  reads=[("h2T", b2)], writes=[("H2T", bb, t)])
            if l == 1:
                h2f = h2fs[b2]
                for k in range(KC):
                    S.tr(pF[:, k, :], h2f[:, k * 128:(k + 1) * 128], self.ident_f, reads=[("h2f", b2), "ident_f"],
                         writes=["pF"])
                S.op("dve", lambda e: e.tensor_copy(h2fT, pF), reads=["pF"], writes=["h2fT"])
                for k in range(KC):
                    S.mm(pL, h2fT[:, k, :], Wr[:, k, :], k == 0, k == KC - 1, reads=["h2fT", "Wr"], writes=["pL"])
                L = lambda i: lg[:, i * NE:(i + 1) * NE]
                V = lambda i: sm[:, i:i + 1]
                S.op("dve", lambda e: e.tensor_copy(L(0), pL), reads=["pL"], writes=["g0"])
                S.op("dve", lambda e: e.reduce_max(V(0), L(0), AX.X), reads=["g0"], writes=["m1"])
                S.op("dve", lambda e: e.tensor_scalar(L(1), L(0), V(0), None, ALU.is_equal),
                     reads=["g0", "m1"], writes=["g1"])
                S.op("dve", lambda e: e.scalar_tensor_tensor(L(2), L(1), -1e30, L(0), ALU.mult, ALU.add),
                     reads=["g0", "g1"], writes=["g2"])
                S.op("dve", lambda e: e.reduce_max(V(1), L(2), AX.X), reads=["g2"], writes=["m2"])
                S.op("dve", lambda e: e.tensor_scalar(L(3), L(0), V(1), None, ALU.is_ge),
                     reads=["g0", "m2"], writes=["g3"])
                S.op("dve", lambda e: e.tensor_scalar(V(2), V(0), -1.0, None, ALU.mult), reads=["m1"], writes=["nm1"])
                S.op("act", lambda e: e.activation(L(4), L(0), AF.Exp, bias=V(2), scale=1.0),
                     reads=["g0", "nm1"], writes=["g4"])
                S.op("dve", lambda e: e.tensor_tensor(L(5), L(4), L(3), ALU.mult), reads=["g4", "g3"], writes=["g5"])
                S.op("dve", lambda e: e.reduce_sum(V(3), L(5), AX.X), reads=["g5"], writes=["ssum"])
                S.op("dve", lambda e: e.reciprocal(V(4), V(3)), reads=["ssum"], writes=["rsum"])
                S.op("dve", lambda e: e.tensor_scalar(L(6), L(5), V(4), None, ALU.mult), reads=["g5", "rsum"],
                     writes=["g6"])
                S.dma("sp", self.GAT[bb, t * 128:(t + 1) * 128, :], L(6), reads=["g6"], writes=[("GAT", bb, t)])
                pP = self.psum[:, 7, 0:NE]
                pC = self.psum[:, 7, NE:2 * NE]
                Rr = lambda i: rt[:, i * NE:(i + 1) * NE]
                if t == 0:
                    S.op("dve", lambda e: e.memset(cnt, 0.0), writes=["cnt"])
                S.mm(pP, LT, L(3), True, True, reads=["LT", "g3"], writes=["pPC"])
                S.mm(pC, ONES, L(3), True, True, reads=["ONES", "g3"], writes=["pPC"])
                S.op("dve", lambda e: e.tensor_tensor(Rr(0), pP, cnt, ALU.add), reads=["pPC", "cnt"], writes=["r0"])
                S.op("dve", lambda e: e.tensor_tensor(cnt, cnt, pC, ALU.add), reads=["pPC", "cnt", "r0"], writes=["cnt"])
                S.op("dve", lambda e: e.tensor_tensor(Rr(0), Rr(0), ecap, ALU.add), reads=["r0", "ecap"], writes=["r0b"])
                S.op("dve", lambda e: e.tensor_tensor(Rr(1), L(3), L(1), ALU.subtract), reads=["g3", "g1"], writes=["r1"])
                sw = slw[b2]
                for col, (msk, mtok, val, vtok) in enumerate(((L(1), "g1", Rr(0), "r0b"), (Rr(1), "r1", Rr(0), "r0b"),
                                                              (L(1), "g1", L(6), "g6"), (Rr(1), "r1", L(6), "g6"))):
                    S.op("dve", lambda e, msk=msk, val=val: e.tensor_tensor(Rr(2), msk, val, ALU.mult),
                         reads=[mtok, vtok], writes=["r2"])
                    S.op("dve", lambda e, col=col, sw=sw: e.reduce_sum(sw[:, col:col + 1], Rr(2), AX.X), reads=["r2"],
                         writes=[("slw", b2, col)])
                S.op("dve", lambda e, sw=sw, b2=b2: e.tensor_copy(idx[b2], sw[:, 0:2]),
                     reads=[("slw", b2, 0), ("slw", b2, 1)], writes=[("idx", b2)])
                for which in range(2):
                    S.op("pool", lambda e, bb=bb, b2=b2, which=which: e.indirect_dma_start(
                        out=self.H2S[bb], out_offset=bass.IndirectOffsetOnAxis(ap=idx[b2][:, which:which + 1], axis=0),
                        in_=h2s[b2], in_offset=None), reads=[("h2", b2), ("idx", b2)], writes=[("H2S", bb)], dma=True)
                S.dma("sp", self.SLI[bb, t * 128:(t + 1) * 128, :], idx[b2], reads=[("idx", b2)], writes=[("SLI", bb, t)])
                S.dma("sp", self.SLW[bb, t * 128:(t + 1) * 128, :], sw[:, 2:4],
                      reads=[("slw", b2, 2), ("slw", b2, 3)], writes=[("SLW", bb, t)])
                if t == ntile - 1:
                    S.op("dve", lambda e: e.tensor_copy(cnti, cnt), reads=["cnt"], writes=["cnti"])
                    S.dma("sp", self.CNT[bb:bb + 1, :], cnti[0:1, :], reads=["cnti"], writes=[("CNT", bb)])

        self.pipeline(ntot, [(stL, 2), (stN, -1), (stY, 0), (stTo, 1), (stTh, -2)])

    def phaseD0(self):
        S, A, I = self.S, self.A, self.I
        self.new_phase()
        NCH = DFF // 128
        Wg = A.bf(KC * DFF).rearrange("p (k n) -> p k n", k=KC)
        Wu = A.bf(KC * DFF).rearrange("p (k n) -> p k n", k=KC)
        Wd = A.bf(NCH * D).rearrange("p (c n) -> p c n", c=NCH)
        for n0 in range(0, DFF, 512):
            n1 = min(n0 + 512, DFF)
            for W, nm in ((Wg, "l0_w_gate"), (Wu, "l0_w_up")):
                S.dma("pool", W[:, :, n0:n1], I[nm][:, n0:n1].rearrange("(k p) n -> p k n", p=128),
                      writes=[(nm, n0)])
        wd_src = I["l0_w_down"].rearrange("(c p) n -> p c n", p=128)
        for c0 in range(0, NCH, 4):
            c1 = min(c0 + 4, NCH)
            S.dma("pool", Wd[:, c0:c1, :], wd_src[:, c0:c1, :], writes=[("wd", c0)])
        wg_toks = [("l0_w_gate", n0) for n0 in range(0, DFF, 512)]
        wu_toks = [("l0_w_up", n0) for n0 in range(0, DFF, 512)]
        wd_toks = [("wd", c0) for c0 in range(0, NCH, 4)]
        G2c = A.f32(D)
        G2l = A.f32(D)
        self.bc_row(G2c, self.MOD[0, 2, 5 * D:6 * D], [], "G2c")
        hT = [A.bf(KC * 512).rearrange("p (k n) -> p k n", k=KC) for _ in range(2)]
        AT = A.bf(NCH * 512).rearrange("p (c n) -> p c n", c=NCH)
        sg = [A.bf(512) for _ in range(2)]
        xts = [A.f32(D) for _ in range(2)]
        x2s = [A.f32(D) for _ in range(2)]
        tmp = A.f32(512)
        pG = [self.pbank(0), self.pbank(1)]
        pU = [self.pbank(2), self.pbank(3)]
        pY = [self.pbank(4 + i) for i in range(4)]
        ngroups = NB * NT // 4
        ti = 0
        yi = 0
        cur_bb = -1
        for g in range(ngroups):
            hb = g % 2
            S.dma("sp", hT[hb], self.H2T[:, :, g * 512:(g + 1) * 512].rearrange("k p n -> p k n"),
                  writes=[("hT", hb)])
            for ci in range(NCH):
                pb = ci % 2
                for k in range(KC):
                    S.mm(pG[pb], Wg[:, k, ci * 128:(ci + 1) * 128], hT[hb][:, k, :], k == 0, k == KC - 1,
                         reads=[("hT", hb), wg_toks[ci // 4]], writes=[("pG", pb)])
                for k in range(KC):
                    S.mm(pU[pb], Wu[:, k, ci * 128:(ci + 1) * 128], hT[hb][:, k, :], k == 0, k == KC - 1,
                         reads=[("hT", hb), wu_toks[ci // 4]], writes=[("pU", pb)])
                S.op("act", lambda e, pb=pb: e.activation(sg[pb], pG[pb], AF.Silu), reads=[("pG", pb)],
                     writes=[("sg", pb)])
                S.op("dve", lambda e, pb=pb, ci=ci: e.tensor_tensor(AT[:, ci, :], sg[pb], pU[pb], ALU.mult),
                     reads=[("sg", pb), ("pU", pb)], writes=[("AT", ci)])
            at_toks = [("AT", ci) for ci in range(NCH)]
            for j in range(4):
                bb, t = divmod(4 * g + j, NT)
                lat = t < NTL
                if lat and bb != cur_bb:
                    cur_bb = bb
                    self.bc_row(G2l, self.MOD[0, bb, 5 * D:6 * D], [], "G2l")
                b2 = ti % 2
                ti += 1
                S.dma("sp", xts[b2], self.XR1[bb, t * 128:(t + 1) * 128, :], writes=[("xt", b2)])
                G2, gtok = (G2l, "G2l") if lat else (G2c, "G2c")
                for hf in range(2):
                    yb = yi % 4
                    yi += 1
                    for ci in range(NCH):
                        S.mm(pY[yb], AT[:, ci, j * 128:(j + 1) * 128], Wd[:, ci, hf * 512:(hf + 1) * 512],
                             ci == 0, ci == NCH - 1, reads=[("AT", ci), wd_toks[ci // 4]], writes=[("pY", yb)])
                    S.op("dve", lambda e, yb=yb, hf=hf, G2=G2: e.tensor_tensor(
                        tmp, pY[yb], G2[:, hf * 512:(hf + 1) * 512], ALU.mult),
                        reads=[("pY", yb), gtok], writes=["tmp"])
                    S.op("dve", lambda e, hf=hf, b2=b2: e.tensor_tensor(
                        x2s[b2][:, hf * 512:(hf + 1) * 512], tmp, xts[b2][:, hf * 512:(hf + 1) * 512], ALU.add),
                        reads=["tmp", ("xt", b2)], writes=[("x2", b2)])
                S.dma("sp", self.XR2[bb, t * 128:(t + 1) * 128, :], x2s[b2], reads=[("x2", b2)],
                      writes=[("XR2", bb, t)])

    def phaseB1(self):
        S, A, I = self.S, self.A, self.I
        self.new_phase()
        lam_init = 0.8 - 0.6 * math.exp(-0.3 * 1)
        lv = A.f32(4 * 64).rearrange("p (a n) -> p a n", a=4)
        for a, nm in enumerate(("q1", "k1", "q2", "k2")):
            self.bc_row(lv[:, a, :], I["l1_lambda_" + nm], [], ("lv", a))
        lp = A.f32(2 * 64).rearrange("p (a n) -> p a n", a=2)
        ls = A.f32(8)
        for a in range(2):
            S.op("dve", lambda e, a=a: e.tensor_tensor(lp[:, a, :], lv[:, 2 * a, :], lv[:, 2 * a + 1, :], ALU.mult),
                 reads=[("lv", 2 * a), ("lv", 2 * a + 1)], writes=[("lp", a)])
            S.op("dve", lambda e, a=a: e.reduce_sum(ls[:, a:a + 1], lp[:, a, :], AX.X), reads=[("lp", a)],
                 writes=[("ls", a)])
        S.op("act", lambda e: e.activation(ls[:, 2:4], ls[:, 0:2], AF.Exp), reads=[("ls", 0), ("ls", 1)],
             writes=["lexp"])
        S.op("dve", lambda e: e.tensor_tensor(ls[:, 4:5], ls[:, 3:4], ls[:, 2:3], ALU.subtract), reads=["lexp"],
             writes=["ldiff"])
        S.op("dve", lambda e: e.tensor_scalar(ls[:, 5:6], ls[:, 4:5], -lam_init, None, ALU.add), reads=["ldiff"],
             writes=["nlam"])
        nlam = ls[:, 5:6]
        SUB = A.f32(128)
        self.bc_row(SUB, I["l1_subln"], [], "SUB0")
        S.op("dve", lambda e: e.tensor_scalar(SUB, SUB, 1.0 - lam_init, None, ALU.mult), reads=["SUB0"], writes=["SUB"])
        QTh = [A.bf(SEQ) for _ in range(2)]
        KTh = [A.bf(T) for _ in range(2)]
        Vh = [A.bf(NT * 129).rearrange("p (t d) -> p t d", t=NT) for _ in range(2)]
        NEB = 4
        E = [A.bf(512) for _ in range(NEB)]
        accS = [A.f32(8 * 129) for _ in range(2)]
        ob = A.f32(4 * 128).rearrange("p (q n) -> p q n", q=4)
        o1 = A.f32(128)
        sq = A.f32(128)
        rr = A.f32(16)
        Oc = [A.bf(NTL * 128).rearrange("p (t n) -> p t n", t=NTL) for _ in range(2)]
        pS = [self.pbank(i) for i in range(4)]

        def acc(s_):
            b = 4 + s_ // 3
            off = (s_ % 3) * 129
            return self.psum[:, b, off:off + 129]

        it = 0
        si = 0
        ci = 0
        for bb in range(NB):
            for h in range(KC):
                b2 = it % 2
                it += 1
                S.dma("sp", QTh[b2], self.QT[bb, h, :, 0:SEQ], writes=[("QTh", b2)])
                S.dma("sp", KTh[b2], self.KT[bb, h], writes=[("KTh", b2)])
                S.dma("sp", Vh[b2], self.VS[bb, :, h * 129:(h + 1) * 129].rearrange("(t p) w -> p t w", p=128),
                      writes=[("Vh", b2)])
                LA = 2
                steps = [(qc, kt, m) for qc in range(4) for kt in range(NT) for m in range(2)]
                meta = {}

                def st1(qc, kt, m, b2=b2):
                    nonlocal si
                    sb = si % 4
                    eb = si % NEB
                    si += 1
                    meta[(qc, kt, m)] = eb
                    pr = slice(64 * m, 64 * m + 64)
                    S.mm(pS[sb], KTh[b2][pr, kt * 128:(kt + 1) * 128], QTh[b2][pr, qc * 512:(qc + 1) * 512],
                         True, True, reads=[("KTh", b2), ("QTh", b2)], writes=[("pS", sb)])
                    S.op("act", lambda e, sb=sb, eb=eb: e.activation(E[eb], pS[sb], AF.Exp),
                         reads=[("pS", sb)], writes=[("E", eb)])

                def st2(qc, kt, m, b2=b2):
                    eb = meta[(qc, kt, m)]
                    for qt in range(4):
                        sl = m * 4 + qt
                        S.mm(acc(sl), E[eb][:, qt * 128:(qt + 1) * 128], Vh[b2][:, kt, :],
                             kt == 0 and sl % 3 == 0, kt == NT - 1, reads=[("E", eb), ("Vh", b2)],
                             writes=[("acc", sl)], skip_group_check=True)
                    if kt == NT - 1 and m == 1:
                        epilogue(qc, b2)

                def epilogue(qc, b2):
                    nonlocal ci
                    ab = ci % 2
                    ci += 1
                    aS = accS[ab]
                    for bk in range(3):
                        n = 387 if bk < 2 else 258
                        S.op("dve", lambda e, bk=bk, n=n, aS=aS: e.tensor_copy(aS[:, bk * 387:bk * 387 + n],
                                                                             self.psum[:, 4 + bk, 0:n]),
                             reads=[("acc", 3 * bk + j) for j in range(3) if 3 * bk + j < 8], writes=[("accS", ab, bk)])
                    atoks = [("accS", ab, bk) for bk in range(3)]
                    a3 = aS.rearrange("p (s n) -> p s n", n=129)
                    S.op("dve", lambda e, a3=a3: e.reciprocal(rr[:, 0:8], a3[:, :, 128]), reads=atoks, writes=["rr"])
                    S.op("dve", lambda e: e.tensor_scalar(rr[:, 8:12], rr[:, 4:8], nlam, None, ALU.mult),
                         reads=["rr", "nlam"], writes=["rr2"])
                    for qt in range(4):
                        S.op("dve", lambda e, qt=qt, a3=a3: e.tensor_scalar(o1, a3[:, qt, 0:128], rr[:, qt:qt + 1], None,
                                                                        ALU.mult), reads=atoks + ["rr"], writes=["o1"])
                        S.op("dve", lambda e, qt=qt, a3=a3: e.scalar_tensor_tensor(
                            ob[:, qt, :], a3[:, 4 + qt, 0:128], rr[:, 8 + qt:9 + qt], o1, ALU.mult, ALU.add),
                            reads=atoks + ["rr2", "o1"], writes=[("ob", qt)])
                        S.op("dve", lambda e, qt=qt: e.tensor_tensor(sq, ob[:, qt, :], ob[:, qt, :], ALU.mult),
                             reads=[("ob", qt)], writes=["sq"])
                        S.op("dve", lambda e, qt=qt: e.reduce_sum(rr[:, 12 + qt:13 + qt], sq, AX.X), reads=["sq"],
                             writes=[("ss", qt)])
                    sst = [("ss", qt) for qt in range(4)]
                    S.op("act", lambda e: e.activation(rr[:, 12:16], rr[:, 12:16], AF.Ln, bias=EPS, scale=1.0 / 128),
                         reads=sst, writes=["lnv"])
                    S.op("act", lambda e: e.activation(rr[:, 12:16], rr[:, 12:16], AF.Exp, scale=-0.5), reads=["lnv"],
                         writes=["rstd"])
                    for qt in range(4):
                        S.op("dve", lambda e, qt=qt, b2=b2, qc=qc: e.scalar_tensor_tensor(
                            Oc[b2][:, qc * 4 + qt, :], ob[:, qt, :], rr[:, 12 + qt:13 + qt], SUB, ALU.mult, ALU.mult),
                            reads=[("ob", qt), "rstd", "SUB"], writes=[("Oc", b2)])

                for idx in range(len(steps) + LA):
                    if idx < len(steps):
                        st1(*steps[idx])
                    if idx >= LA:
                        st2(*steps[idx - LA])
                S.dma("sp", self.OS[bb, 0:SEQ, h * 128:(h + 1) * 128].rearrange("(t p) n -> p t n", p=128),
                      Oc[b2], reads=[("Oc", b2)], writes=[("OS", bb)])

    def phaseD1(self):
        S, A, I = self.S, self.A, self.I
        self.new_phase()
        NG = DFE // 512
        GS = MOE_GS
        NSG = CAP // GS
        TPG = GS // 128
        NST = CAP // 128
        hTe = A.bf(KC * CAP).rearrange("p (k n) -> p k n", k=KC)
        Ye = A.f32(NST * D).rearrange("p (t n) -> p t n", t=NST)
        wg = [A.bf(KC * 512).rearrange("p (k n) -> p k n", k=KC) for _ in range(2)]
        wu = [A.bf(KC * 512).rearrange("p (k n) -> p k n", k=KC) for _ in range(2)]
        wd = [A.bf(4 * D).rearrange("p (c n) -> p c n", c=4) for _ in range(2)]
        AT = [A.bf(4 * GS).rearrange("p (c n) -> p c n", c=4) for _ in range(2)]
        sg = [A.bf(GS) for _ in range(2)]
        hs = [A.bf(D) for _ in range(3)]
        pG = [self.pbank(0)[:, 0:GS], self.pbank(1)[:, 0:GS]]
        pU = [self.pbank(2)[:, 0:GS], self.pbank(3)[:, 0:GS]]
        pY = [self.pbank(4 + i) for i in range(3)]
        pT = self.pbank_bf(7).rearrange("p (k n) -> p k n", k=KC)
        wi = 0
        ai = 0
        yi = 0
        pi = 0
        hi = 0
        S.load_counts(self.CNT.rearrange("(o b) e -> o (b e)", o=1), NB * NE, reads=[("CNT", 0), ("CNT", 1)])
        for bb in range(NB):
            for ex in range(NE):
                for st in range(NST):
                    hb = hi % 3
                    hi += 1
                    S.dma("sp", hs[hb], self.H2S[bb][ex * CAP + st * 128:ex * CAP + (st + 1) * 128, :],
                          writes=[("hs", hb)])
                    S.guard(st * 128, bb * NE + ex)
                    for k in range(KC):
                        S.tr(pT[:, k, :], hs[hb][:, k * 128:(k + 1) * 128], self.ident, reads=[("hs", hb), "ident"],
                             writes=["pT"])
                    S.op("act", lambda e, st=st: e.copy(hTe[:, :, st * 128:(st + 1) * 128], pT), reads=["pT"],
                         writes=[("hTe", st)])
                    S.end_guard()
                for g in range(NG):
                    wb = wi % 2
                    wi += 1
                    S.dma("pool", wg[wb], I["l1_w_gate"][ex][:, g * 512:(g + 1) * 512].rearrange("(k p) n -> p k n", p=128),
                          writes=[("wg", wb)])
                    S.dma("pool", wu[wb], I["l1_w_up"][ex][:, g * 512:(g + 1) * 512].rearrange("(k p) n -> p k n", p=128),
                          writes=[("wu", wb)])
                    S.dma("pool", wd[wb], I["l1_w_down"][ex][g * 512:(g + 1) * 512, :].rearrange("(c p) n -> p c n", p=128),
                          writes=[("wd", wb)])
                    for sgp in range(NSG):
                        S.guard(sgp * GS, bb * NE + ex)
                        ab = ai % 2
                        ai += 1
                        htoks = [("hTe", sgp * TPG + j) for j in range(TPG)]
                        for ci in range(4):
                            pb = pi % 2
                            pi += 1
                            for k in range(KC):
                                S.mm(pG[pb], wg[wb][:, k, ci * 128:(ci + 1) * 128], hTe[:, k, sgp * GS:(sgp + 1) * GS],
                                     k == 0, k == KC - 1, reads=htoks + [("wg", wb)], writes=[("pG", pb)])
                            for k in range(KC):
                                S.mm(pU[pb], wu[wb][:, k, ci * 128:(ci + 1) * 128], hTe[:, k, sgp * GS:(sgp + 1) * GS],
                                     k == 0, k == KC - 1, reads=htoks + [("wu", wb)], writes=[("pU", pb)])
                            S.op("act", lambda e, pb=pb: e.activation(sg[pb], pG[pb], AF.Silu), reads=[("pG", pb)],
                                 writes=[("sg", pb)])
                            S.op("dve", lambda e, pb=pb, ci=ci, ab=ab: e.tensor_tensor(AT[ab][:, ci, :], sg[pb], pU[pb],
                                                                                   ALU.mult),
                                 reads=[("sg", pb), ("pU", pb)], writes=[("AT", ab)])
                        for j in range(TPG):
                            tile = sgp * TPG + j
                            for hf in range(2):
                                yb = yi % 3
                                yi += 1
                                for ci in range(4):
                                    S.mm(pY[yb], AT[ab][:, ci, j * 128:(j + 1) * 128], wd[wb][:, ci, hf * 512:(hf + 1) * 512],
                                         ci == 0, ci == 3, reads=[("AT", ab), ("wd", wb)], writes=[("pY", yb)])
                                ysl = Ye[:, tile, hf * 512:(hf + 1) * 512]
                                if g == 0:
                                    S.op("dve", lambda e, ysl=ysl, yb=yb: e.tensor_copy(ysl, pY[yb]),
                                         reads=[("pY", yb)], writes=[("Ye", tile, hf)])
                                else:
                                    S.op("dve", lambda e, ysl=ysl, yb=yb: e.tensor_tensor(ysl, ysl, pY[yb], ALU.add),
                                         reads=[("pY", yb), ("Ye", tile, hf)], writes=[("Ye", tile, hf)])
                        S.end_guard()
                for st in range(NST):
                    S.dma("sp", self.OUTS[bb][ex * CAP + st * 128:ex * CAP + (st + 1) * 128, :], Ye[:, st, :],
                          reads=[("Ye", st, 0), ("Ye", st, 1)], writes=[("OUTS", bb)])
        if not SKIP_E:
            self.phaseE()

    def phaseE(self):
        S, A, I = self.S, self.A, self.I
        self.new_phase()
        G2l = A.f32(D)
        fin = A.f32(D)
        self.bc_row(fin, I["final_norm"], [], "fin")
        sli = [A.f32(2).bitcast(I32) for _ in range(2)]
        slw = [A.f32(2) for _ in range(2)]
        r1 = [A.f32(D) for _ in range(2)]
        r2 = [A.f32(D) for _ in range(2)]
        xts = [A.f32(D) for _ in range(2)]
        x2s = [A.f32(D) for _ in range(2)]
        outs = [A.f32(D) for _ in range(2)]
        junk = A.bf(D)
        stt = A.f32(4)
        ti = 0
        for bb in range(NB):
            self.bc_row(G2l, self.MOD[1, bb, 5 * D:6 * D], [], "G2l")
            for tile in range(NTL):
                b2 = ti % 2
                ti += 1
                rows = slice(tile * 128, (tile + 1) * 128)
                S.dma("sp", sli[b2], self.SLI[bb, rows, :], writes=[("sli", b2)])
                S.dma("sp", slw[b2], self.SLW[bb, rows, :], writes=[("slw", b2)])
                S.dma("sp", xts[b2], self.XR1[bb, rows, :], writes=[("xt", b2)])
                for which, rr in enumerate((r1, r2)):
                    S.op("pool", lambda e, bb=bb, b2=b2, which=which, rr=rr: e.indirect_dma_start(
                        out=rr[b2], out_offset=None, in_=self.OUTS[bb],
                        in_offset=bass.IndirectOffsetOnAxis(ap=sli[b2][:, which:which + 1], axis=0)),
                        reads=[("sli", b2)], writes=[("r", which, b2)], dma=True)
                S.op("dve", lambda e, b2=b2: e.tensor_scalar(x2s[b2], r1[b2], slw[b2][:, 0:1], None, ALU.mult),
                     reads=[("r", 0, b2), ("slw", b2)], writes=[("y", b2)])
                S.op("dve", lambda e, b2=b2: e.scalar_tensor_tensor(x2s[b2], r2[b2], slw[b2][:, 1:2], x2s[b2],
                                                                   ALU.mult, ALU.add),
                     reads=[("r", 1, b2), ("slw", b2), ("y", b2)], writes=[("y2", b2)])
                S.op("dve", lambda e, b2=b2: e.tensor_tensor(x2s[b2], x2s[b2], G2l, ALU.mult),
                     reads=[("y2", b2), "G2l"], writes=[("x2a", b2)])
                S.op("dve", lambda e, b2=b2: e.tensor_tensor(x2s[b2], x2s[b2], xts[b2], ALU.add),
                     reads=[("x2a", b2), ("xt", b2)], writes=[("x2", b2)])
                if self.dbg:
                    S.dma("sp", self.XR2[bb, rows, :], x2s[b2], reads=[("x2", b2)], writes=[("XR2", bb, tile)])
                S.op("act", lambda e, b2=b2: e.activation(junk, x2s[b2], AF.Square, accum_out=stt[:, 0:1]),
                     reads=[("x2", b2)], writes=["junk", "ssq"])
                S.op("act", lambda e: e.activation(stt[:, 1:2], stt[:, 0:1], AF.Sqrt, bias=EPS, scale=1.0 / D),
                     reads=["ssq"], writes=["std"])
                S.op("dve", lambda e: e.reciprocal(stt[:, 2:3], stt[:, 1:2]), reads=["std"], writes=["rstd"])
                S.op("dve", lambda e, b2=b2: e.scalar_tensor_tensor(outs[b2], x2s[b2], stt[:, 2:3], fin, ALU.mult, ALU.mult),
                     reads=[("x2", b2), "rstd", "fin"], writes=[("out", b2)])
                S.dma("sp", self.out[bb, rows, :], outs[b2], reads=[("out", b2)], writes=[("OUT", bb, tile)])


def _const_tables():
    ident = np.eye(128, dtype=np.float32)
    pos = np.arange(SEQ)
    row = (pos // GRID_W).astype(np.float64)
    col = (pos % GRID_W).astype(np.float64)
    inv = 10000.0 ** (-np.arange(0, 32, 2, dtype=np.float64) / 32.0)
    ar = row[:, None] * inv[None]
    ac = col[:, None] * inv[None]
    cs = np.concatenate([np.cos(ar), np.cos(ar), np.cos(ac), np.cos(ac)], -1)
    sn = np.concatenate([-np.sin(ar), np.sin(ar), -np.sin(ac), np.sin(ac)], -1)
    cs = cs.reshape(NTL, 128, 64).transpose(1, 0, 2).astype(np.float32)
    sn = sn.reshape(NTL, 128, 64).transpose(1, 0, 2).astype(np.float32)
    return ident, np.ascontiguousarray(cs), np.ascontiguousarray(sn)


NA_VAR_TILES = (0, 1, 5, 14, 15)


def _na_index():
    kp = np.arange(128)
    kr, kc = kp // 64, kp % 64
    qp = np.arange(128)
    qr, qc = qp // 64, qp % 64
    dr = np.zeros((5, 128, 5, 128), np.int64)
    dc = np.zeros((5, 128, 5, 128), np.int64)
    ok = np.zeros((5, 128, 5, 128), bool)
    for v, i in enumerate(NA_VAR_TILES):
        j0 = min(max(i - 2, 0), 11)
        for jj in range(5):
            rk = 2 * (j0 + jj) + kr
            r = 2 * i + qr
            rs = np.clip(r - 4, 0, 24)
            vrow = (rk[:, None] >= rs[None, :]) & (rk[:, None] < rs[None, :] + 8)
            ws = np.clip(qc - 8, 0, 48)
            vcol = (kc[:, None] >= ws[None, :]) & (kc[:, None] < ws[None, :] + 16)
            val = vrow & vcol
            ok[v, :, jj, :] = val
            dr[v, :, jj, :] = np.where(val, rk[:, None] - r[None, :] + 7, 0)
            dc[v, :, jj, :] = np.where(val, kc[:, None] - qc[None, :] + 15, 0)
    return dr, dc, ok


def _na_bias(rpb):
    dr, dc, ok = _na_index()
    tab = rpb[:, dr, dc]
    tab = np.where(ok[None], tab, np.float32(-30000.0))
    tab = tab.transpose(1, 0, 2, 3, 4).reshape(5, 16, 128, 640)
    return np.ascontiguousarray(tab.astype(np.float32))


def make_in_maps(inputs, n_cores=8):
    ident, cs, sn = _const_tables()
    nab = _na_bias(np.asarray(inputs["l0_rpb"], np.float32))
    shared = {}
    for k, v in inputs.items():
        if k in ("x", "c", "ctx", "l0_rpb"):
            continue
        shared[k] = np.ascontiguousarray(np.asarray(v, np.float32))
    shared["ident"] = ident
    shared["ltri"] = np.triu(np.ones((128, 128), np.float32), 1)
    shared["ecap"] = np.tile((np.arange(NE, dtype=np.float32) * CAP)[None, :], (128, 1))
    shared["rope_cs"] = cs
    shared["rope_sn"] = sn
    shared["na_bias"] = nab
    maps = []
    for c in range(n_cores):
        m = dict(shared)
        m["x"] = np.ascontiguousarray(inputs["x"][c * NB:(c + 1) * NB])
        m["ctx"] = np.ascontiguousarray(inputs["ctx"][c * NB:(c + 1) * NB])
        m["c"] = np.ascontiguousarray(inputs["c"][c * NB:(c + 1) * NB])
        maps.append(m)
    return maps


_NC_CACHE = {}


def kernel(**inputs):
    if "nc" not in _NC_CACHE:
        _NC_CACHE["nc"] = Builder().build()
    nc = _NC_CACHE["nc"]
    maps = make_in_maps(inputs)
    res = run_bass_kernel_spmd(nc, maps, core_ids=list(range(8)))
    out = np.concatenate([np.asarray(r["out"]) for r in res.results], axis=0)
    return out.astype(np.float32, copy=False)
```
